# Optimizing a Trainium2 kernel written in Bass

```python
import math
import jax, jax.numpy as jnp
from jax import lax
import numpy as np

D_MODEL = 1024
BATCH = 8
SEQ = 2048
DEPTH = 2
DEC_BATCH = 128
DEC_SEQ = 4
PAST_LEN = 16384
PAGE_SIZE = 128

N_MIXERS = 2
N_MOD = 6
CHUNK_A = 128
GMLP_WIDTH = D_MODEL
GMLP_GROUPS = 4
GMLP_GROUP_W = GMLP_WIDTH // GMLP_GROUPS
GLA_HEADS = 4
GLA_DK = D_MODEL // 2
GLA_DV = D_MODEL
GLA_DK_HEAD = GLA_DK // GLA_HEADS
GLA_DV_HEAD = GLA_DV // GLA_HEADS
GLA_GATE_RANK = 16
GLA_TAU = 16.0
GLA_CHUNK = 64
GLA_IN = 2 * GLA_DK + 2 * GLA_DV + GLA_GATE_RANK
D_FF = 4 * D_MODEL
EPS = 1e-6

kernel_name = "hybrid_gmlp_gla_adaln_step"


def rms_norm(x, g):
    xf = x.astype(jnp.float32)
    y = xf * lax.rsqrt(jnp.mean(xf * xf, axis=-1, keepdims=True) + EPS)
    return (y * g.astype(jnp.float32)).astype(x.dtype)


def layer_norm(x, g, b):
    xf = x.astype(jnp.float32)
    mu = jnp.mean(xf, axis=-1, keepdims=True)
    var = jnp.mean(jnp.square(xf - mu), axis=-1, keepdims=True)
    y = (xf - mu) * lax.rsqrt(var + EPS)
    return (y * g.astype(jnp.float32) + b.astype(jnp.float32)).astype(x.dtype)


def gmlp_mixer(h, w_in, b_in, ln_g, ln_b, w_s, b_s, w_out, b_out):
    B, T, _ = h.shape
    z = jax.nn.gelu(h @ w_in + b_in)
    u, v = jnp.split(z, 2, axis=-1)
    v = layer_norm(v, ln_g, ln_b)
    n_chunks = -(-T // CHUNK_A)
    pad = n_chunks * CHUNK_A - T
    vp = jnp.pad(v, ((0, 0), (0, pad), (0, 0))).reshape(B, n_chunks, CHUNK_A, GMLP_GROUPS, GMLP_GROUP_W)
    causal = jnp.tril(jnp.ones((CHUNK_A, CHUNK_A), dtype=bool))
    ws = jnp.where(causal[None], w_s, jnp.zeros_like(w_s))
    s = jnp.einsum('gij,bnjgc->bnigc', ws, vp) + b_s.T[None, None, :, :, None]
    s = s.reshape(B, n_chunks * CHUNK_A, GMLP_WIDTH)[:, :T]
    y = (u * s) @ w_out + b_out
    return y, v


def gla_recurrence(q, k, v, log_a, s0):
    B, T, H, dk = q.shape
    dv = v.shape[-1]
    C = math.gcd(T, GLA_CHUNK)
    N = T // C
    f32 = jnp.float32

    def blocks(t):
        return t.astype(f32).reshape(B, N, C, H, t.shape[-1]).transpose(1, 0, 3, 2, 4)

    causal = jnp.tril(jnp.ones((C, C), dtype=bool))[..., None]

    def step(S, inp):
        qc, kc, vc, lac = inp
        b = jnp.cumsum(lac, axis=-2)
        o_inter = jnp.einsum('bhik,bhkv->bhiv', qc * jnp.exp(b), S)
        diff = b[:, :, :, None, :] - b[:, :, None, :, :]
        decay = jnp.exp(jnp.where(causal, diff, -jnp.inf))
        attn = jnp.einsum('bhik,bhjk,bhijk->bhij', qc, kc, decay)
        o_intra = jnp.einsum('bhij,bhjv->bhiv', attn, vc)
        b_last = b[:, :, -1:, :]
        S_new = jnp.exp(b_last[:, :, 0, :])[..., None] * S + jnp.einsum(
            'bhjk,bhjv->bhkv', kc * jnp.exp(b_last - b), vc)
        return S_new, o_inter + o_intra

    S_fin, o = lax.scan(step, s0.astype(f32), (blocks(q), blocks(k), blocks(v), blocks(log_a)))
    o = o.transpose(1, 0, 3, 2, 4).reshape(B, T, H, dv)
    return o, S_fin


def gla_mixer(h, s0, w_in, w_gate2, b_gate, norm_g, w_out):
    B, T, _ = h.shape
    proj = h @ w_in
    q, k, v, g, a_lr = jnp.split(
        proj, [GLA_DK, 2 * GLA_DK, 2 * GLA_DK + GLA_DV, 2 * GLA_DK + 2 * GLA_DV], axis=-1)
    log_a = jax.nn.log_sigmoid((a_lr @ w_gate2 + b_gate).astype(jnp.float32)) / GLA_TAU
    q = q.reshape(B, T, GLA_HEADS, GLA_DK_HEAD) * (GLA_DK_HEAD ** -0.5)
    k = k.reshape(B, T, GLA_HEADS, GLA_DK_HEAD)
    v = v.reshape(B, T, GLA_HEADS, GLA_DV_HEAD)
    log_a = log_a.reshape(B, T, GLA_HEADS, GLA_DK_HEAD)
    o, S_fin = gla_recurrence(q, k, v, log_a, s0)
    o = rms_norm(o.astype(h.dtype), norm_g).reshape(B, T, GLA_DV)
    y = (o * jax.nn.silu(g)) @ w_out
    return y, S_fin.astype(s0.dtype)


def sqrelu_mlp(h, w1, b1, w2, b2):
    return jnp.square(jax.nn.relu(h @ w1 + b1)) @ w2 + b2


def trunk(x, c, s0, ada_w, ada_b, norm_mix_g, norm_ffn_g, ffn_w1, ffn_b1, ffn_w2, ffn_b2,
          gmlp_w_in, gmlp_b_in, gmlp_ln_g, gmlp_ln_b, gmlp_w_s, gmlp_b_s, gmlp_w_out, gmlp_b_out,
          gla_w_in, gla_w_gate2, gla_b_gate, gla_norm_g, gla_w_out, final_norm_g):
    chunk_v = None
    s_fin = None
    sc = jax.nn.silu(c)
    for i in range(DEPTH):
        mod = sc @ ada_w[i] + ada_b[i]
        sh1, sc1, g1, sh2, sc2, g2 = [m[:, None, :] for m in jnp.split(mod, N_MOD, axis=-1)]
        h = rms_norm(x, norm_mix_g[i]) * (1 + sc1) + sh1
        if i % N_MIXERS == 0:
            y, chunk_v = gmlp_mixer(h, gmlp_w_in, gmlp_b_in, gmlp_ln_g, gmlp_ln_b,
                                    gmlp_w_s, gmlp_b_s, gmlp_w_out, gmlp_b_out)
        else:
            y, s_fin = gla_mixer(h, s0, gla_w_in, gla_w_gate2, gla_b_gate, gla_norm_g, gla_w_out)
        x = x + g1 * y
        h = rms_norm(x, norm_ffn_g[i]) * (1 + sc2) + sh2
        x = x + g2 * sqrelu_mlp(h, ffn_w1[i], ffn_b1[i], ffn_w2[i], ffn_b2[i])
    return rms_norm(x, final_norm_g), chunk_v, s_fin


def setup_inputs(seed: int = 0) -> dict:
    key = jax.random.key(seed)
    ks = jax.random.split(key, 30)
    f32 = jnp.float32

    def nrm(k, shape, scale):
        return jax.random.normal(k, shape, f32) * scale

    D = D_MODEL
    return {
        "x_prompt": nrm(ks[0], (BATCH, SEQ, D), 1.0),
        "x_sample": nrm(ks[1], (DEC_BATCH, DEC_SEQ, D), 1.0),
        "c_prompt": nrm(ks[2], (BATCH, D), 1.0),
        "c_sample": nrm(ks[3], (DEC_BATCH, D), 1.0),
        "state_gla": nrm(ks[4], (DEC_BATCH, GLA_HEADS, GLA_DK_HEAD, GLA_DV_HEAD), 0.5),
        "ada_w": nrm(ks[5], (DEPTH, D, N_MOD * D), 0.5 * D ** -0.5),
        "ada_b": nrm(ks[6], (DEPTH, N_MOD * D), 0.02),
        "norm_mix_g": 1.0 + nrm(ks[7], (DEPTH, D), 0.02),
        "norm_ffn_g": 1.0 + nrm(ks[8], (DEPTH, D), 0.02),
        "ffn_w1": nrm(ks[9], (DEPTH, D, D_FF), D ** -0.5),
        "ffn_b1": nrm(ks[10], (DEPTH, D_FF), 0.02),
        "ffn_w2": nrm(ks[11], (DEPTH, D_FF, D), D_FF ** -0.5),
        "ffn_b2": nrm(ks[12], (DEPTH, D), 0.02),
        "gmlp_w_in": nrm(ks[13], (D, 2 * GMLP_WIDTH), D ** -0.5),
        "gmlp_b_in": nrm(ks[14], (2 * GMLP_WIDTH,), 0.02),
        "gmlp_ln_g": 1.0 + nrm(ks[15], (GMLP_WIDTH,), 0.02),
        "gmlp_ln_b": nrm(ks[16], (GMLP_WIDTH,), 0.02),
        "gmlp_w_s": nrm(ks[17], (GMLP_GROUPS, CHUNK_A, CHUNK_A), CHUNK_A ** -0.5),
        "gmlp_b_s": 1.0 + nrm(ks[18], (GMLP_GROUPS, CHUNK_A), 0.02),
        "gmlp_w_out": nrm(ks[19], (GMLP_WIDTH, D), GMLP_WIDTH ** -0.5),
        "gmlp_b_out": nrm(ks[20], (D,), 0.02),
        "gla_w_in": nrm(ks[21], (D, GLA_IN), D ** -0.5),
        "gla_w_gate2": nrm(ks[22], (GLA_GATE_RANK, GLA_DK), GLA_GATE_RANK ** -0.5),
        "gla_b_gate": nrm(ks[23], (GLA_DK,), 0.1),
        "gla_norm_g": 1.0 + nrm(ks[24], (GLA_DV_HEAD,), 0.02),
        "gla_w_out": nrm(ks[25], (GLA_DV, D), GLA_DV ** -0.5),
        "final_norm_g": 1.0 + nrm(ks[26], (D,), 0.02),
    }


def reference(x_prompt, x_sample, c_prompt, c_sample, state_gla, ada_w, ada_b, norm_mix_g, norm_ffn_g,
              ffn_w1, ffn_b1, ffn_w2, ffn_b2, gmlp_w_in, gmlp_b_in, gmlp_ln_g, gmlp_ln_b, gmlp_w_s,
              gmlp_b_s, gmlp_w_out, gmlp_b_out, gla_w_in, gla_w_gate2, gla_b_gate, gla_norm_g,
              gla_w_out, final_norm_g):
    s0_prompt = jnp.zeros((x_prompt.shape[0], GLA_HEADS, GLA_DK_HEAD, GLA_DV_HEAD), dtype=state_gla.dtype)
    y_prompt, _, state_gla_prompt = trunk(
        x_prompt, c_prompt, s0_prompt, ada_w, ada_b, norm_mix_g, norm_ffn_g, ffn_w1, ffn_b1, ffn_w2, ffn_b2,
        gmlp_w_in, gmlp_b_in, gmlp_ln_g, gmlp_ln_b, gmlp_w_s, gmlp_b_s, gmlp_w_out, gmlp_b_out,
        gla_w_in, gla_w_gate2, gla_b_gate, gla_norm_g, gla_w_out, final_norm_g)
    y_sample, state_chunk_v_sample, state_gla_sample = trunk(
        x_sample, c_sample, state_gla, ada_w, ada_b, norm_mix_g, norm_ffn_g, ffn_w1, ffn_b1, ffn_w2, ffn_b2,
        gmlp_w_in, gmlp_b_in, gmlp_ln_g, gmlp_ln_b, gmlp_w_s, gmlp_b_s, gmlp_w_out, gmlp_b_out,
        gla_w_in, gla_w_gate2, gla_b_gate, gla_norm_g, gla_w_out, final_norm_g)
    return (y_prompt, y_sample, state_gla_prompt, state_gla_sample, state_chunk_v_sample)
```

```python
import numpy as np
import concourse.bass as bass
import concourse.mybir as mybir
from concourse.bass_utils import run_bass_kernel_spmd

F32 = mybir.dt.float32
BF16 = mybir.dt.bfloat16
AF = mybir.ActivationFunctionType
ALU = mybir.AluOpType

D = 1024
NPR = 2048
NSA = 64
NTOK = NPR + NSA
NB = 17
EPS = 1e-6
TILES = [(0, 512, True), (512, 512, True), (1024, 512, True), (1536, 512, True), (2048, 64, False)]
NSLOT = 4
SAME_ENG_WAR = False

R_ADAB, R_NMG, R_NFG, R_B1, R_B2, R_BINU, R_BOUT, R_TOT = 0, 96, 112, 128, 192, 208, 216, 224


class Eng:
    def __init__(self, nc, name, h):
        self.name = name
        self.h = h
        self.sem = nc.alloc_semaphore("s_" + name)
        self.cnt = 0
        self.seen = {}
        self.raw_self = name in ("act", "dve", "pool")


class Buf:
    __slots__ = ("name", "w", "r")

    def __init__(self, name=""):
        self.name = name
        self.w = None
        self.r = {}


class Sched:
    def __init__(self, nc):
        self.nc = nc
        self.PE = Eng(nc, "pe", nc.tensor)
        self.ACT = Eng(nc, "act", nc.scalar)
        self.DVE = Eng(nc, "dve", nc.vector)
        self.SP = Eng(nc, "sp", nc.sync)
        self.POOL = Eng(nc, "pool", nc.gpsimd)
        self.engs = [self.PE, self.ACT, self.DVE, self.SP, self.POOL]
        self.dsems = [Eng(nc, "d%d" % i, None) for i in range(12)]
        self.wsems = [Eng(nc, "w%d" % i, None) for i in range(NSLOT)]
        self.xsem = Eng(nc, "wx", None)
        self.dnext = 0

    def _waits(self, eng, r, w):
        need = {}
        for b in r:
            if b.w is not None and (b.w[0] is not eng or eng.raw_self):
                need[b.w[0]] = max(need.get(b.w[0], 0), b.w[1])
        full = eng.raw_self and SAME_ENG_WAR
        for b in w:
            if b.w is not None and (b.w[0] is not eng or full):
                need[b.w[0]] = max(need.get(b.w[0], 0), b.w[1])
            for e, c in b.r.items():
                if e is not eng or full:
                    need[e] = max(need.get(e, 0), c)
        for e, c in need.items():
            if eng.seen.get(e, 0) < c:
                eng.h.wait_ge(e.sem, c)
                eng.seen[e] = c

    def op(self, eng, fn, r=(), w=()):
        self._waits(eng, r, w)
        ins = fn()
        eng.cnt += 1
        ins.then_inc(eng.sem, 1)
        for b in w:
            b.w = (eng, eng.cnt)
            b.r = {}
        for b in r:
            b.r[eng] = eng.cnt

    def mm(self, mms, r=(), w=()):
        eng = self.PE
        self._waits(eng, r, w)
        n = len(mms)
        ins = None
        for i, (o, l, rh) in enumerate(mms):
            ins = self.nc.tensor.matmul(o, lhsT=l, rhs=rh, start=(i == 0), stop=(i == n - 1))
        eng.cnt += 1
        ins.then_inc(eng.sem, 1)
        for b in w:
            b.w = (eng, eng.cnt)
            b.r = {}
        for b in r:
            b.r[eng] = eng.cnt

    def mm_multi(self, groups, r=(), w=()):
        eng = self.PE
        self._waits(eng, r, w)
        ins = None
        for mms in groups:
            n = len(mms)
            for i, (o, l, rh) in enumerate(mms):
                ins = self.nc.tensor.matmul(o, lhsT=l, rhs=rh, start=(i == 0), stop=(i == n - 1))
        eng.cnt += 1
        ins.then_inc(eng.sem, 1)
        for b in w:
            b.w = (eng, eng.cnt)
            b.r = {}
        for b in r:
            b.r[eng] = eng.cnt

    def transposes(self, items, r=(), w=()):
        eng = self.PE
        self._waits(eng, r, w)
        ins = None
        for (o, i_, ident) in items:
            ins = self.nc.tensor.transpose(o, i_, ident)
        eng.cnt += 1
        ins.then_inc(eng.sem, 1)
        for b in w:
            b.w = (eng, eng.cnt)
            b.r = {}
        for b in r:
            b.r[eng] = eng.cnt

    def dma(self, q, out, in_, r=(), w=(), sem=None):
        if sem is None:
            sem = self.dsems[self.dnext % len(self.dsems)]
            self.dnext += 1
        self._waits(q, r, w)
        ins = q.h.dma_start(out=out, in_=in_)
        sem.cnt += 16
        ins.then_inc(sem.sem, 16)
        for b in w:
            b.w = (sem, sem.cnt)
            b.r = {}
        for b in r:
            b.r[sem] = sem.cnt

    def wait_dmas(self, engs=None, with_x=False):
        engs = engs or [self.PE, self.ACT, self.DVE]
        for e in engs:
            for o in self.dsems + ([self.xsem] if with_x else []):
                if e.seen.get(o, 0) < o.cnt:
                    e.h.wait_ge(o.sem, o.cnt)
                    e.seen[o] = o.cnt

    def barrier(self, engs=None):
        engs = engs or [self.PE, self.ACT, self.DVE, self.SP]
        allp = [self.PE, self.ACT, self.DVE, self.SP] + self.dsems + [self.xsem]
        for e in engs:
            for o in allp:
                if o is e:
                    continue
                if e.seen.get(o, 0) < o.cnt:
                    e.h.wait_ge(o.sem, o.cnt)
                    e.seen[o] = o.cnt


class Arena:
    def __init__(self, nc, name, nbytes):
        self.t = nc.alloc_sbuf_tensor(name, [128, nbytes // 4], F32)
        self.nbytes = nbytes
        self.off = 0

    def reset(self):
        self.off = 0

    def alloc(self, shape, dtype, parts=128):
        esz = 4 if dtype == F32 else 2
        n = 1
        for s in shape:
            n *= s
        nb = (n * esz + 31) // 32 * 32
        assert self.off + nb <= self.nbytes, (self.off, nb, self.nbytes)
        o4 = self.off // 4
        ap = self.t[0:parts, o4:o4 + nb // 4]
        if dtype != F32:
            ap = ap.bitcast(dtype)
        ap = ap[:, 0:n]
        if len(shape) == 2:
            ap = ap.rearrange("p (a b) -> p a b", b=shape[1])
        elif len(shape) == 3:
            ap = ap.rearrange("p (a b c) -> p a b c", b=shape[1], c=shape[2])
        self.off += nb
        return ap


def build():
    nc = bass.Bass("TRN2", target_bir_lowering=False)
    S = Sched(nc)
    PE, ACT, DVE, SP, POOL = S.PE, S.ACT, S.DVE, S.SP, S.POOL

    def din(name, shape, dt=F32):
        return nc.dram_tensor(name, list(shape), dt, kind="ExternalInput").ap()

    def dout(name, shape):
        return nc.dram_tensor(name, list(shape), F32, kind="ExternalOutput").ap()

    xin = din("xin", [NTOK, D])
    cin = din("cin", [NB, D])
    sgl = din("sgl", [16, 128, 4, 256])
    ada_w = din("ada_w", [2, D, 6 * D])
    ffn_w1 = din("ffn_w1", [2, D, 4 * D])
    ffn_w2 = din("ffn_w2", [2, 4 * D, D])
    gmlp_w_in = din("gmlp_w_in", [D, 2 * D])
    gmlp_w_out = din("gmlp_w_out", [D, D])
    gla_w_in = din("gla_w_in", [D, 3088])
    gla_wa = din("gla_wa", [D, 17])
    gla_w_out = din("gla_w_out", [D, D])
    prow = din("prow", [R_TOT, 128])
    binv_bc = din("binv_bc", [128, D])
    lng_bc = din("lng_bc", [128, D])
    lnb_bc = din("lnb_bc", [128, D])
    fng_bc = din("fng_bc", [128, D])
    gng_bc = din("gng_bc", [128, 256])
    wsT_p = din("wsT_p", [128, 4, 128])
    wsT_s = din("wsT_s", [64, 4, 64])
    bs_p = din("bs_p", [128, 4, 128])
    bs_s = din("bs_s", [128, 4, 64])
    cm_p = din("cm_p", [128, 128])
    cm_s = din("cm_s", [64, 64])
    triN_p = din("triN_p", [128, 128])
    triR_p = din("triR_p", [128, 128])
    triN_s = din("triN_s", [64, 64])
    triR_s = din("triR_s", [64, 64])
    qmask = din("qmask", [128, 16, 64])
    kmask = din("kmask", [64, 16])
    wg2a = din("wg2a", [17, 512])
    ident_d = din("ident", [128, 128])

    y_prompt = dout("y_prompt", [NPR, D])
    y_sample = dout("y_sample", [NSA, D])
    st_prompt = dout("st_prompt", [128, 4, 256])
    st_sample = dout("st_sample", [16, 128, 4, 256])
    v_sample = dout("v_sample", [NSA, D])

    xT = nc.alloc_sbuf_tensor("xT", [128, 8, NTOK], F32)
    ring = nc.alloc_sbuf_tensor("ring", [128, NSLOT, 8, 1024], BF16)
    KA = Arena(nc, "KA", 10752)
    rem = nc.sbuf_bytes_remaining - 64
    WA = Arena(nc, "WA", (rem // 32) * 32)
    CA = WA
    EA = WA
    ps = nc.alloc_psum_tensor("ps", [128, 8, 512], F32)
    pb = [Buf("ps%d" % i) for i in range(8)]
    pstate = {"n": 0, "res": set()}

    def palloc(n=1):
        while True:
            if n == 2 and pstate["n"] % 2 == 1:
                pstate["n"] += 1
            b = pstate["n"] % 8
            pstate["n"] += n
            if all(((b + i) % 8) not in pstate["res"] for i in range(n)):
                return b

    ident = KA.alloc([128], F32)
    identb = KA.alloc([128], BF16)
    onesb = KA.alloc([128], BF16)
    PF = KA.alloc([R_TOT], F32)
    MODS = [KA.alloc([6, 8, NB], F32) for _ in range(2)]
    GB1 = KA.alloc([8, NB], F32)
    GB2S = [KA.alloc([8, NB], F32) for _ in range(2)]
    scT = KA.alloc([8, NB], BF16)
    wa = KA.alloc([8, 17], BF16)
    xTbs = [Buf("xT%d" % i) for i in range(17)]

    def xb(t0, n):
        return tuple(xTbs[i] for i in range(t0 // 128, (t0 + n + 127) // 128))
    b_const = Buf("const")
    b_mods = [Buf("mod0"), Buf("mod1")]

    def wview(ap2d):
        return ap2d.rearrange("(k p) n -> p k n", p=128)

    cdef = {}
    for l in range(2):
        for j in range(6):
            cdef["A%d_%d" % (l, j)] = wview(ada_w[l, :, j * 1024:(j + 1) * 1024])
        for q in range(4):
            cdef["F%d_1_%d" % (l, q)] = wview(ffn_w1[l, :, q * 1024:(q + 1) * 1024])
            cdef["F%d_2_%d" % (l, q)] = wview(ffn_w2[l, q * 1024:(q + 1) * 1024, :])
    cdef["GIN0"] = wview(gmlp_w_in[:, 0:1024])
    cdef["GIN1"] = wview(gmlp_w_in[:, 1024:2048])
    cdef["GOUT"] = wview(gmlp_w_out[:, :])
    for j in range(3):
        cdef["L%d" % j] = wview(gla_w_in[:, j * 1024:(j + 1) * 1024])
    cdef["LOUT"] = wview(gla_w_out[:, :])
    seq = ["A0_0", "A0_1", "GIN0", "GIN1", "A0_2", "GOUT", "A0_3", "A0_4", "A0_5"]
    for q in range(3):
        seq += ["F0_1_%d" % q, "F0_2_%d" % q]
    seq += ["A1_0", "A1_1", "F0_1_3", "F0_2_3", "A1_2", "A1_3", "A1_4", "A1_5", "L0", "L1", "L2", "LOUT"]
    for q in range(4):
        seq += ["F1_1_%d" % q, "F1_2_%d" % q]
    slotb = [Buf("slot%d" % i) for i in range(NSLOT)]
    wstate = {"next": 0, "free": list(range(NSLOT)), "slot": {}}

    def wpump():
        while wstate["free"] and wstate["next"] < len(seq):
            sl = wstate["free"].pop(0)
            cid = seq[wstate["next"]]
            wstate["next"] += 1
            S.dma(POOL, ring[:, sl, :, :], cdef[cid], r=(), w=(slotb[sl],), sem=S.wsems[sl])
            wstate["slot"][cid] = sl

    def wslot(cid):
        assert cid in wstate["slot"], (cid, wstate["next"])
        return wstate["slot"][cid]

    def wrelease(cid):
        wstate["free"].append(wstate["slot"].pop(cid))
        wpump()
    wpump()
    S.dma(POOL, wa, gla_wa.rearrange("(k p) n -> p k n", p=128), sem=S.xsem)

    S.dma(SP, ident, ident_d, w=(b_const,))
    EA.reset()
    prt = EA.alloc([128], F32)
    prt2 = EA.alloc([128], F32, parts=96)
    ct = EA.alloc([D], F32, parts=NB)
    b_pr = Buf("pr")
    S.dma(SP, prt, prow[0:128, :])
    S.dma(SP, prt2, prow[128:224, :])
    S.dma(SP, ct, cin)
    S.wait_dmas()
    S.op(DVE, lambda: nc.vector.tensor_copy(out=identb, in_=ident), r=(b_const,), w=(b_const,))
    S.op(DVE, lambda: nc.vector.memset(onesb, 1.0 / 1024.0), w=(b_const,))
    b0 = palloc(1)
    S.transposes([(ps[:, b0, 0:128], prt, ident), (ps[:, b0, 128:224], prt2, ident[0:96, 0:96])],
                 r=(b_pr, b_const), w=(pb[b0],))
    S.op(DVE, lambda: nc.vector.tensor_copy(out=PF, in_=ps[:, b0, 0:R_TOT]), r=(pb[b0],), w=(b_const,))
    csl = EA.alloc([D], F32, parts=NB)
    b_c = Buf("c")
    S.op(ACT, lambda: nc.scalar.activation(out=csl, in_=ct, func=AF.Silu), r=(b_pr,), w=(b_c,))
    b1_ = palloc(1)
    S.transposes([(ps[:, b1_, k * NB:(k + 1) * NB], csl[:, k * 128:(k + 1) * 128], ident[0:NB, 0:NB]) for k in range(8)],
                 r=(b_c, b_const), w=(pb[b1_],))
    S.op(DVE, lambda: nc.vector.tensor_copy(out=scT, in_=ps[:, b1_, 0:8 * NB].rearrange("p (k b) -> p k b", b=NB)),
         r=(pb[b1_],), w=(b_const,))

    NXS = 8
    xst = [EA.alloc([D], F32) for _ in range(NXS)]
    xsb = [Buf("xs%d" % i) for i in range(NXS)]

    def xload(ti):
        t0 = ti * 128
        n = 128 if ti < 16 else 64
        S.dma(SP, xst[ti % NXS][0:n, :], xin[t0:t0 + n, :], w=(xsb[ti % NXS],))

    for ti in range(NXS):
        xload(ti)
    for ti in range(17):
        t0 = ti * 128
        n = 128 if ti < 16 else 64
        sl = ti % NXS
        b = palloc(2)
        S.transposes([(ps[:, b + k // 4, (k % 4) * 128:(k % 4) * 128 + n], xst[sl][0:n, k * 128:(k + 1) * 128], ident[0:n, 0:n])
                      for k in range(8)], r=(xsb[sl], b_const), w=(pb[b], pb[b + 1]))
        src = ps[:, b:b + 2, :].rearrange("p a (k t) -> p (a k) t", t=128)[:, :, 0:n]
        eng = ACT if ti % 2 == 0 else DVE
        if eng is ACT:
            S.op(ACT, lambda: nc.scalar.copy(out=xT[:, :, t0:t0 + n], in_=src), r=(pb[b], pb[b + 1]), w=xb(t0, n))
        else:
            S.op(DVE, lambda: nc.vector.tensor_copy(out=xT[:, :, t0:t0 + n], in_=src), r=(pb[b], pb[b + 1]), w=xb(t0, n))
        if ti + NXS < 17:
            xload(ti + NXS)

    def modsel(M, k, isp):
        if isp:
            return M[:, k, 0:1]
        return M[:, k, 1:17].unsqueeze(2).to_broadcast([128, 16, 4])

    def v3(ap, isp):
        return ap if isp else ap.rearrange("p (b t) -> p b t", t=4)

    def ada_chunk(l, j):
        MOD = MODS[l]
        bm = b_mods[l]
        cid = "A%d_%d" % (l, j)
        s_ = wslot(cid)
        b = palloc(1)
        groups = []
        for m in range(8):
            groups.append([(ps[:, b, m * NB:(m + 1) * NB], ring[:, s_, k, m * 128:(m + 1) * 128], scT[:, k, :]) for k in range(8)])
        S.mm_multi(groups, r=(slotb[s_], b_const), w=(pb[b],))
        wrelease(cid)
        bias = PF[:, R_ADAB + l * 48 + j * 8:R_ADAB + l * 48 + j * 8 + 8].unsqueeze(2).to_broadcast([128, 8, NB])
        S.op(DVE, lambda: nc.vector.tensor_tensor(out=MOD[:, j, :, :], in0=ps[:, b, 0:8 * NB].rearrange("p (m b) -> p m b", b=NB),
                                                  in1=bias, op=ALU.add), r=(pb[b], b_const), w=(bm,))
        if j == 1:
            nmg = PF[:, R_NMG + l * 8:R_NMG + l * 8 + 8].unsqueeze(2).to_broadcast([128, 8, NB])
            S.op(DVE, lambda: nc.vector.scalar_tensor_tensor(out=MOD[:, 1, :, :], in0=MOD[:, 1, :, :], scalar=1.0, in1=nmg, op0=ALU.add, op1=ALU.mult),
                 r=(bm, b_const), w=(bm,))
        if j == 4:
            nfg = PF[:, R_NFG + l * 8:R_NFG + l * 8 + 8].unsqueeze(2).to_broadcast([128, 8, NB])
            S.op(DVE, lambda: nc.vector.scalar_tensor_tensor(out=MOD[:, 4, :, :], in0=MOD[:, 4, :, :], scalar=1.0, in1=nfg, op0=ALU.add, op1=ALU.mult),
                 r=(bm, b_const), w=(bm,))
        if j == 5:
            b2 = PF[:, R_B2 + l * 8:R_B2 + l * 8 + 8].unsqueeze(2).to_broadcast([128, 8, NB])
            S.op(DVE, lambda: nc.vector.tensor_tensor(out=GB2S[l], in0=MOD[:, 5, :, :], in1=b2, op=ALU.mult), r=(bm, b_const), w=(bm,))
        if j == 2 and l == 0:
            bo = PF[:, R_BOUT:R_BOUT + 8].unsqueeze(2).to_broadcast([128, 8, NB])
            S.op(DVE, lambda: nc.vector.tensor_tensor(out=GB1, in0=MOD[:, 2, :, :], in1=bo, op=ALU.mult), r=(bm, b_const), w=(bm,))

    def norm_tile(t0, n, isp, GM, SH, hout, hbuf, tmps, tb, sq_, sqb, rs_, rsb, b_mod, part=0, so=0):
        sq = sq_[:, :, so:so + n]
        rs = rs_[:, so:so + n]
        if part in (0, 1):
            S.op(ACT, lambda: nc.scalar.activation(out=sq[:, :, 0:n], in_=xT[:, :, t0:t0 + n], func=AF.Square), r=xb(t0, n), w=(sqb,))
        if part == 1:
            return
        b = palloc(1)
        S.mm([(ps[:, b, 0:n], onesb, sq[:, k, 0:n]) for k in range(8)], r=(sqb, b_const), w=(pb[b],))
        S.op(ACT, lambda: nc.scalar.activation(out=rs[:, 0:n], in_=ps[:, b, 0:n], func=AF.Ln, bias=EPS, scale=1.0), r=(pb[b],), w=(rsb,))
        S.op(ACT, lambda: nc.scalar.activation(out=rs[:, 0:n], in_=rs[:, 0:n], func=AF.Exp, scale=-0.5), r=(rsb,), w=(rsb,))
        for k in range(8):
            tmp = tmps[k % 2]
            tbb = tb[k % 2]
            S.op(DVE, lambda: nc.vector.tensor_tensor(out=tmp[:, 0:n], in0=xT[:, k, t0:t0 + n], in1=rs[:, 0:n], op=ALU.mult),
                 r=xb(t0, n) + (rsb,), w=(tbb,))
            if isp:
                S.op(ACT, lambda: nc.scalar.activation(out=hout[:, k, 0:n], in_=tmp[:, 0:n], func=AF.Identity,
                                                       scale=GM[:, k, 0:1], bias=SH[:, k, 0:1]), r=(tbb, b_mod), w=(hbuf,))
            else:
                S.op(DVE, lambda: nc.vector.tensor_tensor(out=v3(tmp[:, 0:n], False), in0=v3(tmp[:, 0:n], False), in1=modsel(GM, k, False), op=ALU.mult),
                     r=(tbb, b_mod), w=(tbb,))
                S.op(DVE, lambda: nc.vector.tensor_tensor(out=v3(hout[:, k, 0:n], False), in0=v3(tmp[:, 0:n], False), in1=modsel(SH, k, False), op=ALU.add),
                     r=(tbb, b_mod), w=(hbuf,))

    def resid_update(b, k, t0, n, isp, G, GB, tmp, tbb, b_mod, pcol=0):
        xs = xT[:, k, t0:t0 + n]
        psv = ps[:, b, pcol:pcol + n]
        if isp:
            if GB is not None:
                S.op(ACT, lambda: nc.scalar.activation(out=tmp[:, 0:n], in_=psv, func=AF.Identity, scale=G[:, k, 0:1], bias=GB[:, k, 0:1]),
                     r=(pb[b], b_mod), w=(tbb,))
            else:
                S.op(DVE, lambda: nc.vector.scalar_tensor_tensor(out=xs, in0=psv, scalar=G[:, k, 0:1], in1=xs, op0=ALU.mult, op1=ALU.add),
                     r=(pb[b], b_mod) + xb(t0, n), w=xb(t0, n))
                return
            S.op(DVE, lambda: nc.vector.tensor_tensor(out=xs, in0=xs, in1=tmp[:, 0:n], op=ALU.add), r=(tbb,) + xb(t0, n), w=xb(t0, n))
        else:
            S.op(DVE, lambda: nc.vector.tensor_tensor(out=v3(tmp[:, 0:n], False), in0=v3(psv, False), in1=modsel(G, k, False), op=ALU.mult),
                 r=(pb[b], b_mod), w=(tbb,))
            if GB is not None:
                S.op(DVE, lambda: nc.vector.tensor_tensor(out=v3(tmp[:, 0:n], False), in0=v3(tmp[:, 0:n], False), in1=modsel(GB, k, False), op=ALU.add),
                     r=(tbb, b_mod), w=(tbb,))
            S.op(DVE, lambda: nc.vector.tensor_tensor(out=xs, in0=xs, in1=tmp[:, 0:n], op=ALU.add), r=(tbb,) + xb(t0, n), w=xb(t0, n))

    def gmlp_phase():
        S.barrier()
        CA.reset()
        EA.reset()
        MOD = MODS[0]
        b_mod = b_mods[0]
        GM1 = MOD[:, 1, :, :]
        s_in0, s_in1, s_out = wslot("GIN0"), wslot("GIN1"), wslot("GOUT")
        binv = CA.alloc([D], F32)
        lng = CA.alloc([D], F32)
        lnb = CA.alloc([D], F32)
        bsp = CA.alloc([4, 128], F32)
        bss = CA.alloc([4, 64], F32)
        wsp = CA.alloc([4, 128], BF16)
        wss = CA.alloc([4, 64], BF16, parts=64)
        vgs = [CA.alloc([D], F32) for _ in range(2)]
        vgb = [Buf("vg0"), Buf("vg1")]
        mark = WA.off
        WA.off = mark - 4096
        cmp_ = CA.alloc([128], F32)
        cms = CA.alloc([64], F32, parts=64)
        wtmp = CA.alloc([4, 128], F32)
        wtmps = CA.alloc([4, 64], F32, parts=64)
        assert WA.off <= mark
        WA.off = mark
        bk = Buf("gk")
        for (dst, src) in [(binv, binv_bc), (lng, lng_bc), (lnb, lnb_bc), (bsp, bs_p), (bss, bs_s), (cmp_, cm_p), (cms, cm_s),
                           (wtmp, wsT_p), (wtmps, wsT_s)]:
            S.dma(SP, dst, src)
        S.wait_dmas()
        S.op(DVE, lambda: nc.vector.tensor_tensor(out=wsp, in0=wtmp, in1=cmp_.unsqueeze(1).to_broadcast([128, 4, 128]), op=ALU.mult), r=(bk,), w=(bk,))
        S.op(DVE, lambda: nc.vector.tensor_tensor(out=wss, in0=wtmps, in1=cms.unsqueeze(1).to_broadcast([64, 4, 64]), op=ALU.mult), r=(bk,), w=(bk, vgb[1]))
        hT = EA.alloc([8, 512], BF16)
        hb = Buf("h")
        sq = EA.alloc([8, 512], BF16)
        sqb = Buf("sq")
        rs = EA.alloc([512], F32)
        rsb = Buf("rs")
        tmps = [EA.alloc([512], F32) for _ in range(2)]
        tb = [Buf("t0"), Buf("t1")]
        uTs = [EA.alloc([8, 512], BF16) for _ in range(2)]
        ubs = [Buf("u0"), Buf("u1")]
        vbfs = [EA.alloc([D], BF16) for _ in range(2)]
        vbb = [Buf("vb0"), Buf("vb1")]
        junk = sq.rearrange("p a b -> p (a b)")
        jb = sqb
        sts = [EA.alloc([8], F32) for _ in range(2)]
        stbs = [Buf("st0"), Buf("st1")]
        SH1 = MOD[:, 0, :, :]
        G1 = MOD[:, 2, :, :]
        print('gMLP arena', WA.off, WA.nbytes)
        pend = []
        for ti_, (t0, n, isp) in enumerate(TILES):
            nsub = (n + 127) // 128
            uT = uTs[ti_ % 2]
            usT = uT
            ub = ubs[ti_ % 2]
            usb = ub

            def U(m):
                b = palloc(1)
                S.mm([(ps[:, b, 0:n], ring[:, s_in0, k, m * 128:(m + 1) * 128], hT[:, k, 0:n]) for k in range(8)],
                     r=(slotb[s_in0], hb), w=(pb[b],))
                S.op(ACT, lambda: nc.scalar.activation(out=uT[:, m, 0:n], in_=ps[:, b, 0:n], func=AF.Gelu_apprx_tanh,
                                                       bias=PF[:, R_BINU + m:R_BINU + m + 1]), r=(pb[b], b_const), w=(ub,))

            def Va(j):
                nt = min(128, n - j * 128)
                vg = vgs[j % 2]
                vb_ = vgb[j % 2]
                vbf = vbfs[j % 2]
                st = sts[j % 2]
                stb = stbs[j % 2]
                b = palloc(2)
                for half in range(2):
                    S.mm([(ps[0:nt, b + half, :], hT[:, k, j * 128:j * 128 + nt], ring[:, s_in1, k, half * 512:(half + 1) * 512]) for k in range(8)],
                         r=(slotb[s_in1], hb), w=(pb[b + half],))
                pv = ps[0:nt, b:b + 2, :].rearrange("p a c -> p (a c)")
                S.op(DVE, lambda: nc.vector.tensor_tensor(out=vg[0:nt, :], in0=pv, in1=binv[0:nt, :], op=ALU.add), r=(pb[b], pb[b + 1], bk), w=(vb_,))
                S.op(ACT, lambda: nc.scalar.activation(out=vg[0:nt, :], in_=vg[0:nt, :], func=AF.Gelu_apprx_tanh, accum_out=st[0:nt, 0:1]),
                     r=(vb_,), w=(vb_, stb))
                S.op(ACT, lambda: nc.scalar.activation(out=junk[0:nt, 0:D], in_=vg[0:nt, :], func=AF.Square, accum_out=st[0:nt, 1:2]),
                     r=(vb_,), w=(jb, stb))
                S.op(DVE, lambda: nc.vector.tensor_scalar(out=st[0:nt, 2:3], in0=st[0:nt, 0:1], scalar1=1.0 / D, scalar2=None, op0=ALU.mult), r=(stb,), w=(stb,))
                S.op(DVE, lambda: nc.vector.tensor_tensor(out=st[0:nt, 3:4], in0=st[0:nt, 2:3], in1=st[0:nt, 2:3], op=ALU.mult), r=(stb,), w=(stb,))
                S.op(DVE, lambda: nc.vector.scalar_tensor_tensor(out=st[0:nt, 4:5], in0=st[0:nt, 1:2], scalar=1.0 / D, in1=st[0:nt, 3:4],
                                                                 op0=ALU.mult, op1=ALU.subtract), r=(stb,), w=(stb,))

            def Vb(j):
                nt = min(128, n - j * 128)
                vg = vgs[j % 2]
                vb_ = vgb[j % 2]
                vbf = vbfs[j % 2]
                st = sts[j % 2]
                stb = stbs[j % 2]
                S.op(ACT, lambda: nc.scalar.activation(out=st[0:nt, 5:6], in_=st[0:nt, 4:5], func=AF.Ln, bias=EPS, scale=1.0), r=(stb,), w=(stb,))
                S.op(ACT, lambda: nc.scalar.activation(out=st[0:nt, 5:6], in_=st[0:nt, 5:6], func=AF.Exp, scale=-0.5), r=(stb,), w=(stb,))
                S.op(DVE, lambda: nc.vector.scalar_tensor_tensor(out=st[0:nt, 6:7], in0=st[0:nt, 2:3], scalar=-1.0, in1=st[0:nt, 5:6],
                                                                 op0=ALU.mult, op1=ALU.mult), r=(stb,), w=(stb,))
                S.op(ACT, lambda: nc.scalar.activation(out=vg[0:nt, :], in_=vg[0:nt, :], func=AF.Identity, scale=st[0:nt, 5:6], bias=st[0:nt, 6:7]),
                     r=(vb_, stb), w=(vb_,))
                S.op(DVE, lambda: nc.vector.tensor_tensor(out=vg[0:nt, :], in0=vg[0:nt, :], in1=lng[0:nt, :], op=ALU.mult), r=(vb_, bk), w=(vb_,))
                if isp:
                    S.op(DVE, lambda: nc.vector.tensor_tensor(out=vbf[0:nt, :], in0=vg[0:nt, :], in1=lnb[0:nt, :], op=ALU.add), r=(vb_, bk), w=(vbb[j % 2],))
                else:
                    S.op(DVE, lambda: nc.vector.tensor_tensor(out=vg[0:nt, :], in0=vg[0:nt, :], in1=lnb[0:nt, :], op=ALU.add), r=(vb_, bk), w=(vb_,))
                    S.dma(SP, v_sample, vg[0:nt, :], r=(vb_,))
                    S.op(ACT, lambda: nc.scalar.copy(out=vbf[0:nt, :], in_=vg[0:nt, :]), r=(vb_,), w=(vbb[j % 2],))

            def SPA(j):
                nt = min(128, n - j * 128)
                vbf = vbfs[j % 2]
                b = palloc(2)
                wsm = wsp if isp else wss
                groups = []
                for m in range(8):
                    o = ps[:, b + m // 4, (m % 4) * 128:(m % 4) * 128 + nt]
                    groups.append([(o, vbf[0:nt, m * 128:(m + 1) * 128], wsm[0:nt, m // 2, 0:nt])])
                S.mm_multi(groups, r=(vbb[j % 2], bk), w=(pb[b], pb[b + 1]))
                bsm = bsp if isp else bss
                for a in range(2):
                    pin = ps[:, b + a, :].rearrange("p (g r i) -> p g r i", r=2, i=128)[:, :, :, 0:nt]
                    bsv = bsm[:, 2 * a:2 * a + 2, 0:nt].unsqueeze(2).to_broadcast([128, 2, 2, nt])
                    tt = tmps[a]
                    tv = tt[:, 0:4 * nt].rearrange("p (g r i) -> p g r i", r=2, i=nt)
                    S.op(DVE, lambda: nc.vector.tensor_tensor(out=tv, in0=pin, in1=bsv, op=ALU.add), r=(pb[b + a], bk), w=(tb[a],))
                    uo = usT[:, 4 * a:4 * a + 4, j * 128:j * 128 + nt]
                    ui = uT[:, 4 * a:4 * a + 4, j * 128:j * 128 + nt]
                    tv2 = tt[:, 0:4 * nt].rearrange("p (m i) -> p m i", i=nt)
                    S.op(DVE, lambda: nc.vector.tensor_tensor(out=uo, in0=tv2, in1=ui, op=ALU.mult), r=(tb[a], ub), w=(usb,))

            def make_O(kk, t0=t0, n=n, isp=isp, usT=usT, usb=usb):
                def O_one():
                    b = palloc(1)
                    S.mm([(ps[:, b, 0:n], ring[:, s_out, m, kk * 128:(kk + 1) * 128], usT[:, m, 0:n]) for m in range(8)],
                         r=(slotb[s_out], usb), w=(pb[b],))
                    resid_update(b, kk, t0, n, isp, G1, GB1, tmps[kk % 2], tb[kk % 2], b_mod)
                return O_one

            def flush(k=99):
                while pend and k > 0:
                    pend.pop(0)()
                    k -= 1

            def next_norm():
                if ti_ + 1 < len(TILES):
                    t1, n1, isp1 = TILES[ti_ + 1]
                    norm_tile(t1, n1, isp1, GM1, SH1, hT, hb, tmps, tb, sq, sqb, rs, rsb, b_mod)

            if ti_ == 0:
                norm_tile(t0, n, isp, GM1, SH1, hT, hb, tmps, tb, sq, sqb, rs, rsb, b_mod)
            if nsub == 4:
                Va(0); Va(1); U(0); U(1); Vb(0); U(2); U(3); Vb(1); U(4); U(5); U(6); U(7)
                SPA(0); Va(2); SPA(1); Va(3)
                next_norm()
                flush(4)
                Vb(2)
                flush(2)
                Vb(3)
                flush()
                SPA(2); SPA(3)
            else:
                Va(0)
                for m in range(4):
                    U(m)
                Vb(0)
                for m in range(4, 8):
                    U(m)
                flush()
                SPA(0)
            if t0 == 0:
                ada_chunk(0, 2)
            for kk in range(8):
                pend.append(make_O(kk))
            if t0 == 0:
                ada_chunk(0, 3)
            elif t0 == 512:
                ada_chunk(0, 4)
            elif t0 == 1024:
                ada_chunk(0, 5)
        while pend:
            pend.pop(0)()
        wrelease("GIN0")
        wrelease("GIN1")
        wrelease("GOUT")

    def ffn_phase(l):
        S.barrier()
        CA.reset()
        EA.reset()
        MOD = MODS[l]
        b_mod = b_mods[l]
        GM2 = MOD[:, 4, :, :]
        GB2 = GB2S[l]
        hall = CA.alloc([8, NTOK], BF16)
        hb = Buf("hall")
        sq = EA.alloc([8, 512], BF16)
        sqb = Buf("sq")
        rs = EA.alloc([512], F32)
        rsb = Buf("rs")
        tmps = [EA.alloc([512], F32) for _ in range(2)]
        tb = [Buf("t0"), Buf("t1")]
        hid = [EA.alloc([8, 512], BF16) for _ in range(2)]
        hidb = [Buf("hid0"), Buf("hid1")]
        rl = [EA.alloc([512], F32) for _ in range(2)]
        rlb = [Buf("rl0"), Buf("rl1")]
        SH2 = MOD[:, 3, :, :]
        G2 = MOD[:, 5, :, :]
        FT = [(i * 448, 448, [(i * 448, 448, True)]) for i in range(4)] + [(1792, 320, [(1792, 256, True), (NPR, NSA, False)])]
        NT_ = len(FT)
        hbs = [Buf("hall%d" % i) for i in range(NT_)]

        def nrm(ti, part, tm, tmb):
            t0_, n_, subs = FT[ti]
            for (st0, sn, sisp) in subs:
                norm_tile(st0, sn, sisp, GM2, SH2, hall[:, :, st0:st0 + sn], hbs[ti], tm, tmb, sq, sqb, rs, rsb, b_mod,
                          part=part, so=st0 - t0_)

        nrm(0, 0, tmps, tb)

        def H(q, ti, hd, hdb):
            t0, n, subs = FT[ti]
            s1 = wslot("F%d_1_%d" % (l, q))
            hb = hbs[ti]
            for f in range(8):
                b = palloc(1)
                S.mm([(ps[:, b, 0:n], ring[:, s1, k, f * 128:(f + 1) * 128], hall[:, k, t0:t0 + n]) for k in range(8)],
                     r=(slotb[s1], hb), w=(pb[b],))
                r_ = rl[f % 2]
                rb_ = rlb[f % 2]
                bcol = PF[:, R_B1 + l * 32 + q * 8 + f:R_B1 + l * 32 + q * 8 + f + 1]
                S.op(ACT, lambda: nc.scalar.activation(out=r_[:, 0:n], in_=ps[:, b, 0:n], func=AF.Relu, bias=bcol), r=(pb[b], b_const), w=(rb_,))
                S.op(DVE, lambda: nc.vector.tensor_tensor(out=hd[:, f, 0:n], in0=r_[:, 0:n], in1=r_[:, 0:n], op=ALU.mult), r=(rb_,), w=(hdb,))

        def Y(q, ti, hd, hdb):
            t0, n, subs = FT[ti]
            s2 = wslot("F%d_2_%d" % (l, q))
            for kk in range(8):
                b = palloc(1)
                S.mm([(ps[:, b, 0:n], ring[:, s2, f, kk * 128:(kk + 1) * 128], hd[:, f, 0:n]) for f in range(8)],
                     r=(slotb[s2], hdb), w=(pb[b],))
                for (st0, sn, sisp) in subs:
                    resid_update(b, kk, st0, sn, sisp, G2, GB2 if q == 0 else None, tmps[kk % 2], tb[kk % 2], b_mod, pcol=st0 - t0)
            if l == 0 and q == 2 and ti == 2:
                ada_chunk(1, 0)
                ada_chunk(1, 1)
            if l == 0 and q == 3:
                if ti == 1:
                    ada_chunk(1, 2)
                    ada_chunk(1, 3)
                elif ti == 3:
                    ada_chunk(1, 4)
                    ada_chunk(1, 5)
            if ti == NT_ - 1:
                wrelease("F%d_1_%d" % (l, q))
                wrelease("F%d_2_%d" % (l, q))

        for ti in range(NT_):
            if ti + 1 < NT_:
                nrm(ti + 1, 1, None, None)
            H(0, ti, hid[ti % 2], hidb[ti % 2])
            if ti + 1 < NT_:
                nrm(ti + 1, 2, rl, rlb)
            Y(0, ti, hid[ti % 2], hidb[ti % 2])
        steps = [(q, ti) for q in range(1, 4) for ti in range(NT_)]
        it = NT_
        H(steps[0][0], steps[0][1], hid[it % 2], hidb[it % 2])
        for si, (q, ti) in enumerate(steps):
            cur = it + si
            if si + 1 < len(steps):
                nq, nti = steps[si + 1]
                H(nq, nti, hid[(cur + 1) % 2], hidb[(cur + 1) % 2])
            Y(q, ti, hid[cur % 2], hidb[cur % 2])

    def gla_phase():
        S.barrier()
        WA.reset()
        MOD = MODS[1]
        b_mod = b_mods[1]
        GM1 = MOD[:, 1, :, :]
        sA, sB, sC, sD = wslot("L0"), wslot("L1"), wslot("L2"), wslot("LOUT")
        bk = Buf("gk")
        wg2 = WA.alloc([512], F32, parts=17)
        ngb = WA.alloc([256], F32)
        cmp_ = WA.alloc([128], F32)
        cms = WA.alloc([64], F32, parts=64)
        tNp = WA.alloc([128], F32)
        tNs = WA.alloc([64], F32, parts=64)
        km = WA.alloc([16], F32, parts=64)
        for (dst, src) in [(wg2, wg2a), (ngb, gng_bc), (cmp_, cm_p), (cms, cm_s), (tNp, triN_p), (tNs, triN_s), (km, kmask)]:
            S.dma(SP, dst, src)
        S.wait_dmas(with_x=True)
        Sf = WA.alloc([4, 256], F32)
        Sb = WA.alloc([4, 256], BF16)
        Sfb = Buf("Sf")
        Sbb = Buf("Sb")
        junk = WA.alloc([256], BF16)
        jb = Buf("junk")

        class Set:
            pass

        set_off = []
        sets = []
        for si in range(2):
            set_off.append(WA.off)
            B = Set()
            B.hT = WA.alloc([8, 128], BF16); B.hb = Buf("h")
            B.sq = WA.alloc([8, 128], BF16); B.sqb = Buf("sq")
            B.ogT = WA.alloc([8, 128], BF16); B.ogTb = Buf("ogT")
            B.rs = WA.alloc([128], F32); B.rsb = Buf("rs")
            B.tmps = [WA.alloc([128], F32) for _ in range(2)]; B.tb = [Buf("t0"), Buf("t1")]
            B.ntmps = [WA.alloc([128], F32) for _ in range(2)]; B.ntb = [Buf("nt0"), Buf("nt1")]
            B.qkT = WA.alloc([8, 128], F32); B.qkb = [Buf("q"), Buf("k")]; B.ksb = Buf("ks")
            B.aaug = WA.alloc([128], F32, parts=17); B.aab = Buf("aa")
            B.sp = WA.alloc([512], F32); B.spb = Buf("sp")
            B.eb = WA.alloc([4, 128], F32); B.ebb = Buf("eb")
            B.enb = B.sp.rearrange("p (h t) -> p h t", t=128)
            B.qs = WA.alloc([4, 128], BF16); B.ks = WA.alloc([4, 128], BF16); B.qsb = Buf("qs")
            B.khT = WA.alloc([4, 128], BF16); B.khTb = Buf("khT")
            B.kh = WA.alloc([512], BF16); B.khb = Buf("kh")
            B.vb = WA.alloc([D], BF16); B.vbb = Buf("vb")
            B.sg = WA.alloc([D], F32); B.sgb = Buf("sg")
            B.am = B.khT; B.amb = B.khTb
            B.ogh = B.sp.bitcast(BF16); B.oghb = B.spb
            B.st = WA.alloc([16], F32); B.stb = Buf("st")
            sets.append(B)
        set_end = WA.off
        print('GLA arena', WA.off, WA.nbytes)
        S.op(DVE, lambda: nc.vector.memset(Sf, 0.0), w=(Sfb,))
        S.op(DVE, lambda: nc.vector.memset(Sb, 0.0), w=(Sbb,))
        SH1 = MOD[:, 0, :, :]
        G1 = MOD[:, 2, :, :]
        SCALE = 128.0 ** -0.5
        GT = [(i * 128, 128, True) for i in range(NPR // 128)] + [(NPR, NSA, False)]
        def chunk_gen(ci, t0, n, isp):
            B = sets[ci % 2]
            nt = n
            hT = B.hT
            norm_tile(t0, n, isp, GM1, SH1, hT, B.hb, B.ntmps, B.ntb, B.sq, B.sqb, B.rs, B.rsb, b_mod)
            tN = tNp if isp else tNs
            cm = cmp_ if isp else cms
            yield 'F0'
            b = palloc(1)
            S.mm([(ps[0:17, b, 0:nt], wa[:, k, :], hT[:, k, 0:nt]) for k in range(8)], r=(bk, B.hb), w=(pb[b],))
            S.op(ACT, lambda: nc.scalar.copy(out=B.aaug[0:17, 0:nt], in_=ps[0:17, b, 0:nt]), r=(pb[b],), w=(B.aab,))
            S.op(DVE, lambda: nc.vector.memset(B.aaug[0:1, 0:nt], 1.0), w=(B.aab,))
            yield 'F1'
            bq = palloc(2)
            pstate["res"] |= {bq, bq + 1}

            def qk_groups(ms):
                for m in ms:
                    S.mm([(ps[:, bq + m // 4, (m % 4) * 128:(m % 4) * 128 + nt], ring[:, sA, k, m * 128:(m + 1) * 128], hT[:, k, 0:nt]) for k in range(8)],
                         r=(slotb[sA], B.hb), w=(pb[bq + m // 4],))

            def qk_copy(a_):
                S.op(ACT, lambda: nc.scalar.copy(out=B.qkT[:, 4 * a_:4 * a_ + 4, 0:nt],
                                                 in_=ps[:, bq + a_, :].rearrange("p (m t) -> p m t", t=128)[:, :, 0:nt]),
                     r=(pb[bq + a_],), w=(B.qkb[a_],))
            qk_groups([0, 1])
            yield 'F2'
            qk_groups([2, 3])
            qk_copy(0)
            yield 'F3'
            bx = palloc(1)
            S.mm([(ps[0:nt, bx, :], B.aaug[0:17, 0:nt], wg2[0:17, :])], r=(B.aab, bk), w=(pb[bx],))
            S.op(ACT, lambda: nc.scalar.activation(out=B.sp[0:nt, :], in_=ps[0:nt, bx, :], func=AF.Exp, scale=-1.0), r=(pb[bx],), w=(B.spb,))
            S.op(ACT, lambda: nc.scalar.activation(out=B.sp[0:nt, :], in_=B.sp[0:nt, :], func=AF.Ln, bias=1.0, scale=1.0), r=(B.spb,), w=(B.spb,))
            yield 'F4'
            qk_groups([4, 5])
            yield 'F5'
            qk_groups([6, 7])
            qk_copy(1)
            pstate["res"] -= {bq, bq + 1}
            yield 'F6'
            bb = palloc(1)
            S.mm_multi([[(ps[:, bb, hd * 128:hd * 128 + nt], B.sp[0:nt, hd * 128:(hd + 1) * 128], tN[0:nt, 0:nt])] for hd in range(4)],
                       r=(B.spb, bk), w=(pb[bb],))
            bT = ps[:, bb, :].rearrange("p (h t) -> p h t", t=128)[:, :, 0:nt]
            eb = B.eb
            enb = B.enb
            S.op(ACT, lambda: nc.scalar.activation(out=eb[:, :, 0:nt], in_=bT, func=AF.Exp), r=(pb[bb],), w=(B.ebb,))
            S.op(ACT, lambda: nc.scalar.activation(out=enb[:, :, 0:nt], in_=bT, func=AF.Exp, scale=-1.0), r=(pb[bb],), w=(B.spb,))
            S.op(DVE, lambda: nc.vector.scalar_tensor_tensor(out=B.qs[:, :, 0:nt], in0=B.qkT[:, 0:4, 0:nt], scalar=SCALE, in1=eb[:, :, 0:nt],
                                                             op0=ALU.mult, op1=ALU.mult), r=(B.qkb[0], B.ebb), w=(B.qsb,))
            S.op(DVE, lambda: nc.vector.tensor_tensor(out=B.ks[:, :, 0:nt], in0=B.qkT[:, 4:8, 0:nt], in1=enb[:, :, 0:nt], op=ALU.mult),
                 r=(B.qkb[1], B.spb), w=(B.ksb,))
            if isp:
                for hd in range(4):
                    S.op(DVE, lambda: nc.vector.scalar_tensor_tensor(out=B.khT[:, hd, 0:nt], in0=B.qkT[:, 4 + hd, 0:nt], scalar=eb[:, hd, nt - 1:nt],
                                                                     in1=enb[:, hd, 0:nt], op0=ALU.mult, op1=ALU.mult),
                         r=(B.qkb[1], B.ebb, B.spb), w=(B.khTb,))
            else:
                tmpk = B.ntmps[0]
                for hd in range(4):
                    el = eb[:, hd, 0:nt].rearrange("p (b t) -> p b t", t=4)[:, :, 3:4].to_broadcast([128, 16, 4])
                    S.op(DVE, lambda: nc.vector.tensor_tensor(out=v3(tmpk[:, 0:nt], False), in0=v3(B.qkT[:, 4 + hd, 0:nt], False), in1=el, op=ALU.mult),
                         r=(B.qkb[1], B.ebb), w=(B.ntb[0],))
                    S.op(DVE, lambda: nc.vector.tensor_tensor(out=B.khT[:, hd, 0:nt], in0=tmpk[:, 0:nt], in1=enb[:, hd, 0:nt], op=ALU.mult),
                         r=(B.ntb[0], B.spb), w=(B.khTb,))
            yield 'F7'
            bv = palloc(2)
            pstate["res"] |= {bv, bv + 1}
            S.mm([(ps[0:nt, bv, :], hT[:, k, 0:nt], ring[:, sB, k, 0:512]) for k in range(8)], r=(slotb[sB], B.hb), w=(pb[bv],))
            yield 'F8'
            S.mm([(ps[0:nt, bv + 1, :], hT[:, k, 0:nt], ring[:, sB, k, 512:1024]) for k in range(8)], r=(slotb[sB], B.hb), w=(pb[bv + 1],))
            S.op(ACT, lambda: nc.scalar.copy(out=B.vb[0:nt, :], in_=ps[0:nt, bv:bv + 2, :].rearrange("p a c -> p (a c)")),
                 r=(pb[bv], pb[bv + 1]), w=(B.vbb,))
            pstate["res"] -= {bv, bv + 1}
            yield 'F9'
            bg = palloc(2)
            pstate["res"] |= {bg, bg + 1}
            S.mm([(ps[0:nt, bg, :], hT[:, k, 0:nt], ring[:, sC, k, 0:512]) for k in range(8)], r=(slotb[sC], B.hb), w=(pb[bg],))
            yield 'F10'
            S.mm([(ps[0:nt, bg + 1, :], hT[:, k, 0:nt], ring[:, sC, k, 512:1024]) for k in range(8)], r=(slotb[sC], B.hb), w=(pb[bg + 1],))
            gps = ps[0:nt, bg:bg + 2, :].rearrange("p a c -> p (a c)")
            S.op(ACT, lambda: nc.scalar.activation(out=B.sg[0:nt, :], in_=gps, func=AF.Exp, scale=-1.0), r=(pb[bg], pb[bg + 1]), w=(B.sgb,))
            S.op(ACT, lambda: nc.scalar.activation(out=B.sg[0:nt, :], in_=B.sg[0:nt, :], func=AF.Ln, bias=1.0, scale=1.0), r=(B.sgb,), w=(B.sgb,))
            S.op(ACT, lambda: nc.scalar.activation(out=B.sg[0:nt, :], in_=B.sg[0:nt, :], func=AF.Exp, scale=-1.0), r=(B.sgb,), w=(B.sgb,))
            S.op(DVE, lambda: nc.vector.tensor_tensor(out=B.sg[0:nt, :], in0=gps, in1=B.sg[0:nt, :], op=ALU.mult), r=(pb[bg], pb[bg + 1], B.sgb), w=(B.sgb,))
            sg4 = B.sg[0:nt, :].rearrange("p (h v) -> p h v", v=256)
            S.op(DVE, lambda: nc.vector.tensor_tensor(out=sg4, in0=sg4, in1=ngb[0:nt, :].unsqueeze(1).to_broadcast([nt, 4, 256]), op=ALU.mult),
                 r=(B.sgb, bk), w=(B.sgb,))
            pstate["res"] -= {bg, bg + 1}
            yield 'F11'
            bkt = palloc(1)
            pkt = ps[:, bkt, :].bitcast(BF16)
            S.transposes([(pkt[0:nt, hd * 128:(hd + 1) * 128], B.khT[:, hd, 0:nt], identb) for hd in range(4)], r=(B.khTb, b_const), w=(pb[bkt],))
            S.op(ACT, lambda: nc.scalar.copy(out=B.kh[0:nt, :], in_=pkt[0:nt, 0:512]), r=(pb[bkt],), w=(B.khb,))
            yield 'B0'
            ba = palloc(1)
            S.mm_multi([[(ps[0:nt, ba, hd * 128:hd * 128 + nt], B.ks[:, hd, 0:nt], B.qs[:, hd, 0:nt])] for hd in range(4)],
                       r=(B.qsb, B.ksb), w=(pb[ba],))
            aT = ps[0:nt, ba, :].rearrange("p (h t) -> p h t", t=128)[:, :, 0:nt]
            S.op(DVE, lambda: nc.vector.tensor_tensor(out=B.am[0:nt, :, 0:nt], in0=aT, in1=cm[0:nt, 0:nt].unsqueeze(1).to_broadcast([nt, 4, nt]),
                                                      op=ALU.mult), r=(pb[ba], bk), w=(B.amb,))
            yield 'B1'
            def o_post(obanks, ocols):
                st = B.st
                for hd in range(4):
                    o = ps[0:nt, obanks[hd], ocols[hd]:ocols[hd] + 256]
                    S.op(ACT, lambda: nc.scalar.activation(out=junk[0:nt, :], in_=o, func=AF.Square, accum_out=st[0:nt, hd:hd + 1]),
                         r=(pb[obanks[hd]],), w=(jb, B.stb))
                S.op(ACT, lambda: nc.scalar.activation(out=st[0:nt, 4:8], in_=st[0:nt, 0:4], func=AF.Ln, bias=EPS, scale=1.0 / 256.0), r=(B.stb,), w=(B.stb,))
                S.op(ACT, lambda: nc.scalar.activation(out=st[0:nt, 4:8], in_=st[0:nt, 4:8], func=AF.Exp, scale=-0.5), r=(B.stb,), w=(B.stb,))
                for hd in range(4):
                    o = ps[0:nt, obanks[hd], ocols[hd]:ocols[hd] + 256]
                    S.op(DVE, lambda: nc.vector.scalar_tensor_tensor(out=B.ogh[0:nt, hd * 256:(hd + 1) * 256], in0=o, scalar=st[0:nt, 4 + hd:5 + hd],
                                                                     in1=B.sg[0:nt, hd * 256:(hd + 1) * 256], op0=ALU.mult, op1=ALU.mult),
                         r=(pb[obanks[hd]], B.stb, B.sgb), w=(B.oghb,))
                pstate["res"] -= set(obanks)
            if isp:
                bo = palloc(2)
                pstate["res"] |= {bo, bo + 1}
                obanks = [bo, bo, bo + 1, bo + 1]
                ocols = [0, 256, 0, 256]
                for hd in range(4):
                    o = ps[0:nt, obanks[hd], ocols[hd]:ocols[hd] + 256]
                    S.mm([(o, B.qs[:, hd, 0:nt], Sb[:, hd, :]), (o, B.am[0:nt, hd, 0:nt], B.vb[0:nt, hd * 256:(hd + 1) * 256])],
                         r=(B.qsb, Sbb, B.amb, B.vbb), w=(pb[obanks[hd]],))
                o_post(obanks, ocols)
                yield 'B2'
                bs_ = palloc(2)
                S.mm_multi([[(ps[:, bs_ + hd // 2, (hd % 2) * 256:(hd % 2) * 256 + 256], B.kh[0:nt, hd * 128:(hd + 1) * 128],
                              B.vb[0:nt, hd * 256:(hd + 1) * 256])] for hd in range(4)], r=(B.khb, B.vbb), w=(pb[bs_], pb[bs_ + 1]))
                for hd in range(4):
                    S.op(DVE, lambda: nc.vector.scalar_tensor_tensor(out=Sf[:, hd, :], in0=Sf[:, hd, :], scalar=eb[:, hd, nt - 1:nt],
                                                                     in1=ps[:, bs_ + hd // 2, (hd % 2) * 256:(hd % 2) * 256 + 256],
                                                                     op0=ALU.mult, op1=ALU.add), r=(Sfb, B.ebb, pb[bs_ + hd // 2]), w=(Sfb,))
                S.op(ACT, lambda: nc.scalar.copy(out=Sb, in_=Sf), r=(Sfb,), w=(Sbb,))
                if t0 + nt == NPR:
                    S.dma(SP, st_prompt, Sf, r=(Sfb,))
            else:
                S.barrier(engs=[SP, DVE])
                other = 1 - (ci % 2)
                save_off = WA.off
                WA.off = set_off[other]
                NSL = 2
                s0 = [WA.alloc([4, 256], F32) for _ in range(NSL)]
                s0b = [Buf("s0%d" % i) for i in range(NSL)]
                sn = [WA.alloc([4, 256], F32) for _ in range(2)]
                snb = [Buf("sn0"), Buf("sn1")]
                s0h = [WA.alloc([4, 256], BF16) for _ in range(NSL)]
                s0hb = [Buf("s0h%d" % i) for i in range(NSL)]
                Qb = [WA.alloc([4, 64], BF16) for _ in range(NSL)]
                Qbb = [Buf("Qb%d" % i) for i in range(NSL)]
                Kb = [WA.alloc([512], BF16, parts=64) for _ in range(NSL)]
                Kbb = [Buf("Kb%d" % i) for i in range(NSL)]
                lim = set_end if other == 1 else set_off[1]
                assert WA.off <= lim, (WA.off, lim)
                WA.off = save_off
                ob4 = [0, 1, 2, 3]
                pstate["res"] = set(ob4)
                obanks = ob4
                ocols = [0, 0, 0, 0]
                for i in range(NSL):
                    S.op(DVE, lambda: nc.vector.memset(Qb[i], 0.0), w=(Qbb[i],))
                    S.dma(SP, s0[i], sgl[i], w=(s0b[i],))
                def prep(bi):
                    sl = bi % NSL
                    if bi >= NSL:
                        pv_ = bi - NSL
                        S.op(DVE, lambda: nc.vector.memset(Qb[sl][:, :, 4 * pv_:4 * pv_ + 4], 0.0), w=(Qbb[sl],))
                    S.op(DVE, lambda: nc.vector.tensor_copy(out=Qb[sl][:, :, 4 * bi:4 * bi + 4], in_=B.qs[:, :, 4 * bi:4 * bi + 4]),
                         r=(B.qsb,), w=(Qbb[sl],))
                    S.op(DVE, lambda: nc.vector.tensor_scalar(out=Kb[sl], in0=B.kh[0:64, :], scalar1=km[:, bi:bi + 1], scalar2=None, op0=ALU.mult),
                         r=(B.khb,), w=(Kbb[sl],))

                prep(0)
                for bi in range(16):
                    sl = bi % NSL
                    S.op(ACT, lambda: nc.scalar.copy(out=s0h[sl], in_=s0[sl]), r=(s0b[sl],), w=(s0hb[sl],))
                    for hd in range(4):
                        o = ps[0:64, ob4[hd], 0:256]
                        S._waits(PE, (Qbb[sl], s0hb[sl]), (pb[ob4[hd]],))
                        ins = nc.tensor.matmul(o, lhsT=Qb[sl][:, hd, :], rhs=s0h[sl][:, hd, :], start=(bi == 0), stop=False)
                        PE.cnt += 1
                        ins.then_inc(PE.sem, 1)
                        s0hb[sl].r[PE] = PE.cnt
                        Qbb[sl].r[PE] = PE.cnt
                        pb[ob4[hd]].w = (PE, PE.cnt)
                        pb[ob4[hd]].r = {}
                    bs_ = palloc(2)
                    S.mm_multi([[(ps[:, bs_ + hd // 2, (hd % 2) * 256:(hd % 2) * 256 + 256], Kb[sl][:, hd * 128:(hd + 1) * 128],
                                  B.vb[0:64, hd * 256:(hd + 1) * 256])] for hd in range(4)], r=(Kbb[sl], B.vbb), w=(pb[bs_], pb[bs_ + 1]))
                    if bi + 1 < 16:
                        prep(bi + 1)
                    for hd in range(4):
                        S.op(DVE, lambda: nc.vector.scalar_tensor_tensor(out=sn[bi % 2][:, hd, :], in0=s0[sl][:, hd, :], scalar=eb[:, hd, 4 * bi + 3:4 * bi + 4],
                                                                         in1=ps[:, bs_ + hd // 2, (hd % 2) * 256:(hd % 2) * 256 + 256],
                                                                         op0=ALU.mult, op1=ALU.add), r=(s0b[sl], B.ebb, pb[bs_ + hd // 2]), w=(snb[bi % 2],))
                    S.dma(SP, st_sample[bi], sn[bi % 2], r=(snb[bi % 2],))
                    if bi + NSL < 16:
                        S.dma(SP, s0[sl], sgl[bi + NSL], w=(s0b[sl],))
                for hd in range(4):
                    o = ps[0:64, ob4[hd], 0:256]
                    S._waits(PE, (B.amb, B.vbb), (pb[ob4[hd]],))
                    ins = nc.tensor.matmul(o, lhsT=B.am[0:64, hd, 0:64], rhs=B.vb[0:64, hd * 256:(hd + 1) * 256], start=False, stop=True)
                    PE.cnt += 1
                    ins.then_inc(PE.sem, 1)
                    B.amb.r[PE] = PE.cnt
                    B.vbb.r[PE] = PE.cnt
                    pb[ob4[hd]].w = (PE, PE.cnt)
                    pb[ob4[hd]].r = {}
                o_post(obanks, ocols)
                yield 'B2'
            yield 'B3'
            bt = palloc(1)
            ptb = ps[:, bt, :].bitcast(BF16)
            S.transposes([(ptb[:, m * 128:m * 128 + nt], B.ogh[0:nt, m * 128:(m + 1) * 128], identb[0:nt, 0:nt]) for m in range(8)],
                         r=(B.oghb, b_const), w=(pb[bt],))
            ogT = B.ogT
            S.op(ACT, lambda: nc.scalar.copy(out=ogT[:, :, 0:nt], in_=ptb.rearrange("p (m t) -> p m t", t=128)[:, :, 0:nt]), r=(pb[bt],), w=(B.ogTb,))
            yield 'B4'
            for kk in range(8):
                if kk % 2 == 0:
                    bo2 = palloc(1)
                S.mm([(ps[:, bo2, (kk % 2) * 128:(kk % 2) * 128 + nt], ring[:, sD, m, kk * 128:(kk + 1) * 128], ogT[:, m, 0:nt]) for m in range(8)],
                     r=(slotb[sD], B.ogTb), w=(pb[bo2],))
                if kk % 2 == 1:
                    for k2 in (kk - 1, kk):
                        resid_update(bo2, k2, t0, n, isp, G1, None, B.tmps[k2 % 2], B.tb[k2 % 2], b_mod, pcol=(k2 % 2) * 128)
                    yield 'B%d' % (5 + kk // 2)

        gens = [chunk_gen(ci, t0, n, isp) for ci, (t0, n, isp) in enumerate(GT)]
        NG = len(gens)

        def step(c, want):
            if 0 <= c < NG:
                got = next(gens[c])
                assert got == want, (c, got, want)

        step(0, 'F0')
        for c in range(NG + 1):
            for i in range(1, 12):
                step(c, 'F%d' % i)
                if i == 7:
                    step(c + 1, 'F0')
                if i - 1 <= 8:
                    step(c - 1, 'B%d' % (i - 1))
        for cid in ("L0", "L1", "L2", "LOUT"):
            wrelease(cid)

    def final_phase():
        S.barrier()
        CA.reset()
        EA.reset()
        fng = CA.alloc([D], F32)
        bk = Buf("fk")
        S.dma(SP, fng, fng_bc, w=(bk,))
        yo = [EA.alloc([D], F32) for _ in range(2)]
        yob = [Buf("y0"), Buf("y1")]
        junk = EA.alloc([D], BF16)
        jb = Buf("junk")
        st = EA.alloc([4, 2], F32)
        stb = [Buf("st0"), Buf("st1")]
        for ti in range(17):
            t0 = ti * 128
            n = 128 if ti < 16 else 64
            sl = ti % 2
            b = palloc(2)
            S.transposes([(ps[0:n, b + k // 4, (k % 4) * 128:(k % 4 + 1) * 128], xT[:, k, t0:t0 + n], ident) for k in range(8)],
                         r=xb(t0, n) + (b_const,), w=(pb[b], pb[b + 1]))
            pin = ps[0:n, b:b + 2, :].rearrange("p a c -> p (a c)")
            S.op(ACT, lambda: nc.scalar.activation(out=junk[0:n, :], in_=pin, func=AF.Square, accum_out=st[0:n, sl, 0:1]),
                 r=(pb[b], pb[b + 1]), w=(jb, stb[sl]))
            S.op(ACT, lambda: nc.scalar.activation(out=st[0:n, sl, 1:2], in_=st[0:n, sl, 0:1], func=AF.Ln, bias=EPS, scale=1.0 / D), r=(stb[sl],), w=(stb[sl],))
            S.op(ACT, lambda: nc.scalar.activation(out=st[0:n, sl, 1:2], in_=st[0:n, sl, 1:2], func=AF.Exp, scale=-0.5), r=(stb[sl],), w=(stb[sl],))
            S.op(DVE, lambda: nc.vector.scalar_tensor_tensor(out=yo[sl][0:n, :], in0=pin, scalar=st[0:n, sl, 1:2], in1=fng[0:n, :],
                                                             op0=ALU.mult, op1=ALU.mult), r=(pb[b], pb[b + 1], stb[sl], bk), w=(yob[sl],))
            if ti < 16:
                S.dma(SP, y_prompt[t0:t0 + n, :], yo[sl][0:n, :], r=(yob[sl],))
            else:
                S.dma(SP, y_sample, yo[sl][0:n, :], r=(yob[sl],))

    S.barrier()
    ada_chunk(0, 0)
    ada_chunk(0, 1)
    gmlp_phase()
    ffn_phase(0)
    gla_phase()
    ffn_phase(1)
    final_phase()
    for d in S.dsems:
        if d.cnt > 0:
            nc.sync.wait_ge(d.sem, d.cnt)
    for e in (S.PE, S.ACT, S.DVE):
        nc.sync.wait_ge(e.sem, e.cnt)
    return nc


def _prep_shared(inp):
    f = np.float32
    g = lambda k: np.ascontiguousarray(np.asarray(inp[k], dtype=f))
    sh = {}
    for k in ("ada_w", "ffn_w1", "ffn_w2", "gmlp_w_in", "gmlp_w_out", "gla_w_in", "gla_w_out"):
        sh[k] = g(k)
    sh["gla_wa"] = np.ascontiguousarray(np.concatenate([np.zeros((D, 1), f), sh["gla_w_in"][:, 3072:3088]], axis=1))
    rows = [g("ada_b").reshape(96, 128), g("norm_mix_g").reshape(16, 128), g("norm_ffn_g").reshape(16, 128),
            g("ffn_b1").reshape(64, 128), g("ffn_b2").reshape(16, 128), g("gmlp_b_in")[:D].reshape(8, 128),
            g("gmlp_b_out").reshape(8, 128)]
    sh["prow"] = np.ascontiguousarray(np.concatenate(rows, axis=0))
    rep = lambda v: np.ascontiguousarray(np.broadcast_to(v[None, :], (128, v.shape[0])))
    sh["binv_bc"] = rep(g("gmlp_b_in")[D:])
    sh["lng_bc"] = rep(g("gmlp_ln_g"))
    sh["lnb_bc"] = rep(g("gmlp_ln_b"))
    sh["fng_bc"] = rep(g("final_norm_g"))
    sh["gng_bc"] = rep(g("gla_norm_g"))
    ws = g("gmlp_w_s")
    sh["wsT_p"] = np.ascontiguousarray(ws.transpose(2, 0, 1))
    idx = np.arange(64) % 4
    sh["wsT_s"] = np.ascontiguousarray(ws[:, idx[None, :], idx[:, None]].transpose(1, 0, 2))
    bs = g("gmlp_b_s")
    sh["bs_p"] = np.ascontiguousarray(np.broadcast_to(bs[None], (128, 4, 128)))
    sh["bs_s"] = np.ascontiguousarray(np.broadcast_to(bs[None][:, :, idx], (128, 4, 64)))
    j = np.arange(128)
    sh["cm_p"] = (j[:, None] <= j[None, :]).astype(f)
    j6 = np.arange(64)
    same = (j6[:, None] // 4) == (j6[None, :] // 4)
    sh["cm_s"] = ((j6[:, None] <= j6[None, :]) & same).astype(f)
    sh["triN_p"] = (sh["cm_p"] * (-1.0 / 16.0)).astype(f)
    sh["triR_p"] = ((j[:, None] > j[None, :]).astype(f) * (-1.0 / 16.0)).astype(f)
    sh["triN_s"] = (sh["cm_s"] * (-1.0 / 16.0)).astype(f)
    sh["triR_s"] = (((j6[:, None] > j6[None, :]) & same).astype(f) * (-1.0 / 16.0)).astype(f)
    qm = np.zeros((128, 16, 64), f)
    for b in range(16):
        qm[:, b, 4 * b:4 * b + 4] = 1.0
    sh["qmask"] = qm
    km = np.zeros((64, 16), f)
    for b in range(16):
        km[4 * b:4 * b + 4, b] = 1.0
    sh["kmask"] = km
    sh["wg2a"] = np.ascontiguousarray(np.concatenate([g("gla_b_gate")[None, :], g("gla_w_gate2")], axis=0))
    sh["ident"] = np.eye(128, dtype=f)
    return sh


_NC_CACHE = {}


def kernel(**inp):
    f = np.float32
    sh = _prep_shared(inp)
    xp = np.asarray(inp["x_prompt"], f)
    xs = np.asarray(inp["x_sample"], f)
    cp = np.asarray(inp["c_prompt"], f)
    cs = np.asarray(inp["c_sample"], f)
    sg = np.asarray(inp["state_gla"], f)
    in_maps = []
    for c in range(8):
        m = dict(sh)
        m["xin"] = np.ascontiguousarray(np.concatenate([xp[c], xs[16 * c:16 * c + 16].reshape(64, D)], axis=0))
        m["cin"] = np.ascontiguousarray(np.concatenate([cp[c:c + 1], cs[16 * c:16 * c + 16]], axis=0))
        m["sgl"] = np.ascontiguousarray(sg[16 * c:16 * c + 16].transpose(0, 2, 1, 3))
        in_maps.append(m)
    if "nc" not in _NC_CACHE:
        _NC_CACHE["nc"] = build()
    nc = _NC_CACHE["nc"]
    res = run_bass_kernel_spmd(nc, in_maps, core_ids=list(range(8)))
    R = res.results
    y_prompt = np.stack([R[c]["y_prompt"] for c in range(8)], axis=0).astype(f)
    y_sample = np.concatenate([R[c]["y_sample"].reshape(16, 4, D) for c in range(8)], axis=0).astype(f)
    st_p = np.stack([np.asarray(R[c]["st_prompt"]).transpose(1, 0, 2) for c in range(8)], axis=0).astype(f)
    st_s = np.concatenate([np.asarray(R[c]["st_sample"]).transpose(0, 2, 1, 3) for c in range(8)], axis=0).astype(f)
    v_s = np.concatenate([R[c]["v_sample"].reshape(16, 4, D) for c in range(8)], axis=0).astype(f)
    return (y_prompt, y_sample, st_p, st_s, v_s)
```

```python
import numpy as np
import concourse.bass as bass
import concourse.mybir as mybir
from concourse.bass_utils import run_bass_kernel_spmd

F32 = mybir.dt.float32
BF16 = mybir.dt.bfloat16
AF = mybir.ActivationFunctionType
ALU = mybir.AluOpType

D = 1024
NPR = 2048
NSA = 64
NTOK = NPR + NSA
NB = 17
EPS = 1e-6
TILES = [(0, 512, True), (512, 512, True), (1024, 512, True), (1536, 512, True), (2048, 64, False)]
NSLOT = 4
SAME_ENG_WAR = False

R_ADAB, R_NMG, R_NFG, R_B1, R_B2, R_BINU, R_BOUT, R_TOT = 0, 96, 112, 128, 192, 208, 216, 224


class Eng:
    def __init__(self, nc, name, h):
        self.name = name
        self.h = h
        self.sem = nc.alloc_semaphore("s_" + name)
        self.cnt = 0
        self.seen = {}
        self.raw_self = name in ("act", "dve", "pool")


class Buf:
    __slots__ = ("name", "w", "r")

    def __init__(self, name=""):
        self.name = name
        self.w = None
        self.r = {}


class Sched:
    def __init__(self, nc):
        self.nc = nc
        self.PE = Eng(nc, "pe", nc.tensor)
        self.ACT = Eng(nc, "act", nc.scalar)
        self.DVE = Eng(nc, "dve", nc.vector)
        self.SP = Eng(nc, "sp", nc.sync)
        self.POOL = Eng(nc, "pool", nc.gpsimd)
        self.engs = [self.PE, self.ACT, self.DVE, self.SP, self.POOL]
        self.dsems = [Eng(nc, "d%d" % i, None) for i in range(12)]
        self.wsems = [Eng(nc, "w%d" % i, None) for i in range(NSLOT)]
        self.xsem = Eng(nc, "wx", None)
        self.dnext = 0

    def _waits(self, eng, r, w):
        need = {}
        for b in r:
            if b.w is not None and (b.w[0] is not eng or eng.raw_self):
                need[b.w[0]] = max(need.get(b.w[0], 0), b.w[1])
        full = eng.raw_self and SAME_ENG_WAR
        for b in w:
            if b.w is not None and (b.w[0] is not eng or full):
                need[b.w[0]] = max(need.get(b.w[0], 0), b.w[1])
            for e, c in b.r.items():
                if e is not eng or full:
                    need[e] = max(need.get(e, 0), c)
        for e, c in need.items():
            if eng.seen.get(e, 0) < c:
                eng.h.wait_ge(e.sem, c)
                eng.seen[e] = c

    def op(self, eng, fn, r=(), w=()):
        self._waits(eng, r, w)
        ins = fn()
        eng.cnt += 1
        ins.then_inc(eng.sem, 1)
        for b in w:
            b.w = (eng, eng.cnt)
            b.r = {}
        for b in r:
            b.r[eng] = eng.cnt

    def mm(self, mms, r=(), w=()):
        eng = self.PE
        self._waits(eng, r, w)
        n = len(mms)
        ins = None
        for i, (o, l, rh) in enumerate(mms):
            ins = self.nc.tensor.matmul(o, lhsT=l, rhs=rh, start=(i == 0), stop=(i == n - 1))
        eng.cnt += 1
        ins.then_inc(eng.sem, 1)
        for b in w:
            b.w = (eng, eng.cnt)
            b.r = {}
        for b in r:
            b.r[eng] = eng.cnt

    def mm_multi(self, groups, r=(), w=()):
        eng = self.PE
        self._waits(eng, r, w)
        ins = None
        for mms in groups:
            n = len(mms)
            for i, (o, l, rh) in enumerate(mms):
                ins = self.nc.tensor.matmul(o, lhsT=l, rhs=rh, start=(i == 0), stop=(i == n - 1))
        eng.cnt += 1
        ins.then_inc(eng.sem, 1)
        for b in w:
            b.w = (eng, eng.cnt)
            b.r = {}
        for b in r:
            b.r[eng] = eng.cnt

    def transposes(self, items, r=(), w=()):
        eng = self.PE
        self._waits(eng, r, w)
        ins = None
        for (o, i_, ident) in items:
            ins = self.nc.tensor.transpose(o, i_, ident)
        eng.cnt += 1
        ins.then_inc(eng.sem, 1)
        for b in w:
            b.w = (eng, eng.cnt)
            b.r = {}
        for b in r:
            b.r[eng] = eng.cnt

    def dma(self, q, out, in_, r=(), w=(), sem=None):
        if sem is None:
            sem = self.dsems[self.dnext % len(self.dsems)]
            self.dnext += 1
        self._waits(q, r, w)
        ins = q.h.dma_start(out=out, in_=in_)
        sem.cnt += 16
        ins.then_inc(sem.sem, 16)
        for b in w:
            b.w = (sem, sem.cnt)
            b.r = {}
        for b in r:
            b.r[sem] = sem.cnt

    def wait_dmas(self, engs=None, with_x=False):
        engs = engs or [self.PE, self.ACT, self.DVE]
        for e in engs:
            for o in self.dsems + ([self.xsem] if with_x else []):
                if e.seen.get(o, 0) < o.cnt:
                    e.h.wait_ge(o.sem, o.cnt)
                    e.seen[o] = o.cnt

    def barrier(self, engs=None):
        engs = engs or [self.PE, self.ACT, self.DVE, self.SP]
        allp = [self.PE, self.ACT, self.DVE, self.SP] + self.dsems + [self.xsem]
        for e in engs:
            for o in allp:
                if o is e:
                    continue
                if e.seen.get(o, 0) < o.cnt:
                    e.h.wait_ge(o.sem, o.cnt)
                    e.seen[o] = o.cnt


class Arena:
    def __init__(self, nc, name, nbytes):
        self.t = nc.alloc_sbuf_tensor(name, [128, nbytes // 4], F32)
        self.nbytes = nbytes
        self.off = 0

    def reset(self):
        self.off = 0

    def alloc(self, shape, dtype, parts=128):
        esz = 4 if dtype == F32 else 2
        n = 1
        for s in shape:
            n *= s
        nb = (n * esz + 31) // 32 * 32
        assert self.off + nb <= self.nbytes, (self.off, nb, self.nbytes)
        o4 = self.off // 4
        ap = self.t[0:parts, o4:o4 + nb // 4]
        if dtype != F32:
            ap = ap.bitcast(dtype)
        ap = ap[:, 0:n]
        if len(shape) == 2:
            ap = ap.rearrange("p (a b) -> p a b", b=shape[1])
        elif len(shape) == 3:
            ap = ap.rearrange("p (a b c) -> p a b c", b=shape[1], c=shape[2])
        self.off += nb
        return ap


def build():
    nc = bass.Bass("TRN2", target_bir_lowering=False)
    S = Sched(nc)
    PE, ACT, DVE, SP, POOL = S.PE, S.ACT, S.DVE, S.SP, S.POOL

    def din(name, shape, dt=F32):
        return nc.dram_tensor(name, list(shape), dt, kind="ExternalInput").ap()

    def dout(name, shape):
        return nc.dram_tensor(name, list(shape), F32, kind="ExternalOutput").ap()

    xin = din("xin", [NTOK, D])
    cin = din("cin", [NB, D])
    sgl = din("sgl", [16, 128, 4, 256])
    ada_w = din("ada_w", [2, D, 6 * D])
    ffn_w1 = din("ffn_w1", [2, D, 4 * D])
    ffn_w2 = din("ffn_w2", [2, 4 * D, D])
    gmlp_w_in = din("gmlp_w_in", [D, 2 * D])
    gmlp_w_out = din("gmlp_w_out", [D, D])
    gla_w_in = din("gla_w_in", [D, 3088])
    gla_wa = din("gla_wa", [D, 17])
    gla_w_out = din("gla_w_out", [D, D])
    prow = din("prow", [R_TOT, 128])
    binv_bc = din("binv_bc", [128, D])
    lng_bc = din("lng_bc", [128, D])
    lnb_bc = din("lnb_bc", [128, D])
    fng_bc = din("fng_bc", [128, D])
    gng_bc = din("gng_bc", [128, 256])
    wsT_p = din("wsT_p", [128, 4, 128])
    wsT_s = din("wsT_s", [64, 4, 64])
    bs_p = din("bs_p", [128, 4, 128])
    bs_s = din("bs_s", [128, 4, 64])
    cm_p = din("cm_p", [128, 128])
    cm_s = din("cm_s", [64, 64])
    triN_p = din("triN_p", [128, 128])
    triR_p = din("triR_p", [128, 128])
    triN_s = din("triN_s", [64, 64])
    triR_s = din("triR_s", [64, 64])
    qmask = din("qmask", [128, 16, 64])
    kmask = din("kmask", [64, 16])
    wg2a = din("wg2a", [17, 512])
    ident_d = din("ident", [128, 128])

    y_prompt = dout("y_prompt", [NPR, D])
    y_sample = dout("y_sample", [NSA, D])
    st_prompt = dout("st_prompt", [128, 4, 256])
    st_sample = dout("st_sample", [16, 128, 4, 256])
    v_sample = dout("v_sample", [NSA, D])

    xT = nc.alloc_sbuf_tensor("xT", [128, 8, NTOK], F32)
    ring = nc.alloc_sbuf_tensor("ring", [128, NSLOT, 8, 1024], BF16)
    KA = Arena(nc, "KA", 10752)
    rem = nc.sbuf_bytes_remaining - 64
    WA = Arena(nc, "WA", (rem // 32) * 32)
    CA = WA
    EA = WA
    ps = nc.alloc_psum_tensor("ps", [128, 8, 512], F32)
    pb = [Buf("ps%d" % i) for i in range(8)]
    pstate = {"n": 0, "res": set()}

    def palloc(n=1):
        while True:
            if n == 2 and pstate["n"] % 2 == 1:
                pstate["n"] += 1
            b = pstate["n"] % 8
            pstate["n"] += n
            if all(((b + i) % 8) not in pstate["res"] for i in range(n)):
                return b

    ident = KA.alloc([128], F32)
    identb = KA.alloc([128], BF16)
    onesb = KA.alloc([128], BF16)
    PF = KA.alloc([R_TOT], F32)
    MODS = [KA.alloc([6, 8, NB], F32) for _ in range(2)]
    GB1 = KA.alloc([8, NB], F32)
    GB2S = [KA.alloc([8, NB], F32) for _ in range(2)]
    scT = KA.alloc([8, NB], BF16)
    wa = KA.alloc([8, 17], BF16)
    xTbs = [Buf("xT%d" % i) for i in range(17)]

    def xb(t0, n):
        return tuple(xTbs[i] for i in range(t0 // 128, (t0 + n + 127) // 128))
    b_const = Buf("const")
    b_mods = [Buf("mod0"), Buf("mod1")]

    def wview(ap2d):
        return ap2d.rearrange("(k p) n -> p k n", p=128)

    cdef = {}
    for l in range(2):
        for j in range(6):
            cdef["A%d_%d" % (l, j)] = wview(ada_w[l, :, j * 1024:(j + 1) * 1024])
        for q in range(4):
            cdef["F%d_1_%d" % (l, q)] = wview(ffn_w1[l, :, q * 1024:(q + 1) * 1024])
            cdef["F%d_2_%d" % (l, q)] = wview(ffn_w2[l, q * 1024:(q + 1) * 1024, :])
    cdef["GIN0"] = wview(gmlp_w_in[:, 0:1024])
    cdef["GIN1"] = wview(gmlp_w_in[:, 1024:2048])
    cdef["GOUT"] = wview(gmlp_w_out[:, :])
    for j in range(3):
        cdef["L%d" % j] = wview(gla_w_in[:, j * 1024:(j + 1) * 1024])
    cdef["LOUT"] = wview(gla_w_out[:, :])
    seq = ["A0_0", "A0_1", "GIN0", "GIN1", "A0_2", "GOUT", "A0_3", "A0_4", "A0_5"]
    for q in range(3):
        seq += ["F0_1_%d" % q, "F0_2_%d" % q]
    seq += ["A1_0", "A1_1", "F0_1_3", "F0_2_3", "A1_2", "A1_3", "A1_4", "A1_5", "L0", "L1", "L2", "LOUT"]
    for q in range(4):
        seq += ["F1_1_%d" % q, "F1_2_%d" % q]
    slotb = [Buf("slot%d" % i) for i in range(NSLOT)]
    wstate = {"next": 0, "free": list(range(NSLOT)), "slot": {}}

    def wpump():
        while wstate["free"] and wstate["next"] < len(seq):
            sl = wstate["free"].pop(0)
            cid = seq[wstate["next"]]
            wstate["next"] += 1
            S.dma(POOL, ring[:, sl, :, :], cdef[cid], r=(), w=(slotb[sl],), sem=S.wsems[sl])
            wstate["slot"][cid] = sl

    def wslot(cid):
        assert cid in wstate["slot"], (cid, wstate["next"])
        return wstate["slot"][cid]

    def wrelease(cid):
        wstate["free"].append(wstate["slot"].pop(cid))
        wpump()
    wpump()

    S.dma(SP, ident, ident_d, w=(b_const,))
    EA.reset()
    prt = EA.alloc([128], F32)
    prt2 = EA.alloc([128], F32, parts=96)
    ct = EA.alloc([D], F32, parts=NB)
    b_pr = Buf("pr")
    S.dma(SP, prt, prow[0:128, :])
    S.dma(SP, prt2, prow[128:224, :])
    S.dma(SP, ct, cin)
    waf = EA.alloc([8, 17], F32)
    S.dma(SP, waf, gla_wa.rearrange("(k p) n -> p k n", p=128))
    S.wait_dmas()
    S.op(DVE, lambda: nc.vector.tensor_copy(out=wa, in_=waf), w=(b_const,))
    S.op(DVE, lambda: nc.vector.tensor_copy(out=identb, in_=ident), r=(b_const,), w=(b_const,))
    S.op(DVE, lambda: nc.vector.memset(onesb, 1.0 / 1024.0), w=(b_const,))
    b0 = palloc(1)
    S.transposes([(ps[:, b0, 0:128], prt, ident), (ps[:, b0, 128:224], prt2, ident[0:96, 0:96])],
                 r=(b_pr, b_const), w=(pb[b0],))
    S.op(DVE, lambda: nc.vector.tensor_copy(out=PF, in_=ps[:, b0, 0:R_TOT]), r=(pb[b0],), w=(b_const,))
    csl = EA.alloc([D], F32, parts=NB)
    b_c = Buf("c")
    S.op(ACT, lambda: nc.scalar.activation(out=csl, in_=ct, func=AF.Silu), r=(b_pr,), w=(b_c,))
    b1_ = palloc(1)
    S.transposes([(ps[:, b1_, k * NB:(k + 1) * NB], csl[:, k * 128:(k + 1) * 128], ident[0:NB, 0:NB]) for k in range(8)],
                 r=(b_c, b_const), w=(pb[b1_],))
    S.op(DVE, lambda: nc.vector.tensor_copy(out=scT, in_=ps[:, b1_, 0:8 * NB].rearrange("p (k b) -> p k b", b=NB)),
         r=(pb[b1_],), w=(b_const,))

    NXS = 8
    xst = [EA.alloc([D], F32) for _ in range(NXS)]
    xsb = [Buf("xs%d" % i) for i in range(NXS)]

    def xload(ti):
        t0 = ti * 128
        n = 128 if ti < 16 else 64
        S.dma(SP, xst[ti % NXS][0:n, :], xin[t0:t0 + n, :], w=(xsb[ti % NXS],))

    for ti in range(NXS):
        xload(ti)
    for ti in range(17):
        t0 = ti * 128
        n = 128 if ti < 16 else 64
        sl = ti % NXS
        b = palloc(2)
        S.transposes([(ps[:, b + k // 4, (k % 4) * 128:(k % 4) * 128 + n], xst[sl][0:n, k * 128:(k + 1) * 128], ident[0:n, 0:n])
                      for k in range(8)], r=(xsb[sl], b_const), w=(pb[b], pb[b + 1]))
        src = ps[:, b:b + 2, :].rearrange("p a (k t) -> p (a k) t", t=128)[:, :, 0:n]
        eng = ACT if ti % 2 == 0 else DVE
        if eng is ACT:
            S.op(ACT, lambda: nc.scalar.copy(out=xT[:, :, t0:t0 + n], in_=src), r=(pb[b], pb[b + 1]), w=xb(t0, n))
        else:
            S.op(DVE, lambda: nc.vector.tensor_copy(out=xT[:, :, t0:t0 + n], in_=src), r=(pb[b], pb[b + 1]), w=xb(t0, n))
        if ti + NXS < 17:
            xload(ti + NXS)

    def modsel(M, k, isp):
        if isp:
            return M[:, k, 0:1]
        return M[:, k, 1:17].unsqueeze(2).to_broadcast([128, 16, 4])

    def v3(ap, isp):
        return ap if isp else ap.rearrange("p (b t) -> p b t", t=4)

    def ada_chunk(l, j):
        MOD = MODS[l]
        bm = b_mods[l]
        cid = "A%d_%d" % (l, j)
        s_ = wslot(cid)
        b = palloc(1)
        groups = []
        for m in range(8):
            groups.append([(ps[:, b, m * NB:(m + 1) * NB], ring[:, s_, k, m * 128:(m + 1) * 128], scT[:, k, :]) for k in range(8)])
        S.mm_multi(groups, r=(slotb[s_], b_const), w=(pb[b],))
        wrelease(cid)
        bias = PF[:, R_ADAB + l * 48 + j * 8:R_ADAB + l * 48 + j * 8 + 8].unsqueeze(2).to_broadcast([128, 8, NB])
        S.op(DVE, lambda: nc.vector.tensor_tensor(out=MOD[:, j, :, :], in0=ps[:, b, 0:8 * NB].rearrange("p (m b) -> p m b", b=NB),
                                                  in1=bias, op=ALU.add), r=(pb[b], b_const), w=(bm,))
        if j == 1:
            nmg = PF[:, R_NMG + l * 8:R_NMG + l * 8 + 8].unsqueeze(2).to_broadcast([128, 8, NB])
            S.op(DVE, lambda: nc.vector.scalar_tensor_tensor(out=MOD[:, 1, :, :], in0=MOD[:, 1, :, :], scalar=1.0, in1=nmg, op0=ALU.add, op1=ALU.mult),
                 r=(bm, b_const), w=(bm,))
        if j == 4:
            nfg = PF[:, R_NFG + l * 8:R_NFG + l * 8 + 8].unsqueeze(2).to_broadcast([128, 8, NB])
            S.op(DVE, lambda: nc.vector.scalar_tensor_tensor(out=MOD[:, 4, :, :], in0=MOD[:, 4, :, :], scalar=1.0, in1=nfg, op0=ALU.add, op1=ALU.mult),
                 r=(bm, b_const), w=(bm,))
        if j == 5:
            b2 = PF[:, R_B2 + l * 8:R_B2 + l * 8 + 8].unsqueeze(2).to_broadcast([128, 8, NB])
            S.op(DVE, lambda: nc.vector.tensor_tensor(out=GB2S[l], in0=MOD[:, 5, :, :], in1=b2, op=ALU.mult), r=(bm, b_const), w=(bm,))
        if j == 2 and l == 0:
            bo = PF[:, R_BOUT:R_BOUT + 8].unsqueeze(2).to_broadcast([128, 8, NB])
            S.op(DVE, lambda: nc.vector.tensor_tensor(out=GB1, in0=MOD[:, 2, :, :], in1=bo, op=ALU.mult), r=(bm, b_const), w=(bm,))

    def norm_tile(t0, n, isp, GM, SH, hout, hbuf, tmps, tb, sq_, sqb, rs_, rsb, b_mod, part=0, so=0):
        sq = sq_[:, :, so:so + n]
        rs = rs_[:, so:so + n]
        if part in (0, 1):
            S.op(ACT, lambda: nc.scalar.activation(out=sq[:, :, 0:n], in_=xT[:, :, t0:t0 + n], func=AF.Square), r=xb(t0, n), w=(sqb,))
        if part == 1:
            return
        b = palloc(1)
        S.mm([(ps[:, b, 0:n], onesb, sq[:, k, 0:n]) for k in range(8)], r=(sqb, b_const), w=(pb[b],))
        S.op(ACT, lambda: nc.scalar.activation(out=rs[:, 0:n], in_=ps[:, b, 0:n], func=AF.Ln, bias=EPS, scale=1.0), r=(pb[b],), w=(rsb,))
        S.op(ACT, lambda: nc.scalar.activation(out=rs[:, 0:n], in_=rs[:, 0:n], func=AF.Exp, scale=-0.5), r=(rsb,), w=(rsb,))
        for k in range(8):
            tmp = tmps[k % 2]
            tbb = tb[k % 2]
            S.op(DVE, lambda: nc.vector.tensor_tensor(out=tmp[:, 0:n], in0=xT[:, k, t0:t0 + n], in1=rs[:, 0:n], op=ALU.mult),
                 r=xb(t0, n) + (rsb,), w=(tbb,))
            if isp:
                S.op(ACT, lambda: nc.scalar.activation(out=hout[:, k, 0:n], in_=tmp[:, 0:n], func=AF.Identity,
                                                       scale=GM[:, k, 0:1], bias=SH[:, k, 0:1]), r=(tbb, b_mod), w=(hbuf,))
            else:
                S.op(DVE, lambda: nc.vector.tensor_tensor(out=v3(tmp[:, 0:n], False), in0=v3(tmp[:, 0:n], False), in1=modsel(GM, k, False), op=ALU.mult),
                     r=(tbb, b_mod), w=(tbb,))
                S.op(DVE, lambda: nc.vector.tensor_tensor(out=v3(hout[:, k, 0:n], False), in0=v3(tmp[:, 0:n], False), in1=modsel(SH, k, False), op=ALU.add),
                     r=(tbb, b_mod), w=(hbuf,))

    def resid_update(b, k, t0, n, isp, G, GB, tmp, tbb, b_mod, pcol=0):
        xs = xT[:, k, t0:t0 + n]
        psv = ps[:, b, pcol:pcol + n]
        if isp:
            if GB is not None:
                S.op(ACT, lambda: nc.scalar.activation(out=tmp[:, 0:n], in_=psv, func=AF.Identity, scale=G[:, k, 0:1], bias=GB[:, k, 0:1]),
                     r=(pb[b], b_mod), w=(tbb,))
            else:
                S.op(DVE, lambda: nc.vector.scalar_tensor_tensor(out=xs, in0=psv, scalar=G[:, k, 0:1], in1=xs, op0=ALU.mult, op1=ALU.add),
                     r=(pb[b], b_mod) + xb(t0, n), w=xb(t0, n))
                return
            S.op(DVE, lambda: nc.vector.tensor_tensor(out=xs, in0=xs, in1=tmp[:, 0:n], op=ALU.add), r=(tbb,) + xb(t0, n), w=xb(t0, n))
        else:
            S.op(DVE, lambda: nc.vector.tensor_tensor(out=v3(tmp[:, 0:n], False), in0=v3(psv, False), in1=modsel(G, k, False), op=ALU.mult),
                 r=(pb[b], b_mod), w=(tbb,))
            if GB is not None:
                S.op(DVE, lambda: nc.vector.tensor_tensor(out=v3(tmp[:, 0:n], False), in0=v3(tmp[:, 0:n], False), in1=modsel(GB, k, False), op=ALU.add),
                     r=(tbb, b_mod), w=(tbb,))
            S.op(DVE, lambda: nc.vector.tensor_tensor(out=xs, in0=xs, in1=tmp[:, 0:n], op=ALU.add), r=(tbb,) + xb(t0, n), w=xb(t0, n))

    def gmlp_phase():
        S.barrier()
        CA.reset()
        EA.reset()
        MOD = MODS[0]
        b_mod = b_mods[0]
        GM1 = MOD[:, 1, :, :]
        s_in0, s_in1, s_out = wslot("GIN0"), wslot("GIN1"), wslot("GOUT")
        binv = CA.alloc([D], F32)
        lng = CA.alloc([D], F32)
        lnb = CA.alloc([D], F32)
        bsp = CA.alloc([4, 128], F32)
        bss = CA.alloc([4, 64], F32)
        wsp = CA.alloc([4, 128], BF16)
        wss = CA.alloc([4, 64], BF16, parts=64)
        vgs = [CA.alloc([D], F32) for _ in range(2)]
        vgb = [Buf("vg0"), Buf("vg1")]
        mark = WA.off
        WA.off = mark - 4096
        cmp_ = CA.alloc([128], F32)
        cms = CA.alloc([64], F32, parts=64)
        wtmp = CA.alloc([4, 128], F32)
        wtmps = CA.alloc([4, 64], F32, parts=64)
        assert WA.off <= mark
        WA.off = mark
        bk = Buf("gk")
        for (dst, src) in [(binv, binv_bc), (lng, lng_bc), (lnb, lnb_bc), (bsp, bs_p), (bss, bs_s), (cmp_, cm_p), (cms, cm_s),
                           (wtmp, wsT_p), (wtmps, wsT_s)]:
            S.dma(SP, dst, src)
        S.wait_dmas()
        S.op(DVE, lambda: nc.vector.tensor_tensor(out=wsp, in0=wtmp, in1=cmp_.unsqueeze(1).to_broadcast([128, 4, 128]), op=ALU.mult), r=(bk,), w=(bk,))
        S.op(DVE, lambda: nc.vector.tensor_tensor(out=wss, in0=wtmps, in1=cms.unsqueeze(1).to_broadcast([64, 4, 64]), op=ALU.mult), r=(bk,), w=(bk, vgb[1]))
        hT = EA.alloc([8, 512], BF16)
        hb = Buf("h")
        sq = EA.alloc([8, 512], BF16)
        sqb = Buf("sq")
        rs = EA.alloc([512], F32)
        rsb = Buf("rs")
        tmps = [EA.alloc([512], F32) for _ in range(2)]
        tb = [Buf("t0"), Buf("t1")]
        uTs = [EA.alloc([8, 512], BF16) for _ in range(2)]
        ubs = [Buf("u0"), Buf("u1")]
        vbfs = [EA.alloc([D], BF16) for _ in range(2)]
        vbb = [Buf("vb0"), Buf("vb1")]
        junk = sq.rearrange("p a b -> p (a b)")
        jb = sqb
        sts = [EA.alloc([8], F32) for _ in range(2)]
        stbs = [Buf("st0"), Buf("st1")]
        SH1 = MOD[:, 0, :, :]
        G1 = MOD[:, 2, :, :]
        print('gMLP arena', WA.off, WA.nbytes)
        pend = []
        for ti_, (t0, n, isp) in enumerate(TILES):
            nsub = (n + 127) // 128
            uT = uTs[ti_ % 2]
            usT = uT
            ub = ubs[ti_ % 2]
            usb = ub

            def U(m):
                b = palloc(1)
                S.mm([(ps[:, b, 0:n], ring[:, s_in0, k, m * 128:(m + 1) * 128], hT[:, k, 0:n]) for k in range(8)],
                     r=(slotb[s_in0], hb), w=(pb[b],))
                S.op(ACT, lambda: nc.scalar.activation(out=uT[:, m, 0:n], in_=ps[:, b, 0:n], func=AF.Gelu_apprx_tanh,
                                                       bias=PF[:, R_BINU + m:R_BINU + m + 1]), r=(pb[b], b_const), w=(ub,))

            def Va(j):
                nt = min(128, n - j * 128)
                vg = vgs[j % 2]
                vb_ = vgb[j % 2]
                vbf = vbfs[j % 2]
                st = sts[j % 2]
                stb = stbs[j % 2]
                b = palloc(2)
                for half in range(2):
                    S.mm([(ps[0:nt, b + half, :], hT[:, k, j * 128:j * 128 + nt], ring[:, s_in1, k, half * 512:(half + 1) * 512]) for k in range(8)],
                         r=(slotb[s_in1], hb), w=(pb[b + half],))
                pv = ps[0:nt, b:b + 2, :].rearrange("p a c -> p (a c)")
                S.op(DVE, lambda: nc.vector.tensor_tensor(out=vg[0:nt, :], in0=pv, in1=binv[0:nt, :], op=ALU.add), r=(pb[b], pb[b + 1], bk), w=(vb_,))
                S.op(ACT, lambda: nc.scalar.activation(out=vg[0:nt, :], in_=vg[0:nt, :], func=AF.Gelu_apprx_tanh, accum_out=st[0:nt, 0:1]),
                     r=(vb_,), w=(vb_, stb))
                S.op(ACT, lambda: nc.scalar.activation(out=junk[0:nt, 0:D], in_=vg[0:nt, :], func=AF.Square, accum_out=st[0:nt, 1:2]),
                     r=(vb_,), w=(jb, stb))
                S.op(DVE, lambda: nc.vector.tensor_scalar(out=st[0:nt, 2:3], in0=st[0:nt, 0:1], scalar1=1.0 / D, scalar2=None, op0=ALU.mult), r=(stb,), w=(stb,))
                S.op(DVE, lambda: nc.vector.tensor_tensor(out=st[0:nt, 3:4], in0=st[0:nt, 2:3], in1=st[0:nt, 2:3], op=ALU.mult), r=(stb,), w=(stb,))
                S.op(DVE, lambda: nc.vector.scalar_tensor_tensor(out=st[0:nt, 4:5], in0=st[0:nt, 1:2], scalar=1.0 / D, in1=st[0:nt, 3:4],
                                                                 op0=ALU.mult, op1=ALU.subtract), r=(stb,), w=(stb,))

            def Vb(j):
                nt = min(128, n - j * 128)
                vg = vgs[j % 2]
                vb_ = vgb[j % 2]
                vbf = vbfs[j % 2]
                st = sts[j % 2]
                stb = stbs[j % 2]
                S.op(ACT, lambda: nc.scalar.activation(out=st[0:nt, 5:6], in_=st[0:nt, 4:5], func=AF.Ln, bias=EPS, scale=1.0), r=(stb,), w=(stb,))
                S.op(ACT, lambda: nc.scalar.activation(out=st[0:nt, 5:6], in_=st[0:nt, 5:6], func=AF.Exp, scale=-0.5), r=(stb,), w=(stb,))
                S.op(DVE, lambda: nc.vector.scalar_tensor_tensor(out=st[0:nt, 6:7], in0=st[0:nt, 2:3], scalar=-1.0, in1=st[0:nt, 5:6],
                                                                 op0=ALU.mult, op1=ALU.mult), r=(stb,), w=(stb,))
                S.op(ACT, lambda: nc.scalar.activation(out=vg[0:nt, :], in_=vg[0:nt, :], func=AF.Identity, scale=st[0:nt, 5:6], bias=st[0:nt, 6:7]),
                     r=(vb_, stb), w=(vb_,))
                S.op(DVE, lambda: nc.vector.tensor_tensor(out=vg[0:nt, :], in0=vg[0:nt, :], in1=lng[0:nt, :], op=ALU.mult), r=(vb_, bk), w=(vb_,))
                if isp:
                    S.op(DVE, lambda: nc.vector.tensor_tensor(out=vbf[0:nt, :], in0=vg[0:nt, :], in1=lnb[0:nt, :], op=ALU.add), r=(vb_, bk), w=(vbb[j % 2],))
                else:
                    S.op(DVE, lambda: nc.vector.tensor_tensor(out=vg[0:nt, :], in0=vg[0:nt, :], in1=lnb[0:nt, :], op=ALU.add), r=(vb_, bk), w=(vb_,))
                    S.dma(SP, v_sample, vg[0:nt, :], r=(vb_,))
                    S.op(ACT, lambda: nc.scalar.copy(out=vbf[0:nt, :], in_=vg[0:nt, :]), r=(vb_,), w=(vbb[j % 2],))

            def SPA(j):
                nt = min(128, n - j * 128)
                vbf = vbfs[j % 2]
                b = palloc(2)
                wsm = wsp if isp else wss
                groups = []
                for m in range(8):
                    o = ps[:, b + m // 4, (m % 4) * 128:(m % 4) * 128 + nt]
                    groups.append([(o, vbf[0:nt, m * 128:(m + 1) * 128], wsm[0:nt, m // 2, 0:nt])])
                S.mm_multi(groups, r=(vbb[j % 2], bk), w=(pb[b], pb[b + 1]))
                bsm = bsp if isp else bss
                for a in range(2):
                    pin = ps[:, b + a, :].rearrange("p (g r i) -> p g r i", r=2, i=128)[:, :, :, 0:nt]
                    bsv = bsm[:, 2 * a:2 * a + 2, 0:nt].unsqueeze(2).to_broadcast([128, 2, 2, nt])
                    tt = tmps[a]
                    tv = tt[:, 0:4 * nt].rearrange("p (g r i) -> p g r i", r=2, i=nt)
                    S.op(DVE, lambda: nc.vector.tensor_tensor(out=tv, in0=pin, in1=bsv, op=ALU.add), r=(pb[b + a], bk), w=(tb[a],))
                    uo = usT[:, 4 * a:4 * a + 4, j * 128:j * 128 + nt]
                    ui = uT[:, 4 * a:4 * a + 4, j * 128:j * 128 + nt]
                    tv2 = tt[:, 0:4 * nt].rearrange("p (m i) -> p m i", i=nt)
                    S.op(DVE, lambda: nc.vector.tensor_tensor(out=uo, in0=tv2, in1=ui, op=ALU.mult), r=(tb[a], ub), w=(usb,))

            def make_O(kk, t0=t0, n=n, isp=isp, usT=usT, usb=usb):
                def O_one():
                    b = palloc(1)
                    S.mm([(ps[:, b, 0:n], ring[:, s_out, m, kk * 128:(kk + 1) * 128], usT[:, m, 0:n]) for m in range(8)],
                         r=(slotb[s_out], usb), w=(pb[b],))
                    resid_update(b, kk, t0, n, isp, G1, GB1, tmps[kk % 2], tb[kk % 2], b_mod)
                return O_one

            def flush(k=99):
                while pend and k > 0:
                    pend.pop(0)()
                    k -= 1

            def next_norm():
                if ti_ + 1 < len(TILES):
                    t1, n1, isp1 = TILES[ti_ + 1]
                    norm_tile(t1, n1, isp1, GM1, SH1, hT, hb, tmps, tb, sq, sqb, rs, rsb, b_mod)

            if ti_ == 0:
                norm_tile(t0, n, isp, GM1, SH1, hT, hb, tmps, tb, sq, sqb, rs, rsb, b_mod)
            if nsub == 4:
                Va(0); Va(1); U(0); U(1); Vb(0); U(2); U(3); Vb(1); U(4); U(5); U(6); U(7)
                SPA(0); Va(2); SPA(1); Va(3)
                next_norm()
                flush(4)
                Vb(2)
                flush(2)
                Vb(3)
                flush()
                SPA(2); SPA(3)
            else:
                Va(0)
                for m in range(4):
                    U(m)
                Vb(0)
                for m in range(4, 8):
                    U(m)
                flush()
                SPA(0)
            if t0 == 0:
                ada_chunk(0, 2)
            for kk in range(8):
                pend.append(make_O(kk))
            if t0 == 0:
                ada_chunk(0, 3)
            elif t0 == 512:
                ada_chunk(0, 4)
            elif t0 == 1024:
                ada_chunk(0, 5)
        while pend:
            pend.pop(0)()
        wrelease("GIN0")
        wrelease("GIN1")
        wrelease("GOUT")

    def ffn_phase(l):
        S.barrier()
        CA.reset()
        EA.reset()
        MOD = MODS[l]
        b_mod = b_mods[l]
        GM2 = MOD[:, 4, :, :]
        GB2 = GB2S[l]
        hall = CA.alloc([8, NTOK], BF16)
        hb = Buf("hall")
        sq = EA.alloc([8, 512], BF16)
        sqb = Buf("sq")
        rs = EA.alloc([512], F32)
        rsb = Buf("rs")
        tmps = [EA.alloc([512], F32) for _ in range(2)]
        tb = [Buf("t0"), Buf("t1")]
        hid = [EA.alloc([8, 512], BF16) for _ in range(2)]
        hidb = [Buf("hid0"), Buf("hid1")]
        rl = [EA.alloc([512], F32) for _ in range(2)]
        rlb = [Buf("rl0"), Buf("rl1")]
        SH2 = MOD[:, 3, :, :]
        G2 = MOD[:, 5, :, :]
        FT = [(i * 448, 448, [(i * 448, 448, True)]) for i in range(4)] + [(1792, 320, [(1792, 256, True), (NPR, NSA, False)])]
        NT_ = len(FT)
        hbs = [Buf("hall%d" % i) for i in range(NT_)]

        def nrm(ti, part, tm, tmb):
            t0_, n_, subs = FT[ti]
            for (st0, sn, sisp) in subs:
                norm_tile(st0, sn, sisp, GM2, SH2, hall[:, :, st0:st0 + sn], hbs[ti], tm, tmb, sq, sqb, rs, rsb, b_mod,
                          part=part, so=st0 - t0_)

        nrm(0, 0, tmps, tb)

        def H(q, ti, hd, hdb):
            t0, n, subs = FT[ti]
            s1 = wslot("F%d_1_%d" % (l, q))
            hb = hbs[ti]
            for f in range(8):
                b = palloc(1)
                S.mm([(ps[:, b, 0:n], ring[:, s1, k, f * 128:(f + 1) * 128], hall[:, k, t0:t0 + n]) for k in range(8)],
                     r=(slotb[s1], hb), w=(pb[b],))
                r_ = rl[f % 2]
                rb_ = rlb[f % 2]
                bcol = PF[:, R_B1 + l * 32 + q * 8 + f:R_B1 + l * 32 + q * 8 + f + 1]
                S.op(ACT, lambda: nc.scalar.activation(out=r_[:, 0:n], in_=ps[:, b, 0:n], func=AF.Relu, bias=bcol), r=(pb[b], b_const), w=(rb_,))
                S.op(DVE, lambda: nc.vector.tensor_tensor(out=hd[:, f, 0:n], in0=r_[:, 0:n], in1=r_[:, 0:n], op=ALU.mult), r=(rb_,), w=(hdb,))

        def Y(q, ti, hd, hdb):
            t0, n, subs = FT[ti]
            s2 = wslot("F%d_2_%d" % (l, q))
            for kk in range(8):
                b = palloc(1)
                S.mm([(ps[:, b, 0:n], ring[:, s2, f, kk * 128:(kk + 1) * 128], hd[:, f, 0:n]) for f in range(8)],
                     r=(slotb[s2], hdb), w=(pb[b],))
                for (st0, sn, sisp) in subs:
                    resid_update(b, kk, st0, sn, sisp, G2, GB2 if q == 0 else None, tmps[kk % 2], tb[kk % 2], b_mod, pcol=st0 - t0)
            if l == 0 and q == 2 and ti == 2:
                ada_chunk(1, 0)
                ada_chunk(1, 1)
            if l == 0 and q == 3:
                if ti == 1:
                    ada_chunk(1, 2)
                    ada_chunk(1, 3)
                elif ti == 3:
                    ada_chunk(1, 4)
                    ada_chunk(1, 5)
            if ti == NT_ - 1:
                wrelease("F%d_1_%d" % (l, q))
                wrelease("F%d_2_%d" % (l, q))

        for ti in range(NT_):
            if ti + 1 < NT_:
                nrm(ti + 1, 1, None, None)
            H(0, ti, hid[ti % 2], hidb[ti % 2])
            if ti + 1 < NT_:
                nrm(ti + 1, 2, rl, rlb)
            Y(0, ti, hid[ti % 2], hidb[ti % 2])
        steps = [(q, ti) for q in range(1, 4) for ti in range(NT_)]
        it = NT_
        H(steps[0][0], steps[0][1], hid[it % 2], hidb[it % 2])
        for si, (q, ti) in enumerate(steps):
            cur = it + si
            if si + 1 < len(steps):
                nq, nti = steps[si + 1]
                H(nq, nti, hid[(cur + 1) % 2], hidb[(cur + 1) % 2])
            Y(q, ti, hid[cur % 2], hidb[cur % 2])

    def gla_phase():
        S.barrier()
        WA.reset()
        MOD = MODS[1]
        b_mod = b_mods[1]
        GM1 = MOD[:, 1, :, :]
        sA, sB, sC, sD = wslot("L0"), wslot("L1"), wslot("L2"), wslot("LOUT")
        bk = Buf("gk")
        wg2 = WA.alloc([512], F32, parts=17)
        ngb = WA.alloc([256], F32)
        cmp_ = WA.alloc([128], F32)
        cms = WA.alloc([64], F32, parts=64)
        tNp = WA.alloc([128], F32)
        tNs = WA.alloc([64], F32, parts=64)
        km = WA.alloc([16], F32, parts=64)
        for (dst, src) in [(wg2, wg2a), (ngb, gng_bc), (cmp_, cm_p), (cms, cm_s), (tNp, triN_p), (tNs, triN_s), (km, kmask)]:
            S.dma(SP, dst, src)
        S.wait_dmas(with_x=True)
        Sf = WA.alloc([4, 256], F32)
        Sb = WA.alloc([4, 256], BF16)
        Sfb = Buf("Sf")
        Sbb = Buf("Sb")
        junk = WA.alloc([256], BF16)
        jb = Buf("junk")

        class Set:
            pass

        set_off = []
        sets = []
        for si in range(2):
            set_off.append(WA.off)
            B = Set()
            B.hT = WA.alloc([8, 128], BF16); B.hb = Buf("h")
            B.sq = WA.alloc([8, 128], BF16); B.sqb = Buf("sq")
            B.ogT = WA.alloc([8, 128], BF16); B.ogTb = Buf("ogT")
            B.rs = WA.alloc([128], F32); B.rsb = Buf("rs")
            B.tmps = [WA.alloc([128], F32) for _ in range(2)]; B.tb = [Buf("t0"), Buf("t1")]
            B.ntmps = [WA.alloc([128], F32) for _ in range(2)]; B.ntb = [Buf("nt0"), Buf("nt1")]
            B.qkT = WA.alloc([8, 128], F32); B.qkb = [Buf("q"), Buf("k")]; B.ksb = Buf("ks")
            B.aaug = WA.alloc([128], F32, parts=17); B.aab = Buf("aa")
            B.sp = WA.alloc([512], F32); B.spb = Buf("sp")
            B.eb = WA.alloc([4, 128], F32); B.ebb = Buf("eb")
            B.enb = B.sp.rearrange("p (h t) -> p h t", t=128)
            B.qs = WA.alloc([4, 128], BF16); B.ks = WA.alloc([4, 128], BF16); B.qsb = Buf("qs")
            B.khT = WA.alloc([4, 128], BF16); B.khTb = Buf("khT")
            B.kh = WA.alloc([512], BF16); B.khb = Buf("kh")
            B.vb = WA.alloc([D], BF16); B.vbb = Buf("vb")
            B.sg = WA.alloc([D], F32); B.sgb = Buf("sg")
            B.am = B.khT; B.amb = B.khTb
            B.ogh = B.sp.bitcast(BF16); B.oghb = B.spb
            B.st = WA.alloc([16], F32); B.stb = Buf("st")
            sets.append(B)
        set_end = WA.off
        print('GLA arena', WA.off, WA.nbytes)
        S.op(DVE, lambda: nc.vector.memset(Sf, 0.0), w=(Sfb,))
        S.op(DVE, lambda: nc.vector.memset(Sb, 0.0), w=(Sbb,))
        SH1 = MOD[:, 0, :, :]
        G1 = MOD[:, 2, :, :]
        SCALE = 128.0 ** -0.5
        GT = [(i * 128, 128, True) for i in range(NPR // 128)] + [(NPR, NSA, False)]
        def chunk_gen(ci, t0, n, isp):
            B = sets[ci % 2]
            nt = n
            hT = B.hT
            norm_tile(t0, n, isp, GM1, SH1, hT, B.hb, B.ntmps, B.ntb, B.sq, B.sqb, B.rs, B.rsb, b_mod)
            tN = tNp if isp else tNs
            cm = cmp_ if isp else cms
            yield 'F0'
            b = palloc(1)
            S.mm([(ps[0:17, b, 0:nt], wa[:, k, :], hT[:, k, 0:nt]) for k in range(8)], r=(bk, B.hb), w=(pb[b],))
            S.op(ACT, lambda: nc.scalar.copy(out=B.aaug[0:17, 0:nt], in_=ps[0:17, b, 0:nt]), r=(pb[b],), w=(B.aab,))
            S.op(DVE, lambda: nc.vector.memset(B.aaug[0:1, 0:nt], 1.0), w=(B.aab,))
            yield 'F1'
            bq = palloc(2)
            pstate["res"] |= {bq, bq + 1}

            def qk_groups(ms):
                for m in ms:
                    S.mm([(ps[:, bq + m // 4, (m % 4) * 128:(m % 4) * 128 + nt], ring[:, sA, k, m * 128:(m + 1) * 128], hT[:, k, 0:nt]) for k in range(8)],
                         r=(slotb[sA], B.hb), w=(pb[bq + m // 4],))

            def qk_copy(a_):
                S.op(ACT, lambda: nc.scalar.copy(out=B.qkT[:, 4 * a_:4 * a_ + 4, 0:nt],
                                                 in_=ps[:, bq + a_, :].rearrange("p (m t) -> p m t", t=128)[:, :, 0:nt]),
                     r=(pb[bq + a_],), w=(B.qkb[a_],))
            qk_groups([0, 1])
            yield 'F2'
            qk_groups([2, 3])
            qk_copy(0)
            yield 'F3'
            bx = palloc(1)
            S.mm([(ps[0:nt, bx, :], B.aaug[0:17, 0:nt], wg2[0:17, :])], r=(B.aab, bk), w=(pb[bx],))
            S.op(ACT, lambda: nc.scalar.activation(out=B.sp[0:nt, :], in_=ps[0:nt, bx, :], func=AF.Exp, scale=-1.0), r=(pb[bx],), w=(B.spb,))
            S.op(ACT, lambda: nc.scalar.activation(out=B.sp[0:nt, :], in_=B.sp[0:nt, :], func=AF.Ln, bias=1.0, scale=1.0), r=(B.spb,), w=(B.spb,))
            yield 'F4'
            qk_groups([4, 5])
            yield 'F5'
            qk_groups([6, 7])
            qk_copy(1)
            pstate["res"] -= {bq, bq + 1}
            yield 'F6'
            bb = palloc(1)
            S.mm_multi([[(ps[:, bb, hd * 128:hd * 128 + nt], B.sp[0:nt, hd * 128:(hd + 1) * 128], tN[0:nt, 0:nt])] for hd in range(4)],
                       r=(B.spb, bk), w=(pb[bb],))
            bT = ps[:, bb, :].rearrange("p (h t) -> p h t", t=128)[:, :, 0:nt]
            eb = B.eb
            enb = B.enb
            S.op(ACT, lambda: nc.scalar.activation(out=eb[:, :, 0:nt], in_=bT, func=AF.Exp), r=(pb[bb],), w=(B.ebb,))
            S.op(ACT, lambda: nc.scalar.activation(out=enb[:, :, 0:nt], in_=bT, func=AF.Exp, scale=-1.0), r=(pb[bb],), w=(B.spb,))
            S.op(DVE, lambda: nc.vector.scalar_tensor_tensor(out=B.qs[:, :, 0:nt], in0=B.qkT[:, 0:4, 0:nt], scalar=SCALE, in1=eb[:, :, 0:nt],
                                                             op0=ALU.mult, op1=ALU.mult), r=(B.qkb[0], B.ebb), w=(B.qsb,))
            S.op(DVE, lambda: nc.vector.tensor_tensor(out=B.ks[:, :, 0:nt], in0=B.qkT[:, 4:8, 0:nt], in1=enb[:, :, 0:nt], op=ALU.mult),
                 r=(B.qkb[1], B.spb), w=(B.ksb,))
            if isp:
                for hd in range(4):
                    S.op(DVE, lambda: nc.vector.scalar_tensor_tensor(out=B.khT[:, hd, 0:nt], in0=B.qkT[:, 4 + hd, 0:nt], scalar=eb[:, hd, nt - 1:nt],
                                                                     in1=enb[:, hd, 0:nt], op0=ALU.mult, op1=ALU.mult),
                         r=(B.qkb[1], B.ebb, B.spb), w=(B.khTb,))
            else:
                tmpk = B.ntmps[0]
                for hd in range(4):
                    el = eb[:, hd, 0:nt].rearrange("p (b t) -> p b t", t=4)[:, :, 3:4].to_broadcast([128, 16, 4])
                    S.op(DVE, lambda: nc.vector.tensor_tensor(out=v3(tmpk[:, 0:nt], False), in0=v3(B.qkT[:, 4 + hd, 0:nt], False), in1=el, op=ALU.mult),
                         r=(B.qkb[1], B.ebb), w=(B.ntb[0],))
                    S.op(DVE, lambda: nc.vector.tensor_tensor(out=B.khT[:, hd, 0:nt], in0=tmpk[:, 0:nt], in1=enb[:, hd, 0:nt], op=ALU.mult),
                         r=(B.ntb[0], B.spb), w=(B.khTb,))
            yield 'F7'
            bv = palloc(2)
            pstate["res"] |= {bv, bv + 1}
            S.mm([(ps[0:nt, bv, :], hT[:, k, 0:nt], ring[:, sB, k, 0:512]) for k in range(8)], r=(slotb[sB], B.hb), w=(pb[bv],))
            yield 'F8'
            S.mm([(ps[0:nt, bv + 1, :], hT[:, k, 0:nt], ring[:, sB, k, 512:1024]) for k in range(8)], r=(slotb[sB], B.hb), w=(pb[bv + 1],))
            S.op(ACT, lambda: nc.scalar.copy(out=B.vb[0:nt, :], in_=ps[0:nt, bv:bv + 2, :].rearrange("p a c -> p (a c)")),
                 r=(pb[bv], pb[bv + 1]), w=(B.vbb,))
            pstate["res"] -= {bv, bv + 1}
            yield 'F9'
            bg = palloc(2)
            pstate["res"] |= {bg, bg + 1}
            S.mm([(ps[0:nt, bg, :], hT[:, k, 0:nt], ring[:, sC, k, 0:512]) for k in range(8)], r=(slotb[sC], B.hb), w=(pb[bg],))
            yield 'F10'
            S.mm([(ps[0:nt, bg + 1, :], hT[:, k, 0:nt], ring[:, sC, k, 512:1024]) for k in range(8)], r=(slotb[sC], B.hb), w=(pb[bg + 1],))
            gps = ps[0:nt, bg:bg + 2, :].rearrange("p a c -> p (a c)")
            S.op(ACT, lambda: nc.scalar.activation(out=B.sg[0:nt, :], in_=gps, func=AF.Exp, scale=-1.0), r=(pb[bg], pb[bg + 1]), w=(B.sgb,))
            S.op(ACT, lambda: nc.scalar.activation(out=B.sg[0:nt, :], in_=B.sg[0:nt, :], func=AF.Ln, bias=1.0, scale=1.0), r=(B.sgb,), w=(B.sgb,))
            S.op(ACT, lambda: nc.scalar.activation(out=B.sg[0:nt, :], in_=B.sg[0:nt, :], func=AF.Exp, scale=-1.0), r=(B.sgb,), w=(B.sgb,))
            S.op(DVE, lambda: nc.vector.tensor_tensor(out=B.sg[0:nt, :], in0=gps, in1=B.sg[0:nt, :], op=ALU.mult), r=(pb[bg], pb[bg + 1], B.sgb), w=(B.sgb,))
            sg4 = B.sg[0:nt, :].rearrange("p (h v) -> p h v", v=256)
            S.op(DVE, lambda: nc.vector.tensor_tensor(out=sg4, in0=sg4, in1=ngb[0:nt, :].unsqueeze(1).to_broadcast([nt, 4, 256]), op=ALU.mult),
                 r=(B.sgb, bk), w=(B.sgb,))
            pstate["res"] -= {bg, bg + 1}
            yield 'F11'
            bkt = palloc(1)
            pkt = ps[:, bkt, :].bitcast(BF16)
            S.transposes([(pkt[0:nt, hd * 128:(hd + 1) * 128], B.khT[:, hd, 0:nt], identb) for hd in range(4)], r=(B.khTb, b_const), w=(pb[bkt],))
            S.op(ACT, lambda: nc.scalar.copy(out=B.kh[0:nt, :], in_=pkt[0:nt, 0:512]), r=(pb[bkt],), w=(B.khb,))
            yield 'B0'
            ba = palloc(1)
            S.mm_multi([[(ps[0:nt, ba, hd * 128:hd * 128 + nt], B.ks[:, hd, 0:nt], B.qs[:, hd, 0:nt])] for hd in range(4)],
                       r=(B.qsb, B.ksb), w=(pb[ba],))
            aT = ps[0:nt, ba, :].rearrange("p (h t) -> p h t", t=128)[:, :, 0:nt]
            S.op(DVE, lambda: nc.vector.tensor_tensor(out=B.am[0:nt, :, 0:nt], in0=aT, in1=cm[0:nt, 0:nt].unsqueeze(1).to_broadcast([nt, 4, nt]),
                                                      op=ALU.mult), r=(pb[ba], bk), w=(B.amb,))
            yield 'B1'
            def o_post(obanks, ocols):
                st = B.st
                for hd in range(4):
                    o = ps[0:nt, obanks[hd], ocols[hd]:ocols[hd] + 256]
                    S.op(ACT, lambda: nc.scalar.activation(out=junk[0:nt, :], in_=o, func=AF.Square, accum_out=st[0:nt, hd:hd + 1]),
                         r=(pb[obanks[hd]],), w=(jb, B.stb))
                S.op(ACT, lambda: nc.scalar.activation(out=st[0:nt, 4:8], in_=st[0:nt, 0:4], func=AF.Ln, bias=EPS, scale=1.0 / 256.0), r=(B.stb,), w=(B.stb,))
                S.op(ACT, lambda: nc.scalar.activation(out=st[0:nt, 4:8], in_=st[0:nt, 4:8], func=AF.Exp, scale=-0.5), r=(B.stb,), w=(B.stb,))
                for hd in range(4):
                    o = ps[0:nt, obanks[hd], ocols[hd]:ocols[hd] + 256]
                    S.op(DVE, lambda: nc.vector.scalar_tensor_tensor(out=B.ogh[0:nt, hd * 256:(hd + 1) * 256], in0=o, scalar=st[0:nt, 4 + hd:5 + hd],
                                                                     in1=B.sg[0:nt, hd * 256:(hd + 1) * 256], op0=ALU.mult, op1=ALU.mult),
                         r=(pb[obanks[hd]], B.stb, B.sgb), w=(B.oghb,))
                pstate["res"] -= set(obanks)
            if isp:
                bo = palloc(2)
                pstate["res"] |= {bo, bo + 1}
                obanks = [bo, bo, bo + 1, bo + 1]
                ocols = [0, 256, 0, 256]
                for hd in range(4):
                    o = ps[0:nt, obanks[hd], ocols[hd]:ocols[hd] + 256]
                    S.mm([(o, B.qs[:, hd, 0:nt], Sb[:, hd, :]), (o, B.am[0:nt, hd, 0:nt], B.vb[0:nt, hd * 256:(hd + 1) * 256])],
                         r=(B.qsb, Sbb, B.amb, B.vbb), w=(pb[obanks[hd]],))
                o_post(obanks, ocols)
                yield 'B2'
                bs_ = palloc(2)
                S.mm_multi([[(ps[:, bs_ + hd // 2, (hd % 2) * 256:(hd % 2) * 256 + 256], B.kh[0:nt, hd * 128:(hd + 1) * 128],
                              B.vb[0:nt, hd * 256:(hd + 1) * 256])] for hd in range(4)], r=(B.khb, B.vbb), w=(pb[bs_], pb[bs_ + 1]))
                for hd in range(4):
                    S.op(DVE, lambda: nc.vector.scalar_tensor_tensor(out=Sf[:, hd, :], in0=Sf[:, hd, :], scalar=eb[:, hd, nt - 1:nt],
                                                                     in1=ps[:, bs_ + hd // 2, (hd % 2) * 256:(hd % 2) * 256 + 256],
                                                                     op0=ALU.mult, op1=ALU.add), r=(Sfb, B.ebb, pb[bs_ + hd // 2]), w=(Sfb,))
                S.op(ACT, lambda: nc.scalar.copy(out=Sb, in_=Sf), r=(Sfb,), w=(Sbb,))
                if t0 + nt == NPR:
                    S.dma(SP, st_prompt, Sf, r=(Sfb,))
            else:
                S.barrier(engs=[SP, DVE])
                other = 1 - (ci % 2)
                save_off = WA.off
                WA.off = set_off[other]
                NSL = 2
                s0 = [WA.alloc([4, 256], F32) for _ in range(NSL)]
                s0b = [Buf("s0%d" % i) for i in range(NSL)]
                sn = [WA.alloc([4, 256], F32) for _ in range(2)]
                snb = [Buf("sn0"), Buf("sn1")]
                s0h = [WA.alloc([4, 256], BF16) for _ in range(NSL)]
                s0hb = [Buf("s0h%d" % i) for i in range(NSL)]
                Qb = [WA.alloc([4, 64], BF16) for _ in range(NSL)]
                Qbb = [Buf("Qb%d" % i) for i in range(NSL)]
                Kb = [WA.alloc([512], BF16, parts=64) for _ in range(NSL)]
                Kbb = [Buf("Kb%d" % i) for i in range(NSL)]
                lim = set_end if other == 1 else set_off[1]
                assert WA.off <= lim, (WA.off, lim)
                WA.off = save_off
                ob4 = [0, 1, 2, 3]
                pstate["res"] = set(ob4)
                obanks = ob4
                ocols = [0, 0, 0, 0]
                for i in range(NSL):
                    S.op(DVE, lambda: nc.vector.memset(Qb[i], 0.0), w=(Qbb[i],))
                    S.dma(SP, s0[i], sgl[i], w=(s0b[i],))
                def prep(bi):
                    sl = bi % NSL
                    if bi >= NSL:
                        pv_ = bi - NSL
                        S.op(DVE, lambda: nc.vector.memset(Qb[sl][:, :, 4 * pv_:4 * pv_ + 4], 0.0), w=(Qbb[sl],))
                    S.op(DVE, lambda: nc.vector.tensor_copy(out=Qb[sl][:, :, 4 * bi:4 * bi + 4], in_=B.qs[:, :, 4 * bi:4 * bi + 4]),
                         r=(B.qsb,), w=(Qbb[sl],))
                    S.op(DVE, lambda: nc.vector.tensor_scalar(out=Kb[sl], in0=B.kh[0:64, :], scalar1=km[:, bi:bi + 1], scalar2=None, op0=ALU.mult),
                         r=(B.khb,), w=(Kbb[sl],))

                prep(0)
                for bi in range(16):
                    sl = bi % NSL
                    S.op(ACT, lambda: nc.scalar.copy(out=s0h[sl], in_=s0[sl]), r=(s0b[sl],), w=(s0hb[sl],))
                    for hd in range(4):
                        o = ps[0:64, ob4[hd], 0:256]
                        S._waits(PE, (Qbb[sl], s0hb[sl]), (pb[ob4[hd]],))
                        ins = nc.tensor.matmul(o, lhsT=Qb[sl][:, hd, :], rhs=s0h[sl][:, hd, :], start=(bi == 0), stop=False)
                        PE.cnt += 1
                        ins.then_inc(PE.sem, 1)
                        s0hb[sl].r[PE] = PE.cnt
                        Qbb[sl].r[PE] = PE.cnt
                        pb[ob4[hd]].w = (PE, PE.cnt)
                        pb[ob4[hd]].r = {}
                    bs_ = palloc(2)
                    S.mm_multi([[(ps[:, bs_ + hd // 2, (hd % 2) * 256:(hd % 2) * 256 + 256], Kb[sl][:, hd * 128:(hd + 1) * 128],
                                  B.vb[0:64, hd * 256:(hd + 1) * 256])] for hd in range(4)], r=(Kbb[sl], B.vbb), w=(pb[bs_], pb[bs_ + 1]))
                    if bi + 1 < 16:
                        prep(bi + 1)
                    for hd in range(4):
                        S.op(DVE, lambda: nc.vector.scalar_tensor_tensor(out=sn[bi % 2][:, hd, :], in0=s0[sl][:, hd, :], scalar=eb[:, hd, 4 * bi + 3:4 * bi + 4],
                                                                         in1=ps[:, bs_ + hd // 2, (hd % 2) * 256:(hd % 2) * 256 + 256],
                                                                         op0=ALU.mult, op1=ALU.add), r=(s0b[sl], B.ebb, pb[bs_ + hd // 2]), w=(snb[bi % 2],))
                    S.dma(SP, st_sample[bi], sn[bi % 2], r=(snb[bi % 2],))
                    if bi + NSL < 16:
                        S.dma(SP, s0[sl], sgl[bi + NSL], w=(s0b[sl],))
                for hd in range(4):
                    o = ps[0:64, ob4[hd], 0:256]
                    S._waits(PE, (B.amb, B.vbb), (pb[ob4[hd]],))
                    ins = nc.tensor.matmul(o, lhsT=B.am[0:64, hd, 0:64], rhs=B.vb[0:64, hd * 256:(hd + 1) * 256], start=False, stop=True)
                    PE.cnt += 1
                    ins.then_inc(PE.sem, 1)
                    B.amb.r[PE] = PE.cnt
                    B.vbb.r[PE] = PE.cnt
                    pb[ob4[hd]].w = (PE, PE.cnt)
                    pb[ob4[hd]].r = {}
                o_post(obanks, ocols)
                yield 'B2'
            yield 'B3'
            bt = palloc(1)
            ptb = ps[:, bt, :].bitcast(BF16)
            S.transposes([(ptb[:, m * 128:m * 128 + nt], B.ogh[0:nt, m * 128:(m + 1) * 128], identb[0:nt, 0:nt]) for m in range(8)],
                         r=(B.oghb, b_const), w=(pb[bt],))
            ogT = B.ogT
            S.op(ACT, lambda: nc.scalar.copy(out=ogT[:, :, 0:nt], in_=ptb.rearrange("p (m t) -> p m t", t=128)[:, :, 0:nt]), r=(pb[bt],), w=(B.ogTb,))
            yield 'B4'
            for kk in range(8):
                if kk % 2 == 0:
                    bo2 = palloc(1)
                S.mm([(ps[:, bo2, (kk % 2) * 128:(kk % 2) * 128 + nt], ring[:, sD, m, kk * 128:(kk + 1) * 128], ogT[:, m, 0:nt]) for m in range(8)],
                     r=(slotb[sD], B.ogTb), w=(pb[bo2],))
                if kk % 2 == 1:
                    for k2 in (kk - 1, kk):
                        resid_update(bo2, k2, t0, n, isp, G1, None, B.tmps[k2 % 2], B.tb[k2 % 2], b_mod, pcol=(k2 % 2) * 128)
                    yield 'B%d' % (5 + kk // 2)

        gens = [chunk_gen(ci, t0, n, isp) for ci, (t0, n, isp) in enumerate(GT)]
        NG = len(gens)

        def step(c, want):
            if 0 <= c < NG:
                got = next(gens[c])
                assert got == want, (c, got, want)

        step(0, 'F0')
        for c in range(NG + 1):
            for i in range(1, 12):
                step(c, 'F%d' % i)
                if i == 7:
                    step(c + 1, 'F0')
                if i - 1 <= 8:
                    step(c - 1, 'B%d' % (i - 1))
        for cid in ("L0", "L1", "L2", "LOUT"):
            wrelease(cid)

    def final_phase():
        S.barrier()
        CA.reset()
        EA.reset()
        fng = CA.alloc([D], F32)
        bk = Buf("fk")
        S.dma(SP, fng, fng_bc, w=(bk,))
        yo = [EA.alloc([D], F32) for _ in range(2)]
        yob = [Buf("y0"), Buf("y1")]
        junk = EA.alloc([D], BF16)
        jb = Buf("junk")
        st = EA.alloc([4, 2], F32)
        stb = [Buf("st0"), Buf("st1")]
        for ti in range(17):
            t0 = ti * 128
            n = 128 if ti < 16 else 64
            sl = ti % 2
            b = palloc(2)
            S.transposes([(ps[0:n, b + k // 4, (k % 4) * 128:(k % 4 + 1) * 128], xT[:, k, t0:t0 + n], ident) for k in range(8)],
                         r=xb(t0, n) + (b_const,), w=(pb[b], pb[b + 1]))
            pin = ps[0:n, b:b + 2, :].rearrange("p a c -> p (a c)")
            S.op(ACT, lambda: nc.scalar.activation(out=junk[0:n, :], in_=pin, func=AF.Square, accum_out=st[0:n, sl, 0:1]),
                 r=(pb[b], pb[b + 1]), w=(jb, stb[sl]))
            S.op(ACT, lambda: nc.scalar.activation(out=st[0:n, sl, 1:2], in_=st[0:n, sl, 0:1], func=AF.Ln, bias=EPS, scale=1.0 / D), r=(stb[sl],), w=(stb[sl],))
            S.op(ACT, lambda: nc.scalar.activation(out=st[0:n, sl, 1:2], in_=st[0:n, sl, 1:2], func=AF.Exp, scale=-0.5), r=(stb[sl],), w=(stb[sl],))
            S.op(DVE, lambda: nc.vector.scalar_tensor_tensor(out=yo[sl][0:n, :], in0=pin, scalar=st[0:n, sl, 1:2], in1=fng[0:n, :],
                                                             op0=ALU.mult, op1=ALU.mult), r=(pb[b], pb[b + 1], stb[sl], bk), w=(yob[sl],))
            if ti < 16:
                S.dma(SP, y_prompt[t0:t0 + n, :], yo[sl][0:n, :], r=(yob[sl],))
            else:
                S.dma(SP, y_sample, yo[sl][0:n, :], r=(yob[sl],))

    S.barrier()
    ada_chunk(0, 0)
    ada_chunk(0, 1)
    gmlp_phase()
    ffn_phase(0)
    gla_phase()
    ffn_phase(1)
    final_phase()
    for d in S.dsems:
        if d.cnt > 0:
            nc.sync.wait_ge(d.sem, d.cnt)
    for e in (S.PE, S.ACT, S.DVE):
        nc.sync.wait_ge(e.sem, e.cnt)
    return nc


def _prep_shared(inp):
    f = np.float32
    g = lambda k: np.ascontiguousarray(np.asarray(inp[k], dtype=f))
    sh = {}
    for k in ("ada_w", "ffn_w1", "ffn_w2", "gmlp_w_in", "gmlp_w_out", "gla_w_in", "gla_w_out"):
        sh[k] = g(k)
    sh["gla_wa"] = np.ascontiguousarray(np.concatenate([np.zeros((D, 1), f), sh["gla_w_in"][:, 3072:3088]], axis=1))
    rows = [g("ada_b").reshape(96, 128), g("norm_mix_g").reshape(16, 128), g("norm_ffn_g").reshape(16, 128),
            g("ffn_b1").reshape(64, 128), g("ffn_b2").reshape(16, 128), g("gmlp_b_in")[:D].reshape(8, 128),
            g("gmlp_b_out").reshape(8, 128)]
    sh["prow"] = np.ascontiguousarray(np.concatenate(rows, axis=0))
    rep = lambda v: np.ascontiguousarray(np.broadcast_to(v[None, :], (128, v.shape[0])))
    sh["binv_bc"] = rep(g("gmlp_b_in")[D:])
    sh["lng_bc"] = rep(g("gmlp_ln_g"))
    sh["lnb_bc"] = rep(g("gmlp_ln_b"))
    sh["fng_bc"] = rep(g("final_norm_g"))
    sh["gng_bc"] = rep(g("gla_norm_g"))
    ws = g("gmlp_w_s")
    sh["wsT_p"] = np.ascontiguousarray(ws.transpose(2, 0, 1))
    idx = np.arange(64) % 4
    sh["wsT_s"] = np.ascontiguousarray(ws[:, idx[None, :], idx[:, None]].transpose(1, 0, 2))
    bs = g("gmlp_b_s")
    sh["bs_p"] = np.ascontiguousarray(np.broadcast_to(bs[None], (128, 4, 128)))
    sh["bs_s"] = np.ascontiguousarray(np.broadcast_to(bs[None][:, :, idx], (128, 4, 64)))
    j = np.arange(128)
    sh["cm_p"] = (j[:, None] <= j[None, :]).astype(f)
    j6 = np.arange(64)
    same = (j6[:, None] // 4) == (j6[None, :] // 4)
    sh["cm_s"] = ((j6[:, None] <= j6[None, :]) & same).astype(f)
    sh["triN_p"] = (sh["cm_p"] * (-1.0 / 16.0)).astype(f)
    sh["triR_p"] = ((j[:, None] > j[None, :]).astype(f) * (-1.0 / 16.0)).astype(f)
    sh["triN_s"] = (sh["cm_s"] * (-1.0 / 16.0)).astype(f)
    sh["triR_s"] = (((j6[:, None] > j6[None, :]) & same).astype(f) * (-1.0 / 16.0)).astype(f)
    qm = np.zeros((128, 16, 64), f)
    for b in range(16):
        qm[:, b, 4 * b:4 * b + 4] = 1.0
    sh["qmask"] = qm
    km = np.zeros((64, 16), f)
    for b in range(16):
        km[4 * b:4 * b + 4, b] = 1.0
    sh["kmask"] = km
    sh["wg2a"] = np.ascontiguousarray(np.concatenate([g("gla_b_gate")[None, :], g("gla_w_gate2")], axis=0))
    sh["ident"] = np.eye(128, dtype=f)
    return sh


_NC_CACHE = {}


def kernel(**inp):
    f = np.float32
    sh = _prep_shared(inp)
    xp = np.asarray(inp["x_prompt"], f)
    xs = np.asarray(inp["x_sample"], f)
    cp = np.asarray(inp["c_prompt"], f)
    cs = np.asarray(inp["c_sample"], f)
    sg = np.asarray(inp["state_gla"], f)
    in_maps = []
    for c in range(8):
        m = dict(sh)
        m["xin"] = np.ascontiguousarray(np.concatenate([xp[c], xs[16 * c:16 * c + 16].reshape(64, D)], axis=0))
        m["cin"] = np.ascontiguousarray(np.concatenate([cp[c:c + 1], cs[16 * c:16 * c + 16]], axis=0))
        m["sgl"] = np.ascontiguousarray(sg[16 * c:16 * c + 16].transpose(0, 2, 1, 3))
        in_maps.append(m)
    if "nc" not in _NC_CACHE:
        _NC_CACHE["nc"] = build()
    nc = _NC_CACHE["nc"]
    res = run_bass_kernel_spmd(nc, in_maps, core_ids=list(range(8)))
    R = res.results
    y_prompt = np.stack([R[c]["y_prompt"] for c in range(8)], axis=0).astype(f)
    y_sample = np.concatenate([R[c]["y_sample"].reshape(16, 4, D) for c in range(8)], axis=0).astype(f)
    st_p = np.stack([np.asarray(R[c]["st_prompt"]).transpose(1, 0, 2) for c in range(8)], axis=0).astype(f)
    st_s = np.concatenate([np.asarray(R[c]["st_sample"]).transpose(0, 2, 1, 3) for c in range(8)], axis=0).astype(f)
    v_s = np.concatenate([R[c]["v_sample"].reshape(16, 4, D) for c in range(8)], axis=0).astype(f)
    return (y_prompt, y_sample, st_p, st_s, v_s)
```

```python
import numpy as np
import concourse.bass as bass
import concourse.mybir as mybir
from concourse.bass_utils import run_bass_kernel_spmd

F32 = mybir.dt.float32
BF16 = mybir.dt.bfloat16
AF = mybir.ActivationFunctionType
ALU = mybir.AluOpType

D = 1024
NPR = 2048
NSA = 64
NTOK = NPR + NSA
NB = 17
EPS = 1e-6
TILES = [(0, 512, True), (512, 512, True), (1024, 512, True), (1536, 512, True), (2048, 64, False)]
NSLOT = 4
SAME_ENG_WAR = False

R_ADAB, R_NMG, R_NFG, R_B1, R_B2, R_BINU, R_BOUT, R_TOT = 0, 96, 112, 128, 192, 208, 216, 224


class Eng:
    def __init__(self, nc, name, h):
        self.name = name
        self.h = h
        self.sem = nc.alloc_semaphore("s_" + name)
        self.cnt = 0
        self.seen = {}
        self.raw_self = name in ("act", "dve", "pool")


class Buf:
    __slots__ = ("name", "w", "r")

    def __init__(self, name=""):
        self.name = name
        self.w = None
        self.r = {}


class Sched:
    def __init__(self, nc):
        self.nc = nc
        self.PE = Eng(nc, "pe", nc.tensor)
        self.ACT = Eng(nc, "act", nc.scalar)
        self.DVE = Eng(nc, "dve", nc.vector)
        self.SP = Eng(nc, "sp", nc.sync)
        self.POOL = Eng(nc, "pool", nc.gpsimd)
        self.engs = [self.PE, self.ACT, self.DVE, self.SP, self.POOL]
        self.dsems = [Eng(nc, "d%d" % i, None) for i in range(12)]
        self.wsems = [Eng(nc, "w%d" % i, None) for i in range(NSLOT)]
        self.xsem = Eng(nc, "wx", None)
        self.dnext = 0

    def _waits(self, eng, r, w):
        need = {}
        for b in r:
            if b.w is not None and (b.w[0] is not eng or eng.raw_self):
                need[b.w[0]] = max(need.get(b.w[0], 0), b.w[1])
        full = eng.raw_self and SAME_ENG_WAR
        for b in w:
            if b.w is not None and (b.w[0] is not eng or full):
                need[b.w[0]] = max(need.get(b.w[0], 0), b.w[1])
            for e, c in b.r.items():
                if e is not eng or full:
                    need[e] = max(need.get(e, 0), c)
        for e, c in need.items():
            if eng.seen.get(e, 0) < c:
                eng.h.wait_ge(e.sem, c)
                eng.seen[e] = c

    def op(self, eng, fn, r=(), w=()):
        self._waits(eng, r, w)
        ins = fn()
        eng.cnt += 1
        ins.then_inc(eng.sem, 1)
        for b in w:
            b.w = (eng, eng.cnt)
            b.r = {}
        for b in r:
            b.r[eng] = eng.cnt

    def mm(self, mms, r=(), w=()):
        eng = self.PE
        self._waits(eng, r, w)
        n = len(mms)
        ins = None
        for i, (o, l, rh) in enumerate(mms):
            ins = self.nc.tensor.matmul(o, lhsT=l, rhs=rh, start=(i == 0), stop=(i == n - 1))
        eng.cnt += 1
        ins.then_inc(eng.sem, 1)
        for b in w:
            b.w = (eng, eng.cnt)
            b.r = {}
        for b in r:
            b.r[eng] = eng.cnt

    def mm_multi(self, groups, r=(), w=()):
        eng = self.PE
        self._waits(eng, r, w)
        ins = None
        for mms in groups:
            n = len(mms)
            for i, (o, l, rh) in enumerate(mms):
                ins = self.nc.tensor.matmul(o, lhsT=l, rhs=rh, start=(i == 0), stop=(i == n - 1))
        eng.cnt += 1
        ins.then_inc(eng.sem, 1)
        for b in w:
            b.w = (eng, eng.cnt)
            b.r = {}
        for b in r:
            b.r[eng] = eng.cnt

    def transposes(self, items, r=(), w=()):
        eng = self.PE
        self._waits(eng, r, w)
        ins = None
        for (o, i_, ident) in items:
            ins = self.nc.tensor.transpose(o, i_, ident)
        eng.cnt += 1
        ins.then_inc(eng.sem, 1)
        for b in w:
            b.w = (eng, eng.cnt)
            b.r = {}
        for b in r:
            b.r[eng] = eng.cnt

    def dma(self, q, out, in_, r=(), w=(), sem=None):
        if sem is None:
            sem = self.dsems[self.dnext % len(self.dsems)]
            self.dnext += 1
        self._waits(q, r, w)
        ins = q.h.dma_start(out=out, in_=in_)
        sem.cnt += 16
        ins.then_inc(sem.sem, 16)
        for b in w:
            b.w = (sem, sem.cnt)
            b.r = {}
        for b in r:
            b.r[sem] = sem.cnt

    def wait_dmas(self, engs=None, with_x=False):
        engs = engs or [self.PE, self.ACT, self.DVE]
        for e in engs:
            for o in self.dsems + ([self.xsem] if with_x else []):
                if e.seen.get(o, 0) < o.cnt:
                    e.h.wait_ge(o.sem, o.cnt)
                    e.seen[o] = o.cnt

    def barrier(self, engs=None):
        engs = engs or [self.PE, self.ACT, self.DVE, self.SP]
        allp = [self.PE, self.ACT, self.DVE, self.SP] + self.dsems + [self.xsem]
        for e in engs:
            for o in allp:
                if o is e:
                    continue
                if e.seen.get(o, 0) < o.cnt:
                    e.h.wait_ge(o.sem, o.cnt)
                    e.seen[o] = o.cnt


class Arena:
    def __init__(self, nc, name, nbytes):
        self.t = nc.alloc_sbuf_tensor(name, [128, nbytes // 4], F32)
        self.nbytes = nbytes
        self.off = 0

    def reset(self):
        self.off = 0

    def alloc(self, shape, dtype, parts=128):
        esz = 4 if dtype == F32 else 2
        n = 1
        for s in shape:
            n *= s
        nb = (n * esz + 31) // 32 * 32
        assert self.off + nb <= self.nbytes, (self.off, nb, self.nbytes)
        o4 = self.off // 4
        ap = self.t[0:parts, o4:o4 + nb // 4]
        if dtype != F32:
            ap = ap.bitcast(dtype)
        ap = ap[:, 0:n]
        if len(shape) == 2:
            ap = ap.rearrange("p (a b) -> p a b", b=shape[1])
        elif len(shape) == 3:
            ap = ap.rearrange("p (a b c) -> p a b c", b=shape[1], c=shape[2])
        self.off += nb
        return ap


def build():
    nc = bass.Bass("TRN2", target_bir_lowering=False)
    S = Sched(nc)
    PE, ACT, DVE, SP, POOL = S.PE, S.ACT, S.DVE, S.SP, S.POOL

    def din(name, shape, dt=F32):
        return nc.dram_tensor(name, list(shape), dt, kind="ExternalInput").ap()

    def dout(name, shape):
        return nc.dram_tensor(name, list(shape), F32, kind="ExternalOutput").ap()

    xin = din("xin", [NTOK, D])
    cin = din("cin", [NB, D])
    sgl = din("sgl", [16, 128, 4, 256])
    ada_w = din("ada_w", [2, D, 6 * D])
    ffn_w1 = din("ffn_w1", [2, D, 4 * D])
    ffn_w2 = din("ffn_w2", [2, 4 * D, D])
    gmlp_w_in = din("gmlp_w_in", [D, 2 * D])
    gmlp_w_out = din("gmlp_w_out", [D, D])
    gla_w_in = din("gla_w_in", [D, 3088])
    gla_wa = din("gla_wa", [D, 17])
    gla_w_out = din("gla_w_out", [D, D])
    prow = din("prow", [R_TOT, 128])
    binv_bc = din("binv_bc", [128, D])
    lng_bc = din("lng_bc", [128, D])
    lnb_bc = din("lnb_bc", [128, D])
    fng_bc = din("fng_bc", [128, D])
    gng_bc = din("gng_bc", [128, 256])
    wsT_p = din("wsT_p", [128, 4, 128])
    wsT_s = din("wsT_s", [64, 4, 64])
    bs_p = din("bs_p", [128, 4, 128])
    bs_s = din("bs_s", [128, 4, 64])
    cm_p = din("cm_p", [128, 128])
    cm_s = din("cm_s", [64, 64])
    triN_p = din("triN_p", [128, 128])
    triR_p = din("triR_p", [128, 128])
    triN_s = din("triN_s", [64, 64])
    triR_s = din("triR_s", [64, 64])
    qmask = din("qmask", [128, 16, 64])
    kmask = din("kmask", [64, 16])
    wg2a = din("wg2a", [17, 512])
    ident_d = din("ident", [128, 128])

    y_prompt = dout("y_prompt", [NPR, D])
    y_sample = dout("y_sample", [NSA, D])
    st_prompt = dout("st_prompt", [128, 4, 256])
    st_sample = dout("st_sample", [16, 128, 4, 256])
    v_sample = dout("v_sample", [NSA, D])

    xT = nc.alloc_sbuf_tensor("xT", [128, 8, NTOK], F32)
    ring = nc.alloc_sbuf_tensor("ring", [128, NSLOT, 8, 1024], BF16)
    KA = Arena(nc, "KA", 10752)
    rem = nc.sbuf_bytes_remaining - 64
    WA = Arena(nc, "WA", (rem // 32) * 32)
    CA = WA
    EA = WA
    ps = nc.alloc_psum_tensor("ps", [128, 8, 512], F32)
    pb = [Buf("ps%d" % i) for i in range(8)]
    pstate = {"n": 0, "res": set()}

    def palloc(n=1):
        while True:
            if n == 2 and pstate["n"] % 2 == 1:
                pstate["n"] += 1
            b = pstate["n"] % 8
            pstate["n"] += n
            if all(((b + i) % 8) not in pstate["res"] for i in range(n)):
                return b

    ident = KA.alloc([128], F32)
    identb = KA.alloc([128], BF16)
    onesb = KA.alloc([128], BF16)
    PF = KA.alloc([R_TOT], F32)
    MODS = [KA.alloc([6, 8, NB], F32) for _ in range(2)]
    GB1 = KA.alloc([8, NB], F32)
    GB2S = [KA.alloc([8, NB], F32) for _ in range(2)]
    scT = KA.alloc([8, NB], BF16)
    wa = KA.alloc([8, 17], BF16)
    xTbs = [Buf("xT%d" % i) for i in range(17)]

    def xb(t0, n):
        return tuple(xTbs[i] for i in range(t0 // 128, (t0 + n + 127) // 128))
    b_const = Buf("const")
    b_mods = [Buf("mod0"), Buf("mod1")]

    def wview(ap2d):
        return ap2d.rearrange("(k p) n -> p k n", p=128)

    cdef = {}
    for l in range(2):
        for j in range(6):
            cdef["A%d_%d" % (l, j)] = wview(ada_w[l, :, j * 1024:(j + 1) * 1024])
        for q in range(4):
            cdef["F%d_1_%d" % (l, q)] = wview(ffn_w1[l, :, q * 1024:(q + 1) * 1024])
            cdef["F%d_2_%d" % (l, q)] = wview(ffn_w2[l, q * 1024:(q + 1) * 1024, :])
    cdef["GIN0"] = wview(gmlp_w_in[:, 0:1024])
    cdef["GIN1"] = wview(gmlp_w_in[:, 1024:2048])
    cdef["GOUT"] = wview(gmlp_w_out[:, :])
    for j in range(3):
        cdef["L%d" % j] = wview(gla_w_in[:, j * 1024:(j + 1) * 1024])
    cdef["LOUT"] = wview(gla_w_out[:, :])
    seq = ["A0_0", "A0_1", "GIN0", "GIN1", "A0_2", "GOUT", "A0_3", "A0_4", "A0_5"]
    for q in range(3):
        seq += ["F0_1_%d" % q, "F0_2_%d" % q]
    seq += ["A1_0", "A1_1", "F0_1_3", "F0_2_3", "A1_2", "A1_3", "A1_4", "A1_5", "L0", "L1", "L2", "LOUT"]
    for q in range(4):
        seq += ["F1_1_%d" % q, "F1_2_%d" % q]
    slotb = [Buf("slot%d" % i) for i in range(NSLOT)]
    wstate = {"next": 0, "free": list(range(NSLOT)), "slot": {}}

    def wpump():
        while wstate["free"] and wstate["next"] < len(seq):
            sl = wstate["free"].pop(0)
            cid = seq[wstate["next"]]
            wstate["next"] += 1
            S.dma(POOL, ring[:, sl, :, :], cdef[cid], r=(), w=(slotb[sl],), sem=S.wsems[sl])
            wstate["slot"][cid] = sl

    def wslot(cid):
        assert cid in wstate["slot"], (cid, wstate["next"])
        return wstate["slot"][cid]

    def wrelease(cid):
        wstate["free"].append(wstate["slot"].pop(cid))
        wpump()
    wpump()

    S.dma(SP, ident, ident_d, w=(b_const,))
    EA.reset()
    prt = EA.alloc([128], F32)
    prt2 = EA.alloc([128], F32, parts=96)
    ct = EA.alloc([D], F32, parts=NB)
    b_pr = Buf("pr")
    S.dma(SP, prt, prow[0:128, :])
    S.dma(SP, prt2, prow[128:224, :])
    S.dma(SP, ct, cin)
    waf = EA.alloc([8, 17], F32)
    S.dma(SP, waf, gla_wa.rearrange("(k p) n -> p k n", p=128))
    S.wait_dmas()
    S.op(DVE, lambda: nc.vector.tensor_copy(out=wa, in_=waf), w=(b_const,))
    S.op(DVE, lambda: nc.vector.tensor_copy(out=identb, in_=ident), r=(b_const,), w=(b_const,))
    S.op(DVE, lambda: nc.vector.memset(onesb, 1.0 / 1024.0), w=(b_const,))
    b0 = palloc(1)
    S.transposes([(ps[:, b0, 0:128], prt, ident), (ps[:, b0, 128:224], prt2, ident[0:96, 0:96])],
                 r=(b_pr, b_const), w=(pb[b0],))
    S.op(DVE, lambda: nc.vector.tensor_copy(out=PF, in_=ps[:, b0, 0:R_TOT]), r=(pb[b0],), w=(b_const,))
    csl = EA.alloc([D], F32, parts=NB)
    b_c = Buf("c")
    S.op(ACT, lambda: nc.scalar.activation(out=csl, in_=ct, func=AF.Silu), r=(b_pr,), w=(b_c,))
    b1_ = palloc(1)
    S.transposes([(ps[:, b1_, k * NB:(k + 1) * NB], csl[:, k * 128:(k + 1) * 128], ident[0:NB, 0:NB]) for k in range(8)],
                 r=(b_c, b_const), w=(pb[b1_],))
    S.op(DVE, lambda: nc.vector.tensor_copy(out=scT, in_=ps[:, b1_, 0:8 * NB].rearrange("p (k b) -> p k b", b=NB)),
         r=(pb[b1_],), w=(b_const,))

    NXS = 8
    xst = [EA.alloc([D], F32) for _ in range(NXS)]
    xsb = [Buf("xs%d" % i) for i in range(NXS)]

    def xload(ti):
        t0 = ti * 128
        n = 128 if ti < 16 else 64
        S.dma(SP, xst[ti % NXS][0:n, :], xin[t0:t0 + n, :], w=(xsb[ti % NXS],))

    for ti in range(NXS):
        xload(ti)
    for ti in range(17):
        t0 = ti * 128
        n = 128 if ti < 16 else 64
        sl = ti % NXS
        b = palloc(2)
        S.transposes([(ps[:, b + k // 4, (k % 4) * 128:(k % 4) * 128 + n], xst[sl][0:n, k * 128:(k + 1) * 128], ident[0:n, 0:n])
                      for k in range(8)], r=(xsb[sl], b_const), w=(pb[b], pb[b + 1]))
        src = ps[:, b:b + 2, :].rearrange("p a (k t) -> p (a k) t", t=128)[:, :, 0:n]
        eng = ACT if ti % 2 == 0 else DVE
        if eng is ACT:
            S.op(ACT, lambda: nc.scalar.copy(out=xT[:, :, t0:t0 + n], in_=src), r=(pb[b], pb[b + 1]), w=xb(t0, n))
        else:
            S.op(DVE, lambda: nc.vector.tensor_copy(out=xT[:, :, t0:t0 + n], in_=src), r=(pb[b], pb[b + 1]), w=xb(t0, n))
        if ti + NXS < 17:
            xload(ti + NXS)

    def modsel(M, k, isp):
        if isp:
            return M[:, k, 0:1]
        return M[:, k, 1:17].unsqueeze(2).to_broadcast([128, 16, 4])

    def v3(ap, isp):
        return ap if isp else ap.rearrange("p (b t) -> p b t", t=4)

    def ada_chunk(l, j):
        MOD = MODS[l]
        bm = b_mods[l]
        cid = "A%d_%d" % (l, j)
        s_ = wslot(cid)
        b = palloc(1)
        groups = []
        for m in range(8):
            groups.append([(ps[:, b, m * NB:(m + 1) * NB], ring[:, s_, k, m * 128:(m + 1) * 128], scT[:, k, :]) for k in range(8)])
        S.mm_multi(groups, r=(slotb[s_], b_const), w=(pb[b],))
        wrelease(cid)
        bias = PF[:, R_ADAB + l * 48 + j * 8:R_ADAB + l * 48 + j * 8 + 8].unsqueeze(2).to_broadcast([128, 8, NB])
        S.op(DVE, lambda: nc.vector.tensor_tensor(out=MOD[:, j, :, :], in0=ps[:, b, 0:8 * NB].rearrange("p (m b) -> p m b", b=NB),
                                                  in1=bias, op=ALU.add), r=(pb[b], b_const), w=(bm,))
        if j == 1:
            nmg = PF[:, R_NMG + l * 8:R_NMG + l * 8 + 8].unsqueeze(2).to_broadcast([128, 8, NB])
            S.op(DVE, lambda: nc.vector.scalar_tensor_tensor(out=MOD[:, 1, :, :], in0=MOD[:, 1, :, :], scalar=1.0, in1=nmg, op0=ALU.add, op1=ALU.mult),
                 r=(bm, b_const), w=(bm,))
        if j == 4:
            nfg = PF[:, R_NFG + l * 8:R_NFG + l * 8 + 8].unsqueeze(2).to_broadcast([128, 8, NB])
            S.op(DVE, lambda: nc.vector.scalar_tensor_tensor(out=MOD[:, 4, :, :], in0=MOD[:, 4, :, :], scalar=1.0, in1=nfg, op0=ALU.add, op1=ALU.mult),
                 r=(bm, b_const), w=(bm,))
        if j == 5:
            b2 = PF[:, R_B2 + l * 8:R_B2 + l * 8 + 8].unsqueeze(2).to_broadcast([128, 8, NB])
            S.op(DVE, lambda: nc.vector.tensor_tensor(out=GB2S[l], in0=MOD[:, 5, :, :], in1=b2, op=ALU.mult), r=(bm, b_const), w=(bm,))
        if j == 2 and l == 0:
            bo = PF[:, R_BOUT:R_BOUT + 8].unsqueeze(2).to_broadcast([128, 8, NB])
            S.op(DVE, lambda: nc.vector.tensor_tensor(out=GB1, in0=MOD[:, 2, :, :], in1=bo, op=ALU.mult), r=(bm, b_const), w=(bm,))

    def norm_tile(t0, n, isp, GM, SH, hout, hbuf, tmps, tb, sq_, sqb, rs_, rsb, b_mod, part=0, so=0):
        sq = sq_[:, :, so:so + n]
        rs = rs_[:, so:so + n]
        if part in (0, 1):
            S.op(ACT, lambda: nc.scalar.activation(out=sq[:, :, 0:n], in_=xT[:, :, t0:t0 + n], func=AF.Square), r=xb(t0, n), w=(sqb,))
        if part == 1:
            return
        b = palloc(1)
        S.mm([(ps[:, b, 0:n], onesb, sq[:, k, 0:n]) for k in range(8)], r=(sqb, b_const), w=(pb[b],))
        S.op(ACT, lambda: nc.scalar.activation(out=rs[:, 0:n], in_=ps[:, b, 0:n], func=AF.Ln, bias=EPS, scale=1.0), r=(pb[b],), w=(rsb,))
        S.op(ACT, lambda: nc.scalar.activation(out=rs[:, 0:n], in_=rs[:, 0:n], func=AF.Exp, scale=-0.5), r=(rsb,), w=(rsb,))
        for k in range(8):
            tmp = tmps[k % 2]
            tbb = tb[k % 2]
            S.op(DVE, lambda: nc.vector.tensor_tensor(out=tmp[:, 0:n], in0=xT[:, k, t0:t0 + n], in1=rs[:, 0:n], op=ALU.mult),
                 r=xb(t0, n) + (rsb,), w=(tbb,))
            if isp:
                S.op(ACT, lambda: nc.scalar.activation(out=hout[:, k, 0:n], in_=tmp[:, 0:n], func=AF.Identity,
                                                       scale=GM[:, k, 0:1], bias=SH[:, k, 0:1]), r=(tbb, b_mod), w=(hbuf,))
            else:
                S.op(DVE, lambda: nc.vector.tensor_tensor(out=v3(tmp[:, 0:n], False), in0=v3(tmp[:, 0:n], False), in1=modsel(GM, k, False), op=ALU.mult),
                     r=(tbb, b_mod), w=(tbb,))
                S.op(DVE, lambda: nc.vector.tensor_tensor(out=v3(hout[:, k, 0:n], False), in0=v3(tmp[:, 0:n], False), in1=modsel(SH, k, False), op=ALU.add),
                     r=(tbb, b_mod), w=(hbuf,))

    def resid_update(b, k, t0, n, isp, G, GB, tmp, tbb, b_mod, pcol=0):
        xs = xT[:, k, t0:t0 + n]
        psv = ps[:, b, pcol:pcol + n]
        if isp:
            if GB is not None:
                S.op(ACT, lambda: nc.scalar.activation(out=tmp[:, 0:n], in_=psv, func=AF.Identity, scale=G[:, k, 0:1], bias=GB[:, k, 0:1]),
                     r=(pb[b], b_mod), w=(tbb,))
            else:
                S.op(DVE, lambda: nc.vector.scalar_tensor_tensor(out=xs, in0=psv, scalar=G[:, k, 0:1], in1=xs, op0=ALU.mult, op1=ALU.add),
                     r=(pb[b], b_mod) + xb(t0, n), w=xb(t0, n))
                return
            S.op(DVE, lambda: nc.vector.tensor_tensor(out=xs, in0=xs, in1=tmp[:, 0:n], op=ALU.add), r=(tbb,) + xb(t0, n), w=xb(t0, n))
        else:
            S.op(DVE, lambda: nc.vector.tensor_tensor(out=v3(tmp[:, 0:n], False), in0=v3(psv, False), in1=modsel(G, k, False), op=ALU.mult),
                 r=(pb[b], b_mod), w=(tbb,))
            if GB is not None:
                S.op(DVE, lambda: nc.vector.tensor_tensor(out=v3(tmp[:, 0:n], False), in0=v3(tmp[:, 0:n], False), in1=modsel(GB, k, False), op=ALU.add),
                     r=(tbb, b_mod), w=(tbb,))
            S.op(DVE, lambda: nc.vector.tensor_tensor(out=xs, in0=xs, in1=tmp[:, 0:n], op=ALU.add), r=(tbb,) + xb(t0, n), w=xb(t0, n))

    def gmlp_phase():
        S.barrier()
        CA.reset()
        EA.reset()
        MOD = MODS[0]
        b_mod = b_mods[0]
        GM1 = MOD[:, 1, :, :]
        s_in0, s_in1, s_out = wslot("GIN0"), wslot("GIN1"), wslot("GOUT")
        binv = CA.alloc([D], F32)
        lng = CA.alloc([D], F32)
        lnb = CA.alloc([D], F32)
        bsp = CA.alloc([4, 128], F32)
        bss = CA.alloc([4, 64], F32)
        wsp = CA.alloc([4, 128], BF16)
        wss = CA.alloc([4, 64], BF16, parts=64)
        vgs = [CA.alloc([D], F32) for _ in range(2)]
        vgb = [Buf("vg0"), Buf("vg1")]
        mark = WA.off
        WA.off = mark - 4096
        cmp_ = CA.alloc([128], F32)
        cms = CA.alloc([64], F32, parts=64)
        wtmp = CA.alloc([4, 128], F32)
        wtmps = CA.alloc([4, 64], F32, parts=64)
        assert WA.off <= mark
        WA.off = mark
        bk = Buf("gk")
        for (dst, src) in [(binv, binv_bc), (lng, lng_bc), (lnb, lnb_bc), (bsp, bs_p), (bss, bs_s), (cmp_, cm_p), (cms, cm_s),
                           (wtmp, wsT_p), (wtmps, wsT_s)]:
            S.dma(SP, dst, src)
        S.wait_dmas()
        S.op(DVE, lambda: nc.vector.tensor_tensor(out=wsp, in0=wtmp, in1=cmp_.unsqueeze(1).to_broadcast([128, 4, 128]), op=ALU.mult), r=(bk,), w=(bk,))
        S.op(DVE, lambda: nc.vector.tensor_tensor(out=wss, in0=wtmps, in1=cms.unsqueeze(1).to_broadcast([64, 4, 64]), op=ALU.mult), r=(bk,), w=(bk, vgb[1]))
        hT = EA.alloc([8, 512], BF16)
        hb = Buf("h")
        sq = EA.alloc([8, 512], BF16)
        sqb = Buf("sq")
        rs = EA.alloc([512], F32)
        rsb = Buf("rs")
        tmps = [EA.alloc([512], F32) for _ in range(2)]
        tb = [Buf("t0"), Buf("t1")]
        uTs = [EA.alloc([8, 512], BF16) for _ in range(2)]
        ubs = [Buf("u0"), Buf("u1")]
        vbfs = [EA.alloc([D], BF16) for _ in range(2)]
        vbb = [Buf("vb0"), Buf("vb1")]
        junk = sq.rearrange("p a b -> p (a b)")
        jb = sqb
        sts = [EA.alloc([8], F32) for _ in range(2)]
        stbs = [Buf("st0"), Buf("st1")]
        SH1 = MOD[:, 0, :, :]
        G1 = MOD[:, 2, :, :]
        print('gMLP arena', WA.off, WA.nbytes)
        pend = []
        for ti_, (t0, n, isp) in enumerate(TILES):
            nsub = (n + 127) // 128
            uT = uTs[ti_ % 2]
            usT = uT
            ub = ubs[ti_ % 2]
            usb = ub

            def U(m):
                b = palloc(1)
                S.mm([(ps[:, b, 0:n], ring[:, s_in0, k, m * 128:(m + 1) * 128], hT[:, k, 0:n]) for k in range(8)],
                     r=(slotb[s_in0], hb), w=(pb[b],))
                S.op(ACT, lambda: nc.scalar.activation(out=uT[:, m, 0:n], in_=ps[:, b, 0:n], func=AF.Gelu_apprx_tanh,
                                                       bias=PF[:, R_BINU + m:R_BINU + m + 1]), r=(pb[b], b_const), w=(ub,))

            def Va(j):
                nt = min(128, n - j * 128)
                vg = vgs[j % 2]
                vb_ = vgb[j % 2]
                vbf = vbfs[j % 2]
                st = sts[j % 2]
                stb = stbs[j % 2]
                b = palloc(2)
                for half in range(2):
                    S.mm([(ps[0:nt, b + half, :], hT[:, k, j * 128:j * 128 + nt], ring[:, s_in1, k, half * 512:(half + 1) * 512]) for k in range(8)],
                         r=(slotb[s_in1], hb), w=(pb[b + half],))
                pv = ps[0:nt, b:b + 2, :].rearrange("p a c -> p (a c)")
                S.op(DVE, lambda: nc.vector.tensor_tensor(out=vg[0:nt, :], in0=pv, in1=binv[0:nt, :], op=ALU.add), r=(pb[b], pb[b + 1], bk), w=(vb_,))
                S.op(ACT, lambda: nc.scalar.activation(out=vg[0:nt, :], in_=vg[0:nt, :], func=AF.Gelu_apprx_tanh, accum_out=st[0:nt, 0:1]),
                     r=(vb_,), w=(vb_, stb))
                S.op(ACT, lambda: nc.scalar.activation(out=junk[0:nt, 0:D], in_=vg[0:nt, :], func=AF.Square, accum_out=st[0:nt, 1:2]),
                     r=(vb_,), w=(jb, stb))
                S.op(DVE, lambda: nc.vector.tensor_scalar(out=st[0:nt, 2:3], in0=st[0:nt, 0:1], scalar1=1.0 / D, scalar2=None, op0=ALU.mult), r=(stb,), w=(stb,))
                S.op(DVE, lambda: nc.vector.tensor_tensor(out=st[0:nt, 3:4], in0=st[0:nt, 2:3], in1=st[0:nt, 2:3], op=ALU.mult), r=(stb,), w=(stb,))
                S.op(DVE, lambda: nc.vector.scalar_tensor_tensor(out=st[0:nt, 4:5], in0=st[0:nt, 1:2], scalar=1.0 / D, in1=st[0:nt, 3:4],
                                                                 op0=ALU.mult, op1=ALU.subtract), r=(stb,), w=(stb,))

            def Vb(j):
                nt = min(128, n - j * 128)
                vg = vgs[j % 2]
                vb_ = vgb[j % 2]
                vbf = vbfs[j % 2]
                st = sts[j % 2]
                stb = stbs[j % 2]
                S.op(ACT, lambda: nc.scalar.activation(out=st[0:nt, 5:6], in_=st[0:nt, 4:5], func=AF.Ln, bias=EPS, scale=1.0), r=(stb,), w=(stb,))
                S.op(ACT, lambda: nc.scalar.activation(out=st[0:nt, 5:6], in_=st[0:nt, 5:6], func=AF.Exp, scale=-0.5), r=(stb,), w=(stb,))
                S.op(DVE, lambda: nc.vector.scalar_tensor_tensor(out=st[0:nt, 6:7], in0=st[0:nt, 2:3], scalar=-1.0, in1=st[0:nt, 5:6],
                                                                 op0=ALU.mult, op1=ALU.mult), r=(stb,), w=(stb,))
                S.op(ACT, lambda: nc.scalar.activation(out=vg[0:nt, :], in_=vg[0:nt, :], func=AF.Identity, scale=st[0:nt, 5:6], bias=st[0:nt, 6:7]),
                     r=(vb_, stb), w=(vb_,))
                S.op(DVE, lambda: nc.vector.tensor_tensor(out=vg[0:nt, :], in0=vg[0:nt, :], in1=lng[0:nt, :], op=ALU.mult), r=(vb_, bk), w=(vb_,))
                if isp:
                    S.op(DVE, lambda: nc.vector.tensor_tensor(out=vbf[0:nt, :], in0=vg[0:nt, :], in1=lnb[0:nt, :], op=ALU.add), r=(vb_, bk), w=(vbb[j % 2],))
                else:
                    S.op(DVE, lambda: nc.vector.tensor_tensor(out=vg[0:nt, :], in0=vg[0:nt, :], in1=lnb[0:nt, :], op=ALU.add), r=(vb_, bk), w=(vb_,))
                    S.dma(SP, v_sample, vg[0:nt, :], r=(vb_,))
                    S.op(ACT, lambda: nc.scalar.copy(out=vbf[0:nt, :], in_=vg[0:nt, :]), r=(vb_,), w=(vbb[j % 2],))

            def SPA(j):
                nt = min(128, n - j * 128)
                vbf = vbfs[j % 2]
                b = palloc(2)
                wsm = wsp if isp else wss
                groups = []
                for m in range(8):
                    o = ps[:, b + m // 4, (m % 4) * 128:(m % 4) * 128 + nt]
                    groups.append([(o, vbf[0:nt, m * 128:(m + 1) * 128], wsm[0:nt, m // 2, 0:nt])])
                S.mm_multi(groups, r=(vbb[j % 2], bk), w=(pb[b], pb[b + 1]))
                bsm = bsp if isp else bss
                for a in range(2):
                    pin = ps[:, b + a, :].rearrange("p (g r i) -> p g r i", r=2, i=128)[:, :, :, 0:nt]
                    bsv = bsm[:, 2 * a:2 * a + 2, 0:nt].unsqueeze(2).to_broadcast([128, 2, 2, nt])
                    tt = tmps[a]
                    tv = tt[:, 0:4 * nt].rearrange("p (g r i) -> p g r i", r=2, i=nt)
                    S.op(DVE, lambda: nc.vector.tensor_tensor(out=tv, in0=pin, in1=bsv, op=ALU.add), r=(pb[b + a], bk), w=(tb[a],))
                    uo = usT[:, 4 * a:4 * a + 4, j * 128:j * 128 + nt]
                    ui = uT[:, 4 * a:4 * a + 4, j * 128:j * 128 + nt]
                    tv2 = tt[:, 0:4 * nt].rearrange("p (m i) -> p m i", i=nt)
                    S.op(DVE, lambda: nc.vector.tensor_tensor(out=uo, in0=tv2, in1=ui, op=ALU.mult), r=(tb[a], ub), w=(usb,))

            def make_O(kk, t0=t0, n=n, isp=isp, usT=usT, usb=usb):
                def O_one():
                    b = palloc(1)
                    S.mm([(ps[:, b, 0:n], ring[:, s_out, m, kk * 128:(kk + 1) * 128], usT[:, m, 0:n]) for m in range(8)],
                         r=(slotb[s_out], usb), w=(pb[b],))
                    resid_update(b, kk, t0, n, isp, G1, GB1, tmps[kk % 2], tb[kk % 2], b_mod)
                return O_one

            def flush(k=99):
                while pend and k > 0:
                    pend.pop(0)()
                    k -= 1

            def next_norm():
                if ti_ + 1 < len(TILES):
                    t1, n1, isp1 = TILES[ti_ + 1]
                    norm_tile(t1, n1, isp1, GM1, SH1, hT, hb, tmps, tb, sq, sqb, rs, rsb, b_mod)

            if ti_ == 0:
                norm_tile(t0, n, isp, GM1, SH1, hT, hb, tmps, tb, sq, sqb, rs, rsb, b_mod)
            if nsub == 4:
                Va(0); Va(1); U(0); U(1); Vb(0); U(2); U(3); Vb(1); U(4); U(5); U(6); U(7)
                SPA(0); Va(2); SPA(1); Va(3)
                next_norm()
                flush(4)
                Vb(2)
                flush(2)
                Vb(3)
                flush()
                SPA(2); SPA(3)
            else:
                Va(0)
                for m in range(4):
                    U(m)
                Vb(0)
                for m in range(4, 8):
                    U(m)
                flush()
                SPA(0)
            if t0 == 0:
                ada_chunk(0, 2)
            for kk in range(8):
                pend.append(make_O(kk))
            if t0 == 0:
                ada_chunk(0, 3)
            elif t0 == 512:
                ada_chunk(0, 4)
            elif t0 == 1024:
                ada_chunk(0, 5)
        while pend:
            pend.pop(0)()
        wrelease("GIN0")
        wrelease("GIN1")
        wrelease("GOUT")

    def ffn_phase(l):
        S.barrier()
        CA.reset()
        EA.reset()
        MOD = MODS[l]
        b_mod = b_mods[l]
        GM2 = MOD[:, 4, :, :]
        GB2 = GB2S[l]
        hall = CA.alloc([8, NTOK], BF16)
        hb = Buf("hall")
        sq = EA.alloc([8, 512], BF16)
        sqb = Buf("sq")
        rs = EA.alloc([512], F32)
        rsb = Buf("rs")
        tmps = [EA.alloc([512], F32) for _ in range(2)]
        tb = [Buf("t0"), Buf("t1")]
        hid = [EA.alloc([8, 512], BF16) for _ in range(2)]
        hidb = [Buf("hid0"), Buf("hid1")]
        rl = [EA.alloc([512], F32) for _ in range(2)]
        rlb = [Buf("rl0"), Buf("rl1")]
        SH2 = MOD[:, 3, :, :]
        G2 = MOD[:, 5, :, :]
        FT = [(i * 448, 448, [(i * 448, 448, True)]) for i in range(4)] + [(1792, 320, [(1792, 256, True), (NPR, NSA, False)])]
        NT_ = len(FT)
        hbs = [Buf("hall%d" % i) for i in range(NT_)]

        def nrm(ti, part, tm, tmb):
            t0_, n_, subs = FT[ti]
            for (st0, sn, sisp) in subs:
                norm_tile(st0, sn, sisp, GM2, SH2, hall[:, :, st0:st0 + sn], hbs[ti], tm, tmb, sq, sqb, rs, rsb, b_mod,
                          part=part, so=st0 - t0_)

        nrm(0, 0, tmps, tb)

        def H(q, ti, hd, hdb):
            t0, n, subs = FT[ti]
            s1 = wslot("F%d_1_%d" % (l, q))
            hb = hbs[ti]
            for f in range(8):
                b = palloc(1)
                S.mm([(ps[:, b, 0:n], ring[:, s1, k, f * 128:(f + 1) * 128], hall[:, k, t0:t0 + n]) for k in range(8)],
                     r=(slotb[s1], hb), w=(pb[b],))
                r_ = rl[f % 2]
                rb_ = rlb[f % 2]
                bcol = PF[:, R_B1 + l * 32 + q * 8 + f:R_B1 + l * 32 + q * 8 + f + 1]
                S.op(ACT, lambda: nc.scalar.activation(out=r_[:, 0:n], in_=ps[:, b, 0:n], func=AF.Relu, bias=bcol), r=(pb[b], b_const), w=(rb_,))
                S.op(DVE, lambda: nc.vector.tensor_tensor(out=hd[:, f, 0:n], in0=r_[:, 0:n], in1=r_[:, 0:n], op=ALU.mult), r=(rb_,), w=(hdb,))

        def Y(q, ti, hd, hdb):
            t0, n, subs = FT[ti]
            s2 = wslot("F%d_2_%d" % (l, q))
            for kk in range(8):
                b = palloc(1)
                S.mm([(ps[:, b, 0:n], ring[:, s2, f, kk * 128:(kk + 1) * 128], hd[:, f, 0:n]) for f in range(8)],
                     r=(slotb[s2], hdb), w=(pb[b],))
                for (st0, sn, sisp) in subs:
                    resid_update(b, kk, st0, sn, sisp, G2, GB2 if q == 0 else None, tmps[kk % 2], tb[kk % 2], b_mod, pcol=st0 - t0)
            if l == 0 and q == 2 and ti == 2:
                ada_chunk(1, 0)
                ada_chunk(1, 1)
            if l == 0 and q == 3:
                if ti == 1:
                    ada_chunk(1, 2)
                    ada_chunk(1, 3)
                elif ti == 3:
                    ada_chunk(1, 4)
                    ada_chunk(1, 5)
            if ti == NT_ - 1:
                wrelease("F%d_1_%d" % (l, q))
                wrelease("F%d_2_%d" % (l, q))

        for ti in range(NT_):
            if ti + 1 < NT_:
                nrm(ti + 1, 1, None, None)
            H(0, ti, hid[ti % 2], hidb[ti % 2])
            if ti + 1 < NT_:
                nrm(ti + 1, 2, rl, rlb)
            Y(0, ti, hid[ti % 2], hidb[ti % 2])
        steps = [(q, ti) for q in range(1, 4) for ti in range(NT_)]
        it = NT_
        H(steps[0][0], steps[0][1], hid[it % 2], hidb[it % 2])
        for si, (q, ti) in enumerate(steps):
            cur = it + si
            if si + 1 < len(steps):
                nq, nti = steps[si + 1]
                H(nq, nti, hid[(cur + 1) % 2], hidb[(cur + 1) % 2])
            Y(q, ti, hid[cur % 2], hidb[cur % 2])

    def gla_phase():
        S.barrier()
        WA.reset()
        MOD = MODS[1]
        b_mod = b_mods[1]
        GM1 = MOD[:, 1, :, :]
        sA, sB, sC, sD = wslot("L0"), wslot("L1"), wslot("L2"), wslot("LOUT")
        bk = Buf("gk")
        wg2 = WA.alloc([512], F32, parts=17)
        ngb = WA.alloc([256], F32)
        cmp_ = WA.alloc([128], F32)
        cms = WA.alloc([64], F32, parts=64)
        tNp = WA.alloc([128], F32)
        tNs = WA.alloc([64], F32, parts=64)
        km = WA.alloc([16], F32, parts=64)
        for (dst, src) in [(wg2, wg2a), (ngb, gng_bc), (cmp_, cm_p), (cms, cm_s), (tNp, triN_p), (tNs, triN_s), (km, kmask)]:
            S.dma(SP, dst, src)
        S.wait_dmas(with_x=True)
        Sf = WA.alloc([4, 256], F32)
        Sb = WA.alloc([4, 256], BF16)
        Sfb = Buf("Sf")
        Sbb = Buf("Sb")
        junk = WA.alloc([256], BF16)
        jb = Buf("junk")

        class Set:
            pass

        set_off = []
        sets = []
        for si in range(2):
            set_off.append(WA.off)
            B = Set()
            B.hT = WA.alloc([8, 128], BF16); B.hb = Buf("h")
            B.sq = WA.alloc([8, 128], BF16); B.sqb = Buf("sq")
            B.ogT = WA.alloc([8, 128], BF16); B.ogTb = Buf("ogT")
            B.rs = WA.alloc([128], F32); B.rsb = Buf("rs")
            B.tmps = [WA.alloc([128], F32) for _ in range(2)]; B.tb = [Buf("t0"), Buf("t1")]
            B.ntmps = [WA.alloc([128], F32) for _ in range(2)]; B.ntb = [Buf("nt0"), Buf("nt1")]
            B.qkT = WA.alloc([8, 128], F32); B.qkb = [Buf("q"), Buf("k")]; B.ksb = Buf("ks")
            B.aaug = WA.alloc([128], F32, parts=17); B.aab = Buf("aa")
            B.sp = WA.alloc([512], F32); B.spb = Buf("sp")
            B.eb = WA.alloc([4, 128], F32); B.ebb = Buf("eb")
            B.enb = B.sp.rearrange("p (h t) -> p h t", t=128)
            B.qs = WA.alloc([4, 128], BF16); B.ks = WA.alloc([4, 128], BF16); B.qsb = Buf("qs")
            B.khT = WA.alloc([4, 128], BF16); B.khTb = Buf("khT")
            B.kh = WA.alloc([512], BF16); B.khb = Buf("kh")
            B.vb = WA.alloc([D], BF16); B.vbb = Buf("vb")
            B.sg = WA.alloc([D], F32); B.sgb = Buf("sg")
            B.am = B.khT; B.amb = B.khTb
            B.ogh = B.sp.bitcast(BF16); B.oghb = B.spb
            B.st = WA.alloc([16], F32); B.stb = Buf("st")
            sets.append(B)
        set_end = WA.off
        print('GLA arena', WA.off, WA.nbytes)
        S.op(DVE, lambda: nc.vector.memset(Sf, 0.0), w=(Sfb,))
        S.op(DVE, lambda: nc.vector.memset(Sb, 0.0), w=(Sbb,))
        SH1 = MOD[:, 0, :, :]
        G1 = MOD[:, 2, :, :]
        SCALE = 128.0 ** -0.5
        GT = [(i * 128, 128, True) for i in range(NPR // 128)] + [(NPR, NSA, False)]
        def chunk_gen(ci, t0, n, isp):
            B = sets[ci % 2]
            nt = n
            hT = B.hT
            norm_tile(t0, n, isp, GM1, SH1, hT, B.hb, B.ntmps, B.ntb, B.sq, B.sqb, B.rs, B.rsb, b_mod)
            tN = tNp if isp else tNs
            cm = cmp_ if isp else cms
            yield 'F0'
            b = palloc(1)
            S.mm([(ps[0:17, b, 0:nt], wa[:, k, :], hT[:, k, 0:nt]) for k in range(8)], r=(bk, B.hb), w=(pb[b],))
            S.op(ACT, lambda: nc.scalar.copy(out=B.aaug[0:17, 0:nt], in_=ps[0:17, b, 0:nt]), r=(pb[b],), w=(B.aab,))
            S.op(DVE, lambda: nc.vector.memset(B.aaug[0:1, 0:nt], 1.0), w=(B.aab,))
            yield 'F1'
            bq = palloc(2)
            pstate["res"] |= {bq, bq + 1}

            def qk_groups(ms):
                for m in ms:
                    S.mm([(ps[:, bq + m // 4, (m % 4) * 128:(m % 4) * 128 + nt], ring[:, sA, k, m * 128:(m + 1) * 128], hT[:, k, 0:nt]) for k in range(8)],
                         r=(slotb[sA], B.hb), w=(pb[bq + m // 4],))

            def qk_copy(a_):
                S.op(ACT, lambda: nc.scalar.copy(out=B.qkT[:, 4 * a_:4 * a_ + 4, 0:nt],
                                                 in_=ps[:, bq + a_, :].rearrange("p (m t) -> p m t", t=128)[:, :, 0:nt]),
                     r=(pb[bq + a_],), w=(B.qkb[a_],))
            qk_groups([0, 1])
            yield 'F2'
            qk_groups([2, 3])
            qk_copy(0)
            yield 'F3'
            bx = palloc(1)
            S.mm([(ps[0:nt, bx, :], B.aaug[0:17, 0:nt], wg2[0:17, :])], r=(B.aab, bk), w=(pb[bx],))
            S.op(ACT, lambda: nc.scalar.activation(out=B.sp[0:nt, :], in_=ps[0:nt, bx, :], func=AF.Exp, scale=-1.0), r=(pb[bx],), w=(B.spb,))
            S.op(ACT, lambda: nc.scalar.activation(out=B.sp[0:nt, :], in_=B.sp[0:nt, :], func=AF.Ln, bias=1.0, scale=1.0), r=(B.spb,), w=(B.spb,))
            yield 'F4'
            qk_groups([4, 5])
            yield 'F5'
            qk_groups([6, 7])
            qk_copy(1)
            pstate["res"] -= {bq, bq + 1}
            yield 'F6'
            bb = palloc(1)
            S.mm_multi([[(ps[:, bb, hd * 128:hd * 128 + nt], B.sp[0:nt, hd * 128:(hd + 1) * 128], tN[0:nt, 0:nt])] for hd in range(4)],
                       r=(B.spb, bk), w=(pb[bb],))
            bT = ps[:, bb, :].rearrange("p (h t) -> p h t", t=128)[:, :, 0:nt]
            eb = B.eb
            enb = B.enb
            S.op(ACT, lambda: nc.scalar.activation(out=eb[:, :, 0:nt], in_=bT, func=AF.Exp), r=(pb[bb],), w=(B.ebb,))
            S.op(ACT, lambda: nc.scalar.activation(out=enb[:, :, 0:nt], in_=bT, func=AF.Exp, scale=-1.0), r=(pb[bb],), w=(B.spb,))
            S.op(DVE, lambda: nc.vector.scalar_tensor_tensor(out=B.qs[:, :, 0:nt], in0=B.qkT[:, 0:4, 0:nt], scalar=SCALE, in1=eb[:, :, 0:nt],
                                                             op0=ALU.mult, op1=ALU.mult), r=(B.qkb[0], B.ebb), w=(B.qsb,))
            S.op(DVE, lambda: nc.vector.tensor_tensor(out=B.ks[:, :, 0:nt], in0=B.qkT[:, 4:8, 0:nt], in1=enb[:, :, 0:nt], op=ALU.mult),
                 r=(B.qkb[1], B.spb), w=(B.ksb,))
            if isp:
                for hd in range(4):
                    S.op(DVE, lambda: nc.vector.scalar_tensor_tensor(out=B.khT[:, hd, 0:nt], in0=B.qkT[:, 4 + hd, 0:nt], scalar=eb[:, hd, nt - 1:nt],
                                                                     in1=enb[:, hd, 0:nt], op0=ALU.mult, op1=ALU.mult),
                         r=(B.qkb[1], B.ebb, B.spb), w=(B.khTb,))
            else:
                tmpk = B.ntmps[0]
                for hd in range(4):
                    el = eb[:, hd, 0:nt].rearrange("p (b t) -> p b t", t=4)[:, :, 3:4].to_broadcast([128, 16, 4])
                    S.op(DVE, lambda: nc.vector.tensor_tensor(out=v3(tmpk[:, 0:nt], False), in0=v3(B.qkT[:, 4 + hd, 0:nt], False), in1=el, op=ALU.mult),
                         r=(B.qkb[1], B.ebb), w=(B.ntb[0],))
                    S.op(DVE, lambda: nc.vector.tensor_tensor(out=B.khT[:, hd, 0:nt], in0=tmpk[:, 0:nt], in1=enb[:, hd, 0:nt], op=ALU.mult),
                         r=(B.ntb[0], B.spb), w=(B.khTb,))
            yield 'F7'
            bv = palloc(2)
            pstate["res"] |= {bv, bv + 1}
            S.mm([(ps[0:nt, bv, :], hT[:, k, 0:nt], ring[:, sB, k, 0:512]) for k in range(8)], r=(slotb[sB], B.hb), w=(pb[bv],))
            yield 'F8'
            S.mm([(ps[0:nt, bv + 1, :], hT[:, k, 0:nt], ring[:, sB, k, 512:1024]) for k in range(8)], r=(slotb[sB], B.hb), w=(pb[bv + 1],))
            S.op(ACT, lambda: nc.scalar.copy(out=B.vb[0:nt, :], in_=ps[0:nt, bv:bv + 2, :].rearrange("p a c -> p (a c)")),
                 r=(pb[bv], pb[bv + 1]), w=(B.vbb,))
            pstate["res"] -= {bv, bv + 1}
            yield 'F9'
            bg = palloc(2)
            pstate["res"] |= {bg, bg + 1}
            S.mm([(ps[0:nt, bg, :], hT[:, k, 0:nt], ring[:, sC, k, 0:512]) for k in range(8)], r=(slotb[sC], B.hb), w=(pb[bg],))
            yield 'F10'
            S.mm([(ps[0:nt, bg + 1, :], hT[:, k, 0:nt], ring[:, sC, k, 512:1024]) for k in range(8)], r=(slotb[sC], B.hb), w=(pb[bg + 1],))
            gps = ps[0:nt, bg:bg + 2, :].rearrange("p a c -> p (a c)")
            S.op(ACT, lambda: nc.scalar.activation(out=B.sg[0:nt, :], in_=gps, func=AF.Exp, scale=-1.0), r=(pb[bg], pb[bg + 1]), w=(B.sgb,))
            S.op(ACT, lambda: nc.scalar.activation(out=B.sg[0:nt, :], in_=B.sg[0:nt, :], func=AF.Ln, bias=1.0, scale=1.0), r=(B.sgb,), w=(B.sgb,))
            S.op(ACT, lambda: nc.scalar.activation(out=B.sg[0:nt, :], in_=B.sg[0:nt, :], func=AF.Exp, scale=-1.0), r=(B.sgb,), w=(B.sgb,))
            S.op(DVE, lambda: nc.vector.tensor_tensor(out=B.sg[0:nt, :], in0=gps, in1=B.sg[0:nt, :], op=ALU.mult), r=(pb[bg], pb[bg + 1], B.sgb), w=(B.sgb,))
            sg4 = B.sg[0:nt, :].rearrange("p (h v) -> p h v", v=256)
            S.op(DVE, lambda: nc.vector.tensor_tensor(out=sg4, in0=sg4, in1=ngb[0:nt, :].unsqueeze(1).to_broadcast([nt, 4, 256]), op=ALU.mult),
                 r=(B.sgb, bk), w=(B.sgb,))
            pstate["res"] -= {bg, bg + 1}
            yield 'F11'
            bkt = palloc(1)
            pkt = ps[:, bkt, :].bitcast(BF16)
            S.transposes([(pkt[0:nt, hd * 128:(hd + 1) * 128], B.khT[:, hd, 0:nt], identb) for hd in range(4)], r=(B.khTb, b_const), w=(pb[bkt],))
            S.op(ACT, lambda: nc.scalar.copy(out=B.kh[0:nt, :], in_=pkt[0:nt, 0:512]), r=(pb[bkt],), w=(B.khb,))
            yield 'B0'
            ba = palloc(1)
            S.mm_multi([[(ps[0:nt, ba, hd * 128:hd * 128 + nt], B.ks[:, hd, 0:nt], B.qs[:, hd, 0:nt])] for hd in range(4)],
                       r=(B.qsb, B.ksb), w=(pb[ba],))
            aT = ps[0:nt, ba, :].rearrange("p (h t) -> p h t", t=128)[:, :, 0:nt]
            S.op(DVE, lambda: nc.vector.tensor_tensor(out=B.am[0:nt, :, 0:nt], in0=aT, in1=cm[0:nt, 0:nt].unsqueeze(1).to_broadcast([nt, 4, nt]),
                                                      op=ALU.mult), r=(pb[ba], bk), w=(B.amb,))
            yield 'B1'
            def o_post(obanks, ocols):
                st = B.st
                for hd in range(4):
                    o = ps[0:nt, obanks[hd], ocols[hd]:ocols[hd] + 256]
                    S.op(ACT, lambda: nc.scalar.activation(out=junk[0:nt, :], in_=o, func=AF.Square, accum_out=st[0:nt, hd:hd + 1]),
                         r=(pb[obanks[hd]],), w=(jb, B.stb))
                S.op(ACT, lambda: nc.scalar.activation(out=st[0:nt, 4:8], in_=st[0:nt, 0:4], func=AF.Ln, bias=EPS, scale=1.0 / 256.0), r=(B.stb,), w=(B.stb,))
                S.op(ACT, lambda: nc.scalar.activation(out=st[0:nt, 4:8], in_=st[0:nt, 4:8], func=AF.Exp, scale=-0.5), r=(B.stb,), w=(B.stb,))
                for hd in range(4):
                    o = ps[0:nt, obanks[hd], ocols[hd]:ocols[hd] + 256]
                    S.op(DVE, lambda: nc.vector.scalar_tensor_tensor(out=B.ogh[0:nt, hd * 256:(hd + 1) * 256], in0=o, scalar=st[0:nt, 4 + hd:5 + hd],
                                                                     in1=B.sg[0:nt, hd * 256:(hd + 1) * 256], op0=ALU.mult, op1=ALU.mult),
                         r=(pb[obanks[hd]], B.stb, B.sgb), w=(B.oghb,))
                pstate["res"] -= set(obanks)
            if isp:
                bo = palloc(2)
                pstate["res"] |= {bo, bo + 1}
                obanks = [bo, bo, bo + 1, bo + 1]
                ocols = [0, 256, 0, 256]
                for hd in range(4):
                    o = ps[0:nt, obanks[hd], ocols[hd]:ocols[hd] + 256]
                    S.mm([(o, B.qs[:, hd, 0:nt], Sb[:, hd, :]), (o, B.am[0:nt, hd, 0:nt], B.vb[0:nt, hd * 256:(hd + 1) * 256])],
                         r=(B.qsb, Sbb, B.amb, B.vbb), w=(pb[obanks[hd]],))
                o_post(obanks, ocols)
                yield 'B2'
                bs_ = palloc(2)
                S.mm_multi([[(ps[:, bs_ + hd // 2, (hd % 2) * 256:(hd % 2) * 256 + 256], B.kh[0:nt, hd * 128:(hd + 1) * 128],
                              B.vb[0:nt, hd * 256:(hd + 1) * 256])] for hd in range(4)], r=(B.khb, B.vbb), w=(pb[bs_], pb[bs_ + 1]))
                for hd in range(4):
                    S.op(DVE, lambda: nc.vector.scalar_tensor_tensor(out=Sf[:, hd, :], in0=Sf[:, hd, :], scalar=eb[:, hd, nt - 1:nt],
                                                                     in1=ps[:, bs_ + hd // 2, (hd % 2) * 256:(hd % 2) * 256 + 256],
                                                                     op0=ALU.mult, op1=ALU.add), r=(Sfb, B.ebb, pb[bs_ + hd // 2]), w=(Sfb,))
                S.op(ACT, lambda: nc.scalar.copy(out=Sb, in_=Sf), r=(Sfb,), w=(Sbb,))
                if t0 + nt == NPR:
                    S.dma(SP, st_prompt, Sf, r=(Sfb,))
            else:
                S.barrier(engs=[SP, DVE])
                other = 1 - (ci % 2)
                save_off = WA.off
                WA.off = set_off[other]
                NL = 3
                NSL = 2
                s0 = [WA.alloc([4, 256], F32) for _ in range(NL)]
                s0b = [Buf("s0%d" % i) for i in range(NL)]
                sn = [WA.alloc([4, 256], F32) for _ in range(2)]
                snb = [Buf("sn0"), Buf("sn1")]
                s0h = [WA.alloc([4, 256], BF16) for _ in range(NSL)]
                s0hb = [Buf("s0h%d" % i) for i in range(NSL)]
                Qb = [WA.alloc([4, 64], BF16) for _ in range(NSL)]
                Qbb = [Buf("Qb%d" % i) for i in range(NSL)]
                Kb = [WA.alloc([512], BF16, parts=64) for _ in range(NSL)]
                Kbb = [Buf("Kb%d" % i) for i in range(NSL)]
                lim = set_end if other == 1 else set_off[1]
                assert WA.off <= lim, (WA.off, lim)
                WA.off = save_off
                ob4 = [0, 1, 2, 3]
                pstate["res"] = set(ob4)
                obanks = ob4
                ocols = [0, 0, 0, 0]
                for i in range(NSL):
                    S.op(DVE, lambda: nc.vector.memset(Qb[i], 0.0), w=(Qbb[i],))
                for i in range(NL):
                    S.dma(SP, s0[i], sgl[i], w=(s0b[i],))

                def prep(bi):
                    sl = bi % NSL
                    if bi >= NSL:
                        pv_ = bi - NSL
                        S.op(DVE, lambda: nc.vector.memset(Qb[sl][:, :, 4 * pv_:4 * pv_ + 4], 0.0), w=(Qbb[sl],))
                    S.op(DVE, lambda: nc.vector.tensor_copy(out=Qb[sl][:, :, 4 * bi:4 * bi + 4], in_=B.qs[:, :, 4 * bi:4 * bi + 4]),
                         r=(B.qsb,), w=(Qbb[sl],))
                    S.op(DVE, lambda: nc.vector.tensor_scalar(out=Kb[sl], in0=B.kh[0:64, :], scalar1=km[:, bi:bi + 1], scalar2=None, op0=ALU.mult),
                         r=(B.khb,), w=(Kbb[sl],))

                prep(0)
                for bi in range(16):
                    sl = bi % NSL
                    ll = bi % NL
                    S.op(ACT, lambda: nc.scalar.copy(out=s0h[sl], in_=s0[ll]), r=(s0b[ll],), w=(s0hb[sl],))
                    for hd in range(4):
                        o = ps[0:64, ob4[hd], 0:256]
                        S._waits(PE, (Qbb[sl], s0hb[sl]), (pb[ob4[hd]],))
                        ins = nc.tensor.matmul(o, lhsT=Qb[sl][:, hd, :], rhs=s0h[sl][:, hd, :], start=(bi == 0), stop=False)
                        PE.cnt += 1
                        ins.then_inc(PE.sem, 1)
                        s0hb[sl].r[PE] = PE.cnt
                        Qbb[sl].r[PE] = PE.cnt
                        pb[ob4[hd]].w = (PE, PE.cnt)
                        pb[ob4[hd]].r = {}
                    bs_ = palloc(2)
                    S.mm_multi([[(ps[:, bs_ + hd // 2, (hd % 2) * 256:(hd % 2) * 256 + 256], Kb[sl][:, hd * 128:(hd + 1) * 128],
                                  B.vb[0:64, hd * 256:(hd + 1) * 256])] for hd in range(4)], r=(Kbb[sl], B.vbb), w=(pb[bs_], pb[bs_ + 1]))
                    if bi + 1 < 16:
                        prep(bi + 1)
                    for hd in range(4):
                        S.op(DVE, lambda: nc.vector.scalar_tensor_tensor(out=sn[bi % 2][:, hd, :], in0=s0[ll][:, hd, :], scalar=eb[:, hd, 4 * bi + 3:4 * bi + 4],
                                                                         in1=ps[:, bs_ + hd // 2, (hd % 2) * 256:(hd % 2) * 256 + 256],
                                                                         op0=ALU.mult, op1=ALU.add), r=(s0b[ll], B.ebb, pb[bs_ + hd // 2]), w=(snb[bi % 2],))
                    S.dma(SP, st_sample[bi], sn[bi % 2], r=(snb[bi % 2],))
                    if bi + NL < 16:
                        S.dma(SP, s0[ll], sgl[bi + NL], w=(s0b[ll],))
                for hd in range(4):
                    o = ps[0:64, ob4[hd], 0:256]
                    S._waits(PE, (B.amb, B.vbb), (pb[ob4[hd]],))
                    ins = nc.tensor.matmul(o, lhsT=B.am[0:64, hd, 0:64], rhs=B.vb[0:64, hd * 256:(hd + 1) * 256], start=False, stop=True)
                    PE.cnt += 1
                    ins.then_inc(PE.sem, 1)
                    B.amb.r[PE] = PE.cnt
                    B.vbb.r[PE] = PE.cnt
                    pb[ob4[hd]].w = (PE, PE.cnt)
                    pb[ob4[hd]].r = {}
                o_post(obanks, ocols)
                yield 'B2'
            yield 'B3'
            bt = palloc(1)
            ptb = ps[:, bt, :].bitcast(BF16)
            S.transposes([(ptb[:, m * 128:m * 128 + nt], B.ogh[0:nt, m * 128:(m + 1) * 128], identb[0:nt, 0:nt]) for m in range(8)],
                         r=(B.oghb, b_const), w=(pb[bt],))
            ogT = B.ogT
            S.op(ACT, lambda: nc.scalar.copy(out=ogT[:, :, 0:nt], in_=ptb.rearrange("p (m t) -> p m t", t=128)[:, :, 0:nt]), r=(pb[bt],), w=(B.ogTb,))
            yield 'B4'
            for kk in range(8):
                if kk % 2 == 0:
                    bo2 = palloc(1)
                S.mm([(ps[:, bo2, (kk % 2) * 128:(kk % 2) * 128 + nt], ring[:, sD, m, kk * 128:(kk + 1) * 128], ogT[:, m, 0:nt]) for m in range(8)],
                     r=(slotb[sD], B.ogTb), w=(pb[bo2],))
                if kk % 2 == 1:
                    for k2 in (kk - 1, kk):
                        resid_update(bo2, k2, t0, n, isp, G1, None, B.tmps[k2 % 2], B.tb[k2 % 2], b_mod, pcol=(k2 % 2) * 128)
                    yield 'B%d' % (5 + kk // 2)

        gens = [chunk_gen(ci, t0, n, isp) for ci, (t0, n, isp) in enumerate(GT)]
        NG = len(gens)

        def step(c, want):
            if 0 <= c < NG:
                got = next(gens[c])
                assert got == want, (c, got, want)

        step(0, 'F0')
        for c in range(NG + 1):
            for i in range(1, 12):
                step(c, 'F%d' % i)
                if i == 7:
                    step(c + 1, 'F0')
                if i - 1 <= 8:
                    step(c - 1, 'B%d' % (i - 1))
        for cid in ("L0", "L1", "L2", "LOUT"):
            wrelease(cid)

    def final_phase():
        S.barrier()
        CA.reset()
        EA.reset()
        fng = CA.alloc([D], F32)
        bk = Buf("fk")
        S.dma(SP, fng, fng_bc, w=(bk,))
        yo = [EA.alloc([D], F32) for _ in range(2)]
        yob = [Buf("y0"), Buf("y1")]
        junk = EA.alloc([D], BF16)
        jb = Buf("junk")
        st = EA.alloc([4, 2], F32)
        stb = [Buf("st0"), Buf("st1")]
        for ti in range(17):
            t0 = ti * 128
            n = 128 if ti < 16 else 64
            sl = ti % 2
            b = palloc(2)
            S.transposes([(ps[0:n, b + k // 4, (k % 4) * 128:(k % 4 + 1) * 128], xT[:, k, t0:t0 + n], ident) for k in range(8)],
                         r=xb(t0, n) + (b_const,), w=(pb[b], pb[b + 1]))
            pin = ps[0:n, b:b + 2, :].rearrange("p a c -> p (a c)")
            S.op(ACT, lambda: nc.scalar.activation(out=junk[0:n, :], in_=pin, func=AF.Square, accum_out=st[0:n, sl, 0:1]),
                 r=(pb[b], pb[b + 1]), w=(jb, stb[sl]))
            S.op(ACT, lambda: nc.scalar.activation(out=st[0:n, sl, 1:2], in_=st[0:n, sl, 0:1], func=AF.Ln, bias=EPS, scale=1.0 / D), r=(stb[sl],), w=(stb[sl],))
            S.op(ACT, lambda: nc.scalar.activation(out=st[0:n, sl, 1:2], in_=st[0:n, sl, 1:2], func=AF.Exp, scale=-0.5), r=(stb[sl],), w=(stb[sl],))
            S.op(DVE, lambda: nc.vector.scalar_tensor_tensor(out=yo[sl][0:n, :], in0=pin, scalar=st[0:n, sl, 1:2], in1=fng[0:n, :],
                                                             op0=ALU.mult, op1=ALU.mult), r=(pb[b], pb[b + 1], stb[sl], bk), w=(yob[sl],))
            if ti < 16:
                S.dma(SP, y_prompt[t0:t0 + n, :], yo[sl][0:n, :], r=(yob[sl],))
            else:
                S.dma(SP, y_sample, yo[sl][0:n, :], r=(yob[sl],))

    S.barrier()
    ada_chunk(0, 0)
    ada_chunk(0, 1)
    gmlp_phase()
    ffn_phase(0)
    gla_phase()
    ffn_phase(1)
    final_phase()
    for d in S.dsems:
        if d.cnt > 0:
            nc.sync.wait_ge(d.sem, d.cnt)
    for e in (S.PE, S.ACT, S.DVE):
        nc.sync.wait_ge(e.sem, e.cnt)
    return nc


def _prep_shared(inp):
    f = np.float32
    g = lambda k: np.ascontiguousarray(np.asarray(inp[k], dtype=f))
    sh = {}
    for k in ("ada_w", "ffn_w1", "ffn_w2", "gmlp_w_in", "gmlp_w_out", "gla_w_in", "gla_w_out"):
        sh[k] = g(k)
    sh["gla_wa"] = np.ascontiguousarray(np.concatenate([np.zeros((D, 1), f), sh["gla_w_in"][:, 3072:3088]], axis=1))
    rows = [g("ada_b").reshape(96, 128), g("norm_mix_g").reshape(16, 128), g("norm_ffn_g").reshape(16, 128),
            g("ffn_b1").reshape(64, 128), g("ffn_b2").reshape(16, 128), g("gmlp_b_in")[:D].reshape(8, 128),
            g("gmlp_b_out").reshape(8, 128)]
    sh["prow"] = np.ascontiguousarray(np.concatenate(rows, axis=0))
    rep = lambda v: np.ascontiguousarray(np.broadcast_to(v[None, :], (128, v.shape[0])))
    sh["binv_bc"] = rep(g("gmlp_b_in")[D:])
    sh["lng_bc"] = rep(g("gmlp_ln_g"))
    sh["lnb_bc"] = rep(g("gmlp_ln_b"))
    sh["fng_bc"] = rep(g("final_norm_g"))
    sh["gng_bc"] = rep(g("gla_norm_g"))
    ws = g("gmlp_w_s")
    sh["wsT_p"] = np.ascontiguousarray(ws.transpose(2, 0, 1))
    idx = np.arange(64) % 4
    sh["wsT_s"] = np.ascontiguousarray(ws[:, idx[None, :], idx[:, None]].transpose(1, 0, 2))
    bs = g("gmlp_b_s")
    sh["bs_p"] = np.ascontiguousarray(np.broadcast_to(bs[None], (128, 4, 128)))
    sh["bs_s"] = np.ascontiguousarray(np.broadcast_to(bs[None][:, :, idx], (128, 4, 64)))
    j = np.arange(128)
    sh["cm_p"] = (j[:, None] <= j[None, :]).astype(f)
    j6 = np.arange(64)
    same = (j6[:, None] // 4) == (j6[None, :] // 4)
    sh["cm_s"] = ((j6[:, None] <= j6[None, :]) & same).astype(f)
    sh["triN_p"] = (sh["cm_p"] * (-1.0 / 16.0)).astype(f)
    sh["triR_p"] = ((j[:, None] > j[None, :]).astype(f) * (-1.0 / 16.0)).astype(f)
    sh["triN_s"] = (sh["cm_s"] * (-1.0 / 16.0)).astype(f)
    sh["triR_s"] = (((j6[:, None] > j6[None, :]) & same).astype(f) * (-1.0 / 16.0)).astype(f)
    qm = np.zeros((128, 16, 64), f)
    for b in range(16):
        qm[:, b, 4 * b:4 * b + 4] = 1.0
    sh["qmask"] = qm
    km = np.zeros((64, 16), f)
    for b in range(16):
        km[4 * b:4 * b + 4, b] = 1.0
    sh["kmask"] = km
    sh["wg2a"] = np.ascontiguousarray(np.concatenate([g("gla_b_gate")[None, :], g("gla_w_gate2")], axis=0))
    sh["ident"] = np.eye(128, dtype=f)
    return sh


_NC_CACHE = {}


def kernel(**inp):
    f = np.float32
    sh = _prep_shared(inp)
    xp = np.asarray(inp["x_prompt"], f)
    xs = np.asarray(inp["x_sample"], f)
    cp = np.asarray(inp["c_prompt"], f)
    cs = np.asarray(inp["c_sample"], f)
    sg = np.asarray(inp["state_gla"], f)
    in_maps = []
    for c in range(8):
        m = dict(sh)
        m["xin"] = np.ascontiguousarray(np.concatenate([xp[c], xs[16 * c:16 * c + 16].reshape(64, D)], axis=0))
        m["cin"] = np.ascontiguousarray(np.concatenate([cp[c:c + 1], cs[16 * c:16 * c + 16]], axis=0))
        m["sgl"] = np.ascontiguousarray(sg[16 * c:16 * c + 16].transpose(0, 2, 1, 3))
        in_maps.append(m)
    if "nc" not in _NC_CACHE:
        _NC_CACHE["nc"] = build()
    nc = _NC_CACHE["nc"]
    res = run_bass_kernel_spmd(nc, in_maps, core_ids=list(range(8)))
    R = res.results
    y_prompt = np.stack([R[c]["y_prompt"] for c in range(8)], axis=0).astype(f)
    y_sample = np.concatenate([R[c]["y_sample"].reshape(16, 4, D) for c in range(8)], axis=0).astype(f)
    st_p = np.stack([np.asarray(R[c]["st_prompt"]).transpose(1, 0, 2) for c in range(8)], axis=0).astype(f)
    st_s = np.concatenate([np.asarray(R[c]["st_sample"]).transpose(0, 2, 1, 3) for c in range(8)], axis=0).astype(f)
    v_s = np.concatenate([R[c]["v_sample"].reshape(16, 4, D) for c in range(8)], axis=0).astype(f)
    return (y_prompt, y_sample, st_p, st_s, v_s)
```
